# Optimizing a Trainium2 kernel written in Bass

```python
import jax, jax.numpy as jnp
from jax import lax
import numpy as np

D_MODEL = 1024
BATCH = 4
SEQ = 8192
DEPTH = 1

HG_HEADS = 8
HG_DK = 128
HG_DV = D_MODEL // HG_HEADS
HG_DIM_K = HG_HEADS * HG_DK
HG_DIM_V = HG_HEADS * HG_DV
RET_HEADS = 4
RET_DK = 256
RET_DV = 256
RET_DIM_K = RET_HEADS * RET_DK
RET_DIM_V = RET_HEADS * RET_DV
ROPE_BASE = 10000.0
MAX_POS_OFFSET = 4096
CHUNK = 64
PROJ_WIDTHS = (HG_DIM_K, HG_DIM_K, HG_DIM_K, HG_DIM_V, HG_DIM_V, RET_DIM_K, RET_DIM_K, RET_DIM_V, RET_DIM_V, D_MODEL, D_MODEL)
PROJ_DIM = HG_DIM_K * 3 + HG_DIM_V * 2 + RET_DIM_K * 2 + RET_DIM_V * 2 + D_MODEL * 2
N_GROUPS = 4
EXPERTS_PER_GROUP = 8
N_EXPERTS = N_GROUPS * EXPERTS_PER_GROUP
TOP_K = 2
D_EXPERT = 512
MOE_BLOCK = 128
DEEPNORM_ALPHA = (2 * DEPTH) ** 0.25
DEEPNORM_BETA = (8 * DEPTH) ** -0.25
LN_EPS = 1e-5
RMS_EPS = 1e-6

kernel_name = 'hybrid_hgrn2_retention_hmoe_deepnorm'


def _layer_norm(x, g, b):
    xf = x.astype(jnp.float32)
    mu = jnp.mean(xf, -1, keepdims=True)
    var = jnp.mean(jnp.square(xf - mu), -1, keepdims=True)
    return ((xf - mu) * lax.rsqrt(var + LN_EPS) * g + b).astype(x.dtype)


def _rms_norm_heads(o, g):
    of = o.astype(jnp.float32)
    of = of * lax.rsqrt(jnp.mean(of * of, -1, keepdims=True) + RMS_EPS)
    return of.reshape(*o.shape[:-2], -1) * g


def _to_chunks(t):
    b, l, h, d = t.shape
    return t.reshape(b, l // CHUNK, CHUNK, h, d).transpose(1, 0, 3, 2, 4)


def _from_chunks(t):
    n, b, h, c, d = t.shape
    return t.transpose(1, 0, 3, 2, 4).reshape(b, n * c, h, d)


def _hgrn2_scan(q, k, v, log_f):
    dt = v.dtype
    qc, kc, vc, lfc = (_to_chunks(t.astype(jnp.float32)) for t in (q, k, v, log_f))
    b, h, dk, dv = qc.shape[1], qc.shape[2], qc.shape[-1], vc.shape[-1]
    causal = jnp.tril(jnp.ones((CHUNK, CHUNK), bool))[:, :, None]

    def step(S, inp):
        qb, kb, vb, lf = inp
        G = jnp.cumsum(lf, axis=2)
        g_last = G[:, :, -1:, :]
        decay = jnp.exp(jnp.where(causal, G[:, :, :, None, :] - G[:, :, None, :, :], -jnp.inf))
        attn = jnp.einsum('bhtd,bhsd,bhtsd->bhts', qb, kb, decay)
        o = jnp.einsum('bhts,bhsv->bhtv', attn, vb) + jnp.einsum('bhtd,bhdv->bhtv', qb * jnp.exp(G), S)
        S = jnp.exp(g_last[:, :, 0, :, None]) * S + jnp.einsum('bhsd,bhsv->bhdv', kb * jnp.exp(g_last - G), vb)
        return S, o

    S0 = jnp.zeros((b, h, dk, dv), jnp.float32)
    _, o = lax.scan(step, S0, (qc, kc, vc, lfc))
    return _from_chunks(o).astype(dt)


def _retention_scan(q, k, v, log_gamma):
    dt = v.dtype
    qc, kc, vc = (_to_chunks(t.astype(jnp.float32)) for t in (q, k, v))
    b, h, dk, dv = qc.shape[1], qc.shape[2], qc.shape[-1], vc.shape[-1]
    idx = jnp.arange(CHUNK, dtype=jnp.float32)
    rel = idx[:, None] - idx[None, :]
    lg = log_gamma[:, None]
    decay = jnp.where(rel >= 0, jnp.exp(jnp.maximum(rel, 0.0) * log_gamma[:, None, None]), 0.0)
    cross = jnp.exp((idx + 1.0) * lg)[:, :, None]
    k_dec = jnp.exp((CHUNK - 1.0 - idx) * lg)[:, :, None]
    chunk_dec = jnp.exp(CHUNK * log_gamma)[:, None, None]

    def step(R, inp):
        qb, kb, vb = inp
        inner = jnp.einsum('bhtd,bhsd->bhts', qb, kb) * decay
        o = jnp.einsum('bhts,bhsv->bhtv', inner, vb) + cross * jnp.einsum('bhtd,bhdv->bhtv', qb, R)
        R = chunk_dec * R + jnp.einsum('bhsd,bhsv->bhdv', kb * k_dec, vb)
        return R, o

    R0 = jnp.zeros((b, h, dk, dv), jnp.float32)
    _, o = lax.scan(step, R0, (qc, kc, vc))
    return _from_chunks(o).astype(dt)


def _rotary(t, positions):
    d = t.shape[-1]
    inv = 1.0 / (ROPE_BASE ** jnp.linspace(0.0, 1.0, d // 2, dtype=jnp.float32))
    ang = positions.astype(jnp.float32)[..., None] * inv
    sin, cos = jnp.sin(ang)[:, :, None, :], jnp.cos(ang)[:, :, None, :]
    tf = t.astype(jnp.float32)
    t1, t2 = tf[..., 0::2], tf[..., 1::2]
    out = jnp.stack([t1 * cos - t2 * sin, t1 * sin + t2 * cos], axis=-1).reshape(t.shape)
    return out.astype(t.dtype)


def _token_mixer(x, positions, w_in, lb, hg_norm_g, ret_norm_g, w_branch_hg, w_branch_ret, w_out):
    b, l, _ = x.shape
    proj = x @ w_in
    hq, hf_fwd, hf_bwd, hi, hgate, rq, rk, rv, rgate, ga, gb = jnp.split(proj, np.cumsum(PROJ_WIDTHS)[:-1].tolist(), axis=-1)
    heads = lambda t, n: t.reshape(b, l, n, -1)
    flip = lambda t: jnp.flip(t, axis=1)

    q_h = heads(jax.nn.silu(hq), HG_HEADS)
    v_h = heads(hi, HG_HEADS)

    def hgrn_direction(f_logits, qd, vd):
        f = lb + (1.0 - lb) * jax.nn.sigmoid(f_logits.astype(jnp.float32))
        return _hgrn2_scan(qd, heads(1.0 - f, HG_HEADS), vd, heads(jnp.log(f), HG_HEADS))

    o_hg = hgrn_direction(hf_fwd, q_h, v_h) + flip(hgrn_direction(flip(hf_bwd), flip(q_h), flip(v_h)))
    o_hg = _rms_norm_heads(o_hg, hg_norm_g) * jax.nn.silu(hgate.astype(jnp.float32))
    y_hg = o_hg.astype(x.dtype) @ w_branch_hg

    rq_h = _rotary(heads(rq, RET_HEADS), positions)
    rk_h = _rotary(heads(rk, RET_HEADS), positions) * RET_DK ** -0.5
    rv_h = heads(rv, RET_HEADS)
    log_gamma = jnp.log(1.0 - 2.0 ** (-5.0 - jnp.arange(RET_HEADS, dtype=jnp.float32)))
    o_ret = _retention_scan(rq_h, rk_h, rv_h, log_gamma) + flip(_retention_scan(flip(rq_h), flip(rk_h), flip(rv_h), log_gamma))
    o_ret = _rms_norm_heads(o_ret, ret_norm_g) * jax.nn.silu(rgate.astype(jnp.float32))
    y_ret = o_ret.astype(x.dtype) @ w_branch_ret

    merged = jax.nn.sigmoid(ga) * y_hg + jax.nn.sigmoid(gb) * y_ret
    return merged @ w_out


def _hier_moe(x, w_group, b_group, w_router, b_router, w1, w3, w2):
    t, d = x.shape
    g_logits = (x @ w_group).astype(jnp.float32) + b_group
    grp = jnp.argmax(g_logits, -1)
    p_grp = jnp.take_along_axis(jax.nn.softmax(g_logits, -1), grp[:, None], -1)
    e_logits = ((x @ w_router).astype(jnp.float32) + b_router).reshape(t, N_GROUPS, EXPERTS_PER_GROUP)
    e_logits = jnp.take_along_axis(e_logits, grp[:, None, None], 1)[:, 0]
    top_p, top_i = lax.top_k(jax.nn.softmax(e_logits, -1), TOP_K)
    top_p = top_p / jnp.sum(top_p, -1, keepdims=True)
    wts = (p_grp * top_p).reshape(-1)
    eid = (grp[:, None] * EXPERTS_PER_GROUP + top_i).reshape(-1)
    n_assign = t * TOP_K
    tok = jnp.arange(n_assign, dtype=jnp.int32) // TOP_K

    order = jnp.argsort(eid)
    e_sorted = eid[order]
    counts = jnp.zeros((N_EXPERTS,), jnp.int32).at[eid].add(1)
    start = jnp.cumsum(counts) - counts
    padded = (counts + MOE_BLOCK - 1) // MOE_BLOCK * MOE_BLOCK
    pend = jnp.cumsum(padded)
    pstart = pend - padded
    dest = pstart[e_sorted] + (jnp.arange(n_assign, dtype=jnp.int32) - start[e_sorted])
    n_blocks = -(-n_assign // MOE_BLOCK) + N_EXPERTS
    rows = jnp.full((n_blocks * MOE_BLOCK,), t, jnp.int32).at[dest].set(tok[order])
    row_w = jnp.zeros((n_blocks * MOE_BLOCK,), jnp.float32).at[dest].set(wts[order])
    block_expert = jnp.clip(jnp.searchsorted(pend, jnp.arange(n_blocks, dtype=jnp.int32) * MOE_BLOCK, side='right'), 0, N_EXPERTS - 1)
    x_pad = jnp.concatenate([x, jnp.zeros((1, d), x.dtype)], 0)
    xb = x_pad[rows].reshape(n_blocks, MOE_BLOCK, d)

    def expert_block(args):
        xs, e = args
        return (jax.nn.silu(xs @ w1[e]) * (xs @ w3[e])) @ w2[e]

    yb = lax.map(expert_block, (xb, block_expert))
    y = yb.reshape(-1, d) * row_w[:, None].astype(x.dtype)
    return jax.ops.segment_sum(y, rows, num_segments=t + 1)[:t]


def setup_inputs(seed: int = 0) -> dict:
    key = jax.random.key(seed)
    ks = jax.random.split(key, 20)
    nrm = lambda k, shape, s: jax.random.normal(k, shape, jnp.float32) * s
    beta = DEEPNORM_BETA
    x = nrm(ks[0], (BATCH, SEQ, D_MODEL), 1.0)
    positions = jax.random.randint(ks[1], (BATCH, 1), 0, MAX_POS_OFFSET, jnp.int32) + jnp.arange(SEQ, dtype=jnp.int32)[None, :]
    col_scales = (1.0, 1.0, 1.0, beta, 1.0, 1.0, 1.0, beta, 1.0, 1.0, 1.0)
    col_scale = jnp.concatenate([jnp.full((w,), s, jnp.float32) for w, s in zip(PROJ_WIDTHS, col_scales)])
    w_in = nrm(ks[2], (DEPTH, D_MODEL, PROJ_DIM), D_MODEL ** -0.5) * col_scale
    hg_lb_logits = nrm(ks[3], (DEPTH + 1, HG_DIM_K), 0.5)
    hg_norm_g = 1.0 + nrm(ks[4], (DEPTH, HG_DIM_V), 0.02)
    ret_norm_g = 1.0 + nrm(ks[5], (DEPTH, RET_DIM_V), 0.02)
    w_branch_hg = nrm(ks[6], (DEPTH, HG_DIM_V, D_MODEL), HG_DIM_V ** -0.5 * beta)
    w_branch_ret = nrm(ks[7], (DEPTH, RET_DIM_V, D_MODEL), RET_DIM_V ** -0.5 * beta)
    w_out = nrm(ks[8], (DEPTH, D_MODEL, D_MODEL), D_MODEL ** -0.5 * beta)
    ln1_g = 1.0 + nrm(ks[9], (DEPTH, D_MODEL), 0.02)
    ln1_b = nrm(ks[10], (DEPTH, D_MODEL), 0.02)
    w_group = nrm(ks[11], (DEPTH, D_MODEL, N_GROUPS), D_MODEL ** -0.5)
    b_group = nrm(ks[12], (DEPTH, N_GROUPS), 0.01)
    w_router = nrm(ks[13], (DEPTH, D_MODEL, N_EXPERTS), D_MODEL ** -0.5)
    b_router = nrm(ks[14], (DEPTH, N_EXPERTS), 0.01)
    w1 = nrm(ks[15], (DEPTH, N_EXPERTS, D_MODEL, D_EXPERT), D_MODEL ** -0.5)
    w3 = nrm(ks[16], (DEPTH, N_EXPERTS, D_MODEL, D_EXPERT), D_MODEL ** -0.5)
    w2 = nrm(ks[17], (DEPTH, N_EXPERTS, D_EXPERT, D_MODEL), D_EXPERT ** -0.5 * beta)
    ln2_g = 1.0 + nrm(ks[18], (DEPTH, D_MODEL), 0.02)
    ln2_b = nrm(ks[19], (DEPTH, D_MODEL), 0.02)
    return {'x': x, 'positions': positions, 'w_in': w_in, 'hg_lb_logits': hg_lb_logits,
            'hg_norm_g': hg_norm_g, 'ret_norm_g': ret_norm_g, 'w_branch_hg': w_branch_hg,
            'w_branch_ret': w_branch_ret, 'w_out': w_out, 'ln1_g': ln1_g, 'ln1_b': ln1_b,
            'w_group': w_group, 'b_group': b_group, 'w_router': w_router, 'b_router': b_router,
            'w1': w1, 'w3': w3, 'w2': w2, 'ln2_g': ln2_g, 'ln2_b': ln2_b}


def reference(x, positions, w_in, hg_lb_logits, hg_norm_g, ret_norm_g, w_branch_hg, w_branch_ret,
              w_out, ln1_g, ln1_b, w_group, b_group, w_router, b_router, w1, w3, w2, ln2_g, ln2_b):
    lb_cum = jnp.cumsum(jax.nn.softmax(hg_lb_logits.astype(jnp.float32), axis=0), axis=0)
    for layer in range(DEPTH):
        lb = lb_cum[layer + 1] - lb_cum[0]
        mix = _token_mixer(x, positions, w_in[layer], lb, hg_norm_g[layer], ret_norm_g[layer],
                           w_branch_hg[layer], w_branch_ret[layer], w_out[layer])
        x = _layer_norm(DEEPNORM_ALPHA * x + mix, ln1_g[layer], ln1_b[layer])
        moe = _hier_moe(x.reshape(-1, D_MODEL), w_group[layer], b_group[layer], w_router[layer],
                        b_router[layer], w1[layer], w3[layer], w2[layer]).reshape(x.shape)
        x = _layer_norm(DEEPNORM_ALPHA * x + moe, ln2_g[layer], ln2_b[layer])
    return x
```

```python
from contextlib import ExitStack
import math
import numpy as np
import concourse.bass as bass
import concourse.mybir as mybir
from concourse.bass_utils import run_bass_kernel_spmd

F32 = mybir.dt.float32
BF16 = mybir.dt.bfloat16
I32 = mybir.dt.int32
ALU = mybir.AluOpType
AF = mybir.ActivationFunctionType

D = 1024
NKC = 8
TT = 512
HG_H = 8
RET_H = 4
NEXP = 32
DEXP = 512
ALPHA = 2.0 ** 0.25
LN_EPS = 1e-5
RMS_EPS = 1e-6
OFF_RET = HG_H * 640
OFF_G = OFF_RET + RET_H * 1024
TWO_PI = 2.0 * math.pi
CW1 = 6.28125
CW2 = TWO_PI - CW1


class Res:
    __slots__ = ("w", "r")

    def __init__(self):
        self.w = None
        self.r = {}


class Tile:
    def __init__(self, t, nres=1):
        self.t = t
        self.res = [Res() for _ in range(nres)]

    @property
    def r(self):
        return self.res[0]

    def __getitem__(self, k):
        return self.t[k]


class Sched:
    ENG = ("pe", "act", "dve", "pool", "sp")
    NDS = 8

    def __init__(self, nc):
        self.nc = nc
        self.ops = {e: [] for e in self.ENG}
        self.cnt = {e: 0 for e in self.ENG}
        self.waited = {e: {} for e in self.ENG}
        self.dcount = {e: 0 for e in self.ENG}
        self.stack = ExitStack()

    def sb(self, name, shape, dtype, nres=1, stack=None):
        t = (stack or self.stack).enter_context(self.nc.sbuf_tensor("sb_" + name, list(shape), dtype))
        return Tile(t, nres)

    def ps(self, name, shape, dtype, nres=1):
        t = self.stack.enter_context(self.nc.psum_tensor("ps_" + name, list(shape), dtype))
        return Tile(t, nres)

    def _collect(self, eng, reads, writes, extra=()):
        deps = {}

        def add(tok):
            if tok is None:
                return
            k, v = tok
            if deps.get(k, 0) < v:
                deps[k] = v

        for r in reads:
            add(r.w)
        for w in writes:
            add(w.w)
            for k, v in w.r.items():
                add((k, v))
        for t in extra:
            add(t)
        waits = []
        for k, v in deps.items():
            if k == eng and eng == "pe":
                continue
            if self.waited[eng].get(k, 0) >= v:
                continue
            self.waited[eng][k] = v
            waits.append((k, v))
        return waits

    def _record(self, tok, reads, writes):
        k, v = tok
        for r in reads:
            if r.r.get(k, 0) < v:
                r.r[k] = v
        for w in writes:
            w.w = tok
            w.r = {}

    def op(self, eng, fn, reads=(), writes=(), inc=True):
        assert inc or eng == "pe"
        waits = self._collect(eng, reads, writes)
        if inc:
            self.cnt[eng] += 1
            tok = (eng, self.cnt[eng])
            incinfo = (eng, 1)
        else:
            tok = (eng, self.cnt[eng] + 1)
            incinfo = None
        self.ops[eng].append((waits, fn, incinfo))
        self._record(tok, reads, writes)
        return tok

    def dma(self, q, fn, reads=(), writes=()):
        n = self.dcount[q]
        self.dcount[q] += 1
        slot, rnd = n % self.NDS, n // self.NDS
        key = ("dma", q, slot)
        extra = [(key, 16 * rnd)] if rnd > 0 else []
        waits = self._collect(q, reads, writes, extra)
        tok = (key, 16 * (rnd + 1))
        self.ops[q].append((waits, fn, (key, 16)))
        self._record(tok, reads, writes)
        return tok

    def wait_all(self, eng, toks):
        waits = self._collect(eng, (), (), toks)
        self.cnt[eng] += 1
        self.ops[eng].append((waits, None, (eng, 1)))

    def emit(self):
        nc = self.nc
        keys = set()
        for e in self.ENG:
            for waits, fn, incinfo in self.ops[e]:
                for k, v in waits:
                    keys.add(k)
                if incinfo:
                    keys.add(incinfo[0])
        sems = {}
        for k in sorted(keys, key=str):
            nm = k if isinstance(k, str) else f"d_{k[1]}_{k[2]}"
            sems[k] = self.stack.enter_context(nc.semaphore("s_" + nm))

        def run(name, e):
            for waits, fn, incinfo in self.ops[name]:
                for k, v in waits:
                    e.wait_ge(sems[k], v)
                ins = e.nop() if fn is None else fn(e)
                if incinfo:
                    ins.then_inc(sems[incinfo[0]], incinfo[1])

        with nc.Block() as block:
            @block.tensor
            def _(e):
                run("pe", e)

            @block.scalar
            def _(e):
                run("act", e)

            @block.vector
            def _(e):
                run("dve", e)

            @block.gpsimd
            def _(e):
                run("pool", e)

            @block.sync
            def _(e):
                run("sp", e)


def bc(ap, n):
    return ap.unsqueeze(ap.ndim).to_broadcast(list(ap.shape) + [n])


def build(T, debug=False):
    assert T % TT == 0
    NT = T // TT
    NB = T // 128
    nc = bass.Bass("TRN2", target_bir_lowering=False)
    dt_in = lambda name, shape, dt=F32: nc.dram_tensor(name, list(shape), dt, kind="ExternalInput").ap()
    x_d = dt_in("x", [2 * T, D])
    pos_d = dt_in("pos", [1, 2 * T], I32)
    win_d = dt_in("w_in", [D, 11264])
    lbl_d = dt_in("lbl", [2, D])
    hgg_d = dt_in("hgg", [1, D])
    retg_d = dt_in("retg", [1, D])
    wbh_d = dt_in("wbh", [D, D])
    wbr_d = dt_in("wbr", [D, D])
    wo_d = dt_in("wo", [D, D])
    l1g_d = dt_in("l1g", [1, D])
    l1b_d = dt_in("l1b", [1, D])
    l2g_d = dt_in("l2g", [1, D])
    l2b_d = dt_in("l2b", [1, D])
    wgr_d = dt_in("wgr", [D, 36])
    bgr_d = dt_in("bgr", [1, 36])
    w1_d = dt_in("w1", [NEXP, D, DEXP])
    w3_d = dt_in("w3", [NEXP, D, DEXP])
    w2_d = dt_in("w2", [NEXP, DEXP, D])
    invf_d = dt_in("invf", [128, 1])
    rexpo_d = dt_in("rexpo", [128, 10 * 128])
    out_d = nc.dram_tensor("out", [T, D], F32, kind="ExternalOutput").ap()
    dk = "ExternalOutput" if debug else "Internal"
    xTd = nc.dram_tensor("xTd", [2 * NT, 128, NKC * TT], BF16, kind="Internal").ap()
    csd = nc.dram_tensor("csd", [2 * NT, 128, 2 * TT], F32, kind="Internal").ap()
    oS = nc.dram_tensor("oS", [2 * D, T], BF16, kind=dk).ap()
    x1f = nc.dram_tensor("x1f", [T, D], F32, kind=dk).ap()
    x1Td = nc.dram_tensor("x1Td", [NT, 128, NKC * TT], BF16, kind="Internal").ap()
    NBLK = (2 * T) // 128 + NEXP
    x1bd = nc.dram_tensor("x1bd", [T, D], BF16, kind="Internal").ap()
    xs_d = nc.dram_tensor("xs_d", [NBLK * 128, D], BF16, kind="Internal").ap()
    ys_d = nc.dram_tensor("ys_d", [NBLK * 128, D], F32, kind="Internal").ap()
    WS = nc.dram_tensor("WS", [NEXP * 128, 12288], BF16, kind="Internal").ap()

    S = Sched(nc)
    op, dma = S.op, S.dma

    pmm = [S.ps(f"pmm{i}", [128, 512], F32) for i in range(3)]
    ptr = [S.ps(f"ptr{i}", [128, NKC, 128], BF16) for i in range(2)]
    pat = S.ps("pat", [128, 512], F32, nres=4)
    po = S.ps("po", [128, 512], F32, nres=2)
    psS = S.ps("psS", [128, 512], F32, nres=4)
    pat.res = [pat.res[0]] * 4
    po.res = [po.res[0]] * 2
    psS.res = [psS.res[0]] * 4
    cnt = {"mm": 0, "tr": 0}

    mmbanks = {"l": pmm}

    def next_pmm():
        cnt["mm"] += 1
        l = mmbanks["l"]
        return l[cnt["mm"] % len(l)]

    def next_ptr():
        cnt["tr"] += 1
        return ptr[cnt["tr"] % 2]

    ident = S.sb("ident", [128, 128], BF16)
    maskF = S.sb("maskF", [128, 128], F32)
    maskB = S.sb("maskB", [128, 128], F32)
    ones = S.sb("ones", [128, 512], F32)
    onesb = S.sb("onesb", [128, 128], BF16)
    lbT = S.sb("lbT", [128, 4, HG_H], F32)
    lraw = S.sb("lraw", [128, 2, HG_H], F32)
    hggT = S.sb("hggT", [128, HG_H], F32)
    retgT = S.sb("retgT", [128, 8], F32)
    invf = S.sb("invf", [128, 1], F32)
    epsr = S.sb("epsr", [128, 2], F32)

    op("pool", lambda e: e.memset(ident[:], 1.0), writes=[ident.r])
    op("pool", lambda e: e.affine_select(out=ident[:], in_=ident[:], pattern=[[-1, 128]], compare_op=ALU.is_equal,
                                         fill=0.0, base=0, channel_multiplier=1), reads=[ident.r], writes=[ident.r])
    op("pool", lambda e: e.memset(maskF[:], 1.0), writes=[maskF.r])
    op("pool", lambda e: e.affine_select(out=maskF[:], in_=maskF[:], pattern=[[1, 128]], compare_op=ALU.is_ge,
                                         fill=0.0, base=0, channel_multiplier=-1), reads=[maskF.r], writes=[maskF.r])
    op("pool", lambda e: e.memset(maskB[:], 1.0), writes=[maskB.r])
    op("pool", lambda e: e.affine_select(out=maskB[:], in_=maskB[:], pattern=[[-1, 128]], compare_op=ALU.is_ge,
                                         fill=0.0, base=0, channel_multiplier=1), reads=[maskB.r], writes=[maskB.r])
    op("dve", lambda e: e.memset(ones[:], 1.0), writes=[ones.r])
    op("dve", lambda e: e.memset(onesb[:], 1.0), writes=[onesb.r])
    op("dve", lambda e: e.memset(epsr[:, 0:1], RMS_EPS), writes=[epsr.r])
    op("dve", lambda e: e.memset(epsr[:, 1:2], LN_EPS), writes=[epsr.r])
    dma("sp", lambda e: e.dma_start(out=lraw[:], in_=lbl_d.rearrange("r (h p) -> p r h", p=128), allow_slow_non_contiguous=True), writes=[lraw.r])
    dma("sp", lambda e: e.dma_start(out=hggT[:], in_=hgg_d.rearrange("r (h p) -> p (r h)", p=128), allow_slow_non_contiguous=True), writes=[hggT.r])
    dma("sp", lambda e: e.dma_start(out=retgT[:], in_=retg_d.rearrange("r (h p) -> p (r h)", p=128), allow_slow_non_contiguous=True), writes=[retgT.r])
    dma("sp", lambda e: e.dma_start(out=invf[:], in_=invf_d), writes=[invf.r])
    op("dve", lambda e: e.tensor_tensor(out=lbT[:, 3, :], in0=lraw[:, 1, :], in1=lraw[:, 0, :], op=ALU.subtract),
       reads=[lraw.r], writes=[lbT.r])
    op("act", lambda e: e.activation(out=lbT[:, 0, :], in_=lbT[:, 3, :], func=AF.Sigmoid), reads=[lbT.r], writes=[lbT.r])
    op("dve", lambda e: e.tensor_scalar(out=lbT[:, 1, :], in0=lbT[:, 0, :], scalar1=-1.0, scalar2=1.0, op0=ALU.mult, op1=ALU.add),
       reads=[lbT.r], writes=[lbT.r])
    op("dve", lambda e: e.tensor_scalar(out=lbT[:, 2, :], in0=lbT[:, 1, :], scalar1=-1.0, scalar2=None, op0=ALU.mult),
       reads=[lbT.r], writes=[lbT.r])

    with ExitStack() as st:
        xb = [S.sb(f"xb{i}", [128, 4, D], BF16, stack=st) for i in range(2)]
        xf = [S.sb(f"xf{i}", [128, 4, D], F32, stack=st) for i in range(2)]

        def xload(jj):
            xff = xf[jj % 2]
            dma("sp", lambda e: e.dma_start(out=xff[:], in_=x_d[jj * TT:(jj + 1) * TT, :].rearrange("(b p) d -> p b d", p=128)), writes=[xff.r])

        xload(0)
        xTt = [S.sb(f"xTt{i}", [128, NKC, TT], BF16, stack=st) for i in range(2)]
        posi = [S.sb(f"posi{i}", [128, TT], I32, stack=st) for i in range(2)]
        ang = [S.sb(f"ang{i}", [128, TT], F32, stack=st) for i in range(2)]
        kf = [S.sb(f"kf{i}", [128, TT], F32, stack=st) for i in range(2)]
        ki = [S.sb(f"ki{i}", [128, TT], I32, stack=st) for i in range(2)]
        rr = [S.sb(f"rr{i}", [128, TT], F32, stack=st) for i in range(2)]
        yy = [S.sb(f"yy{i}", [128, TT], F32, stack=st) for i in range(2)]
        mm_ = [S.sb(f"mm{i}", [128, TT], F32, stack=st) for i in range(2)]
        cs = [S.sb(f"cs{i}", [128, 2, TT], F32, stack=st) for i in range(2)]
        nblk = 0
        for j in range(2 * NT):
            xt = xTt[j % 2]
            xbb = xb[j % 2]
            xff = xf[j % 2]
            if j + 1 < 2 * NT:
                xload(j + 1)
            for b in range(4):
                if b < 3:
                    op("act", lambda e, xff=xff, xbb=xbb, b=b: e.copy(out=xbb[:, b, :], in_=xff[:, b, :]), reads=[xff.r], writes=[xbb.r])
                else:
                    op("dve", lambda e, xff=xff, xbb=xbb, b=b: e.tensor_copy(out=xbb[:, b, :], in_=xff[:, b, :]), reads=[xff.r], writes=[xbb.r])
            for b in range(4):
                pt = next_ptr()
                for kc in range(NKC):
                    op("pe", lambda e, pt=pt, xbb=xbb, kc=kc, b=b: e.transpose(pt[:, kc, :], xbb[:, b, kc * 128:(kc + 1) * 128], ident[:]),
                       reads=[xbb.r, ident.r], writes=[pt.r], inc=(kc == NKC - 1))
                eng = "act" if b % 2 == 0 else "dve"
                if eng == "act":
                    op("act", lambda e, pt=pt, xt=xt, b=b: e.copy(out=xt[:, :, b * 128:(b + 1) * 128], in_=pt[:]),
                       reads=[pt.r], writes=[xt.r])
                else:
                    op("dve", lambda e, pt=pt, xt=xt, b=b: e.tensor_copy(out=xt[:, :, b * 128:(b + 1) * 128], in_=pt[:]),
                       reads=[pt.r], writes=[xt.r])
            dma("sp", lambda e, xt=xt, j=j: e.dma_start(out=xTd[j].rearrange("p (k t) -> p k t", t=TT), in_=xt[:]), reads=[xt.r])
            s = j % 2
            pi_, an, kf_, ki_, r_, y_, m_, c_ = posi[s], ang[s], kf[s], ki[s], rr[s], yy[s], mm_[s], cs[s]
            dma("sp", lambda e, pi_=pi_, j=j: e.dma_start(out=pi_[:], in_=pos_d[0:1, j * TT:(j + 1) * TT].partition_broadcast(128)),
                writes=[pi_.r])
            op("dve", lambda e, pi_=pi_, an=an: e.tensor_copy(out=an[:], in_=pi_[:]), reads=[pi_.r], writes=[an.r])
            op("dve", lambda e, an=an: e.tensor_scalar(out=an[:], in0=an[:], scalar1=invf[:, 0:1], scalar2=None, op0=ALU.mult),
               reads=[an.r, invf.r], writes=[an.r])
            op("dve", lambda e, an=an, ki_=ki_: e.tensor_scalar(out=ki_[:], in0=an[:], scalar1=1.0 / TWO_PI, scalar2=None, op0=ALU.mult),
               reads=[an.r], writes=[ki_.r])
            op("dve", lambda e, kf_=kf_, ki_=ki_: e.tensor_copy(out=kf_[:], in_=ki_[:]), reads=[ki_.r], writes=[kf_.r])
            op("dve", lambda e, r_=r_, kf_=kf_, an=an: e.scalar_tensor_tensor(out=r_[:], in0=kf_[:], scalar=-CW1, in1=an[:], op0=ALU.mult, op1=ALU.add),
               reads=[kf_.r, an.r], writes=[r_.r])
            op("dve", lambda e, r_=r_, kf_=kf_: e.scalar_tensor_tensor(out=r_[:], in0=kf_[:], scalar=-CW2, in1=r_[:], op0=ALU.mult, op1=ALU.add),
               reads=[kf_.r, r_.r], writes=[r_.r])
            for which, shift in ((1, 0.0), (0, math.pi / 2)):
                op("pool", lambda e, y_=y_, r_=r_, shift=shift: e.tensor_scalar(out=y_[:], in0=r_[:], scalar1=shift, scalar2=None, op0=ALU.add),
                   reads=[r_.r], writes=[y_.r])
                op("dve", lambda e, y_=y_, m_=m_: e.tensor_scalar(out=m_[:], in0=y_[:], scalar1=math.pi, scalar2=-TWO_PI, op0=ALU.is_gt, op1=ALU.mult),
                   reads=[y_.r], writes=[m_.r])
                op("dve", lambda e, y_=y_, m_=m_: e.tensor_tensor(out=y_[:], in0=y_[:], in1=m_[:], op=ALU.add), reads=[y_.r, m_.r], writes=[y_.r])
                op("dve", lambda e, y_=y_, m_=m_: e.tensor_scalar(out=m_[:], in0=y_[:], scalar1=-math.pi, scalar2=TWO_PI, op0=ALU.is_lt, op1=ALU.mult),
                   reads=[y_.r], writes=[m_.r])
                op("dve", lambda e, y_=y_, m_=m_: e.tensor_tensor(out=y_[:], in0=y_[:], in1=m_[:], op=ALU.add), reads=[y_.r, m_.r], writes=[y_.r])
                op("dve", lambda e, y_=y_: e.tensor_scalar(out=y_[:], in0=y_[:], scalar1=-3.1415925, scalar2=3.1415925, op0=ALU.max, op1=ALU.min),
                   reads=[y_.r], writes=[y_.r])
                op("act", lambda e, y_=y_, c_=c_, which=which: e.activation(out=c_[:, which, :], in_=y_[:], func=AF.Sin), reads=[y_.r], writes=[c_.r])
            dma("sp", lambda e, c_=c_, j=j: e.dma_start(out=csd[j].rearrange("p (k t) -> p k t", t=TT), in_=c_[:]), reads=[c_.r])
    def fence(queues=("sp",)):
        toks = []
        for q in queues:
            n = S.dcount[q]
            for sl in range(S.NDS):
                if n > sl:
                    toks.append((("dma", q, sl), 16 * ((n - 1 - ((n - 1 - sl) % S.NDS)) // S.NDS + 1)))
        waits = S._collect("sp", (), (), toks)
        S.cnt["sp"] += 1
        S.ops["sp"].append((waits, None, ("sp", 1)))
        r = Res()
        r.w = ("sp", S.cnt["sp"])
        return r

    def barrier(extra_res):
        toks = [(eng, S.cnt[eng]) for eng in ("pe", "act", "dve", "pool")] + [extra_res.w]
        for eng in S.ENG:
            waits = S._collect(eng, (), (), toks)
            if waits:
                S.cnt[eng] += 1
                S.ops[eng].append((waits, None, (eng, 1)))

    xsync = fence(("sp", "pool"))
    barrier(xsync)

    ph_stack = ExitStack()
    st = ph_stack
    rexpo = S.sb("rexpo", [128, 10, 128], F32, stack=st)
    rtab = S.sb("rtab", [128, 10, 128], F32, stack=st)
    Sinit_h = S.sb("Sinit_h", [128, HG_H, 128], F32, stack=st)
    Sinit_r = S.sb("Sinit_r", [128, RET_H, 2, 256], F32, stack=st)
    dma("sp", lambda e: e.dma_start(out=rexpo[:], in_=rexpo_d.rearrange("p (k i) -> p k i", i=128)), writes=[rexpo.r])
    Wh = [S.sb(f"Wh{i}", [128, NKC, 1024], BF16, stack=st) for i in range(2)]
    xTt = [S.sb(f"xTl{i}", [128, NKC, TT], BF16, stack=st) for i in range(2)]
    cst = [S.sb(f"cst{i}", [128, 2, TT], F32, stack=st) for i in range(1)]
    qS = S.sb("qS", [128, 2, T], BF16, stack=st, nres=NT)
    vS = S.sb("vS", [128, NB, 256], BF16, stack=st, nres=NT)
    oF = S.sb("oF", [128, 2, T], BF16, stack=st, nres=NT)
    Sst = S.sb("Sst", [128, 2, 256], F32, stack=st)
    Sbfs = [S.sb(f"Sbf{i}", [128, 2, 256], BF16, stack=st) for i in range(2)]
    scur = {"i": 0}
    NSET = 2
    tA = [S.sb(f"tA{i}", [128, TT], F32, stack=st) for i in range(NSET)]
    tB = [S.sb(f"tB{i}", [128, TT], F32, stack=st) for i in range(NSET)]
    tC = [S.sb(f"tC{i}", [128, TT], F32, stack=st) for i in range(NSET)]
    tG = [S.sb(f"tG{i}", [128, TT], F32, stack=st) for i in range(NSET)]
    tE = [S.sb(f"tE{i}", [128, TT], F32, stack=st) for i in range(NSET)]
    tF = [S.sb(f"tF{i}", [128, TT], F32, stack=st) for i in range(NSET)]
    tBc = [S.sb(f"tBc{i}", [128, 8], F32, stack=st) for i in range(NSET)]
    tcd = [S.sb(f"tcd{i}", [128, 8], F32, stack=st) for i in range(NSET)]
    tcb = [S.sb(f"tcb{i}", [128, 4], F32, stack=st) for i in range(NSET)]
    QT = [S.sb(f"QT{i}", [128, 2, TT], BF16, stack=st) for i in range(NSET)]
    QM = [S.sb(f"QM{i}", [128, 2, TT], BF16, stack=st) for i in range(NSET)]
    KA = [S.sb(f"KA{i}", [128, 2, TT], BF16, stack=st) for i in range(NSET)]
    KB = [S.sb(f"KB{i}", [128, 2, TT], BF16, stack=st) for i in range(NSET)]
    KM = [S.sb(f"KM{i}", [128, 2, TT], BF16, stack=st) for i in range(NSET)]
    vT = [S.sb(f"vT{i}", [128, 4, 256], BF16, stack=st) for i in range(NSET)]
    krot = [S.sb(f"krot{i}", [128, 2, TT], BF16, stack=st) for i in range(NSET)]
    ATm = [S.sb(f"ATm{i}", [128, 128], BF16, stack=st) for i in range(4)]
    kTs = [S.sb(f"kTs{i}", [128, 2, 128], BF16, stack=st) for i in range(4)]
    osum = [S.sb(f"osum{i}", [128, 2, TT], F32, stack=st) for i in range(2)]
    sq = [S.sb(f"sq{i}", [128, 2, TT], BF16, stack=st) for i in range(1)] * 2
    sg = [S.sb(f"sg{i}", [128, 2, TT], BF16, stack=st) for i in range(2)]
    rstd = [S.sb(f"rstd{i}", [128, TT], F32, stack=st) for i in range(1)] * 2
    oOut = [S.sb(f"oOut{i}", [128, 2, TT], BF16, stack=st) for i in range(2)]
    visit = {"n": 0, "blk": 0, "wh": 0, "xl": 0}

    wsched = []
    for _ph in (0, 1):
        wsched += [(h * 640, 640) for h in range(HG_H)] + [(OFF_RET + hr * 1024, 1024) for hr in range(RET_H)]
    wstate = {"issued": 0, "cur": -1}

    def _issue_w():
        i = wstate["issued"]
        if i >= len(wsched):
            return
        c0, ncols = wsched[i]
        W = Wh[i % 2]
        dma("pool", lambda e: e.dma_start(out=W[:, :, 0:ncols], in_=win_d[:, c0:c0 + ncols].rearrange("(k p) c -> p k c", p=128)),
            writes=[W.r])
        wstate["issued"] += 1

    def load_w(c0, ncols):
        wstate["cur"] += 1
        i = wstate["cur"]
        assert wsched[i] == (c0, ncols)
        while wstate["issued"] <= i:
            _issue_w()
        return Wh[i % 2]

    def prefetch_w():
        if wstate["issued"] <= wstate["cur"] + 1:
            _issue_w()

    pc_jobs = []
    for ex in range(NEXP):
        pc_jobs.append((w1_d[ex].rearrange("(k p) f -> p k f", p=128), ex, 0, 4096, DEXP))
        pc_jobs.append((w3_d[ex].rearrange("(k p) f -> p k f", p=128), ex, 4096, 4096, DEXP))
        pc_jobs.append((w2_d[ex].rearrange("(k p) f -> p k f", p=128), ex, 8192, 4096, D))
    pc_state = {"i": 0}

    def precast_step(n=1):
        for _ in range(n):
            i = pc_state["i"]
            if i >= len(pc_jobs):
                return
            src, ex, c0, w, inner = pc_jobs[i]
            pc_state["i"] += 1
            dma("pool", lambda e, src=src, ex=ex, c0=c0, w=w, inner=inner: e.dma_start(
                out=WS[ex * 128:(ex + 1) * 128, c0:c0 + w].rearrange("p (k f) -> p k f", f=inner), in_=src))

    xpend = {}

    def prefetch_xT(gt):
        if gt is None or gt in xpend:
            return
        visit["xl"] += 1
        X = xTt[visit["xl"] % 2]
        dma("sp", lambda e: e.dma_start(out=X[:], in_=xTd[gt].rearrange("p (k t) -> p k t", t=TT)), reads=[xsync], writes=[X.r])
        xpend[gt] = X

    def load_xT(gt, nxt=None):
        prefetch_xT(gt)
        X = xpend.pop(gt)
        prefetch_xT(nxt)
        precast_step(1)
        return X

    def load_cs(gt):
        C = cst[0]
        dma("sp", lambda e: e.dma_start(out=C[:], in_=csd[gt].rearrange("p (k t) -> p k t", t=TT)), reads=[xsync], writes=[C.r])
        return C

    def inproj_fm(W, c0, X):
        pm = next_pmm()
        for kc in range(NKC):
            op("pe", lambda e, kc=kc: e.matmul(pm[:, :], lhsT=W[:, kc, c0:c0 + 128], rhs=X[:, kc, :], start=(kc == 0), stop=(kc == NKC - 1)),
               reads=[W.r, X.r], writes=[pm.r], inc=(kc == NKC - 1))
        return pm

    def inproj_tm(W, c0, dv, X, dst, dst_res, dst_b0):
        nb_per = 512 // dv
        for g in range(4 // nb_per):
            pm = next_pmm()
            for bb in range(nb_per):
                b = g * nb_per + bb
                for kc in range(NKC):
                    op("pe", lambda e, kc=kc, b=b, bb=bb, pm=pm: e.matmul(pm[:, bb * dv:(bb + 1) * dv], lhsT=X[:, kc, b * 128:(b + 1) * 128],
                                                             rhs=W[:, kc, c0:c0 + dv], start=(kc == 0), stop=(kc == NKC - 1)),
                       reads=[W.r, X.r], writes=[pm.r], inc=(kc == NKC - 1 and bb == nb_per - 1))
            b0 = dst_b0 + g * nb_per
            op("act", lambda e, pm=pm, b0=b0: e.copy(out=dst[:, b0:b0 + nb_per, 0:dv], in_=pm[:, :].rearrange("p (b v) -> p b v", v=dv)),
               reads=[pm.r], writes=[dst_res])

    def v4(ap):
        return ap.rearrange("p (b c i) -> p b c i", c=2, i=64)

    def v8(ap):
        return ap.rearrange("p (c i) -> p c i", i=64)

    def hg_prep(h, pm_f, qsrc, qres, s, dirn, prescan):
        A, B, C, G, E, Fh, Bc, cd, cb = tA[s], tB[s], tC[s], tG[s], tE[s], tF[s], tBc[s], tcd[s], tcb[s]
        first = 0 if dirn == 0 else 1
        second = 1 - first
        lb_h, oml_h, noml_h = lbT[:, 0, h:h + 1], lbT[:, 1, h:h + 1], lbT[:, 2, h:h + 1]
        op("act", lambda e: e.activation(out=B[:], in_=pm_f[:, :], func=AF.Exp, scale=-1.0), reads=[pm_f.r], writes=[B.r])
        yield
        op("act", lambda e: e.activation(out=B[:], in_=B[:], func=AF.Ln, bias=1.0), reads=[B.r], writes=[B.r])
        op("act", lambda e: e.activation(out=A[:], in_=B[:], func=AF.Exp, scale=-1.0), reads=[B.r], writes=[A.r])
        yield
        op("act", lambda e: e.activation(out=B[:], in_=A[:], func=AF.Ln, scale=oml_h, bias=lb_h), reads=[A.r, lbT.r], writes=[B.r])
        op("dve", lambda e: e.tensor_scalar(out=C[:], in0=A[:], scalar1=noml_h, scalar2=oml_h, op0=ALU.mult, op1=ALU.add),
           reads=[A.r, lbT.r], writes=[C.r])
        yield
        op("dve", lambda e: e.tensor_tensor_scan(out=G[:], data0=ones[:], data1=B[:], initial=0.0, op0=ALU.mult, op1=ALU.add),
           reads=[ones.r, B.r], writes=[G.r])
        yield
        if dirn == 0:
            op("pool", lambda e: e.memset(Bc[:, 0:1], 0.0), writes=[Bc.r])
            op("pool", lambda e: e.tensor_copy(out=Bc[:, 1:8], in_=G[:, 63:511:64]), reads=[G.r], writes=[Bc.r])
            op("dve", lambda e: e.tensor_tensor(out=v8(A[:]), in0=v8(G[:]), in1=bc(Bc[:, 0:8], 64), op=ALU.subtract),
               reads=[G.r, Bc.r], writes=[A.r])
        else:
            op("dve", lambda e: e.tensor_tensor(out=A[:], in0=B[:], in1=G[:], op=ALU.subtract), reads=[B.r, G.r], writes=[A.r])
            op("dve", lambda e: e.tensor_tensor(out=v8(A[:]), in0=v8(A[:]), in1=bc(G[:, 63:512:64], 64), op=ALU.add),
               reads=[A.r, G.r], writes=[A.r])
        yield
        op("act", lambda e: e.activation(out=E[:], in_=A[:], func=AF.Exp), reads=[A.r], writes=[E.r])
        op("act", lambda e: e.activation(out=Fh[:], in_=A[:], func=AF.Exp, scale=-1.0), reads=[A.r], writes=[Fh.r])
        yield
        op("dve", lambda e: e.tensor_tensor(out=C[:], in0=C[:], in1=Fh[:], op=ALU.mult), reads=[C.r, Fh.r], writes=[C.r])
        far = 63 if dirn == 0 else 0
        op("pool", lambda e: e.tensor_copy(out=cd[:, 0:8], in_=E[:, far:512:64]), reads=[E.r], writes=[cd.r])
        cd4 = cd[:, 0:8].rearrange("p (b c) -> p b c", c=2)
        op("pool", lambda e: e.tensor_tensor(out=cb[:, 0:4], in0=cd4[:, :, 0], in1=cd4[:, :, 1], op=ALU.mult), reads=[cd.r], writes=[cb.r])
        yield
        op("dve", lambda e: e.tensor_tensor(out=v8(Fh[:]), in0=v8(C[:]), in1=bc(cd[:, 0:8], 64), op=ALU.mult),
           reads=[C.r, cd.r], writes=[Fh.r])
        yield
        km = KM[s]
        F4, C4, km4 = v4(Fh[:]), v4(C[:]), v4(km[:, 0, :])
        op("pool", lambda e: e.tensor_tensor(out=km4[:, :, first, :], in0=F4[:, :, first, :], in1=bc(cd4[:, :, second], 64), op=ALU.mult),
           reads=[Fh.r, cd.r], writes=[km.r])
        op("pool", lambda e: e.tensor_copy(out=km4[:, :, second, :], in_=F4[:, :, second, :]), reads=[Fh.r], writes=[km.r])
        yield
        if prescan:
            return
        qt, qm, ka, kb = QT[s], QM[s], KA[s], KB[s]
        op("dve", lambda e: e.tensor_tensor(out=qt[:, 0, :], in0=qsrc, in1=E[:], op=ALU.mult), reads=[qres, E.r], writes=[qt.r])
        op("pool", lambda e: e.tensor_copy(out=ka[:, 0, :], in_=C[:]), reads=[C.r], writes=[ka.r])
        yield
        kb4, qm4, qt4 = v4(kb[:, 0, :]), v4(qm[:, 0, :]), v4(qt[:, 0, :])
        op("pool", lambda e: e.tensor_copy(out=kb4[:, :, first, :], in_=F4[:, :, first, :]), reads=[Fh.r], writes=[kb.r])
        op("pool", lambda e: e.tensor_copy(out=kb4[:, :, second, :], in_=C4[:, :, second, :]), reads=[C.r], writes=[kb.r])
        yield
        op("pool", lambda e: e.tensor_copy(out=qm4[:, :, first, :], in_=qt4[:, :, first, :]), reads=[qt.r], writes=[qm.r])
        op("pool", lambda e: e.tensor_tensor(out=qm4[:, :, second, :], in0=qt4[:, :, second, :], in1=bc(cd4[:, :, first], 64), op=ALU.mult),
           reads=[qt.r, cd.r], writes=[qm.r])
        yield

    def silu_from_psum(pm, tmp, dst_ap, dst_res):
        op("act", lambda e: e.activation(out=tmp[:], in_=pm[:, :], func=AF.Exp, scale=-1.0), reads=[pm.r], writes=[tmp.r])
        op("act", lambda e: e.activation(out=tmp[:], in_=tmp[:], func=AF.Ln, bias=1.0), reads=[tmp.r], writes=[tmp.r])
        op("act", lambda e: e.activation(out=tmp[:], in_=tmp[:], func=AF.Exp, scale=-1.0), reads=[tmp.r], writes=[tmp.r])
        op("dve", lambda e: e.tensor_tensor(out=dst_ap, in0=pm[:, :], in1=tmp[:], op=ALU.mult), reads=[pm.r, tmp.r], writes=[dst_res])

    def step(g, n):
        if g is None:
            return
        for _ in range(n):
            try:
                next(g)
            except StopIteration:
                return

    def exhaust(g):
        if g is None:
            return
        for _ in g:
            pass

    def pipeline(tiles, make_A, do_B):
        tiles = list(tiles)
        ctxs = [dict() for _ in tiles]
        nx = lambda i: tiles[i + 1] if i + 1 < len(tiles) else None
        exhaust(make_A(tiles[0], ctxs[0], nx(0)))
        for i, j in enumerate(tiles):
            g = make_A(tiles[i + 1], ctxs[i + 1], nx(i + 1)) if i + 1 < len(tiles) else None
            do_B(j, ctxs[i], g)
            exhaust(g)

    def ret_tables(hr):
        lg = math.log(1.0 - 2.0 ** (-5.0 - hr))
        for k in range(10):
            kind = k % 5
            bias = math.log(0.0625) if kind >= 2 else 0.0
            op("act", lambda e, k=k, bias=bias: e.activation(out=rtab[:, k, :], in_=rexpo[:, k, :], func=AF.Exp, scale=lg, bias=bias),
               reads=[rexpo.r], writes=[rtab.r])

    def ret_rot(pmA, pmB, C_, dst, dst_res, s):
        t1, t2 = tA[s], tB[s]
        op("dve", lambda e: e.tensor_tensor(out=t1[:], in0=pmA[:, :], in1=C_[:, 0, :], op=ALU.mult), reads=[pmA.r, C_.r], writes=[t1.r])
        op("dve", lambda e: e.tensor_tensor(out=t2[:], in0=pmB[:, :], in1=C_[:, 1, :], op=ALU.mult), reads=[pmB.r, C_.r], writes=[t2.r])
        op("pool", lambda e: e.tensor_tensor(out=dst[0], in0=t1[:], in1=t2[:], op=ALU.subtract), reads=[t1.r, t2.r], writes=[dst_res])
        t3, t4 = tC[s], tG[s]
        op("dve", lambda e: e.tensor_tensor(out=t3[:], in0=pmA[:, :], in1=C_[:, 1, :], op=ALU.mult), reads=[pmA.r, C_.r], writes=[t3.r])
        op("dve", lambda e: e.tensor_tensor(out=t4[:], in0=pmB[:, :], in1=C_[:, 0, :], op=ALU.mult), reads=[pmB.r, C_.r], writes=[t4.r])
        op("pool", lambda e: e.tensor_tensor(out=dst[1], in0=t3[:], in1=t4[:], op=ALU.add), reads=[t3.r, t4.r], writes=[dst_res])

    def ret_mix(s, dirn, qsrc, qres, ksrc, kres, prescan):
        base = 5 * dirn

        def tb(kind):
            return rtab[:, base + kind, :].unsqueeze(1).to_broadcast([128, 4, 128])

        def b4(ap):
            return ap.rearrange("p (b i) -> p b i", i=128)

        for kc in range(2):
            eng = "dve" if kc == 0 else "pool"
            op(eng, lambda e, kc=kc: e.tensor_tensor(out=b4(KM[s][:, kc, :]), in0=b4(ksrc[kc]), in1=tb(4), op=ALU.mult),
               reads=[kres, rtab.r], writes=[KM[s].r])
            yield
            if prescan:
                continue
            op("dve", lambda e, kc=kc: e.tensor_tensor(out=b4(QT[s][:, kc, :]), in0=b4(qsrc[kc]), in1=tb(0), op=ALU.mult),
               reads=[qres, rtab.r], writes=[QT[s].r])
            op("pool", lambda e, kc=kc: e.tensor_tensor(out=b4(QM[s][:, kc, :]), in0=b4(qsrc[kc]), in1=tb(1), op=ALU.mult),
               reads=[qres, rtab.r], writes=[QM[s].r])
            op("dve", lambda e, kc=kc: e.tensor_tensor(out=b4(KA[s][:, kc, :]), in0=b4(ksrc[kc]), in1=tb(2), op=ALU.mult),
               reads=[kres, rtab.r], writes=[KA[s].r])
            op("pool", lambda e, kc=kc: e.tensor_tensor(out=b4(KB[s][:, kc, :]), in0=b4(ksrc[kc]), in1=tb(3), op=ALU.mult),
               reads=[kres, rtab.r], writes=[KB[s].r])
            yield

    def scan_block(s, b, dirn, nkc, nmc, vap, vres, cdb, mode, oF_slice=None, oF_res=None, osum_t=None):
        dv = nmc * 128
        bl = slice(b * 128, (b + 1) * 128)
        visit["blk"] += 1
        nb = visit["blk"]
        cur = scur["i"]
        Sbf, Sn = Sbfs[cur], Sbfs[1 - cur]
        first = 0 if dirn == 0 else 1
        pt = next_ptr()
        for kc in range(nkc):
            op("pe", lambda e, kc=kc: e.transpose(pt[:, kc, :], KM[s][:, kc, bl], ident[:]), reads=[KM[s].r, ident.r], writes=[pt.r],
               inc=(kc == nkc - 1))
        kt = kTs[nb % 4]
        op("act", lambda e: e.copy(out=kt[:, 0:nkc, :], in_=pt[:, 0:nkc, :]), reads=[pt.r], writes=[kt.r])
        spv = [psS[:, 0:256], psS[:, 256:512]]
        sprl = list(psS.res)
        for kc in range(nkc):
            op("pe", lambda e, kc=kc: e.matmul(spv[kc], lhsT=kt[:, kc, :], rhs=vap, start=True, stop=True),
               reads=[kt.r, vres], writes=sprl, inc=(kc == nkc - 1))
        for kc in range(nkc):
            sc = cdb[kc] if isinstance(cdb, (list, tuple)) else cdb
            op("dve", lambda e, kc=kc, sc=sc: e.scalar_tensor_tensor(out=Sst[:, kc, 0:dv], in0=Sst[:, kc, 0:dv], scalar=sc, in1=spv[kc],
                                                                   op0=ALU.mult, op1=ALU.add),
               reads=[Sst.r] + sprl + ([tcb[s].r] if not isinstance(sc, float) else []), writes=[Sst.r])
        op("act", lambda e: e.copy(out=Sn[:, 0:nkc, 0:dv], in_=Sst[:, 0:nkc, 0:dv]), reads=[Sst.r], writes=[Sn.r])
        scur["i"] = 1 - cur
        if mode == "pre":
            return
        c1 = slice(first * 64, first * 64 + 64)
        c2 = slice((1 - first) * 64, (1 - first) * 64 + 64)
        asl = nb % 4
        at = pat[:, asl * 128:(asl + 1) * 128]
        atr = pat.res[asl]
        for (cols, Ksrc) in ((c1, KA[s]), (c2, KB[s])):
            for kc in range(nkc):
                op("pe", lambda e, cols=cols, Ksrc=Ksrc, kc=kc: e.matmul(
                    at[:, cols], lhsT=Ksrc[:, kc, bl], rhs=QT[s][:, kc, b * 128 + cols.start:b * 128 + cols.stop],
                    start=(kc == 0), stop=(kc == nkc - 1)),
                   reads=[Ksrc.r, QT[s].r], writes=[atr], inc=(kc == nkc - 1))
        am = ATm[nb % 4]
        mk = maskF if dirn == 0 else maskB
        op("dve", lambda e: e.tensor_tensor(out=am[:], in0=at, in1=mk[:], op=ALU.mult), reads=[atr, mk.r], writes=[am.r])
        osl = nb % 2
        ot = po[:, osl * 256:osl * 256 + dv]
        otr = po.res[osl]
        for mc in range(nmc):
            op("pe", lambda e, mc=mc: e.matmul(ot[:, mc * 128:(mc + 1) * 128], lhsT=vap[:, mc * 128:(mc + 1) * 128], rhs=am[:],
                                               start=True, stop=False), reads=[vres, am.r], writes=[otr], inc=False)
            for kc in range(nkc):
                op("pe", lambda e, mc=mc, kc=kc: e.matmul(ot[:, mc * 128:(mc + 1) * 128], lhsT=Sbf[:, kc, mc * 128:(mc + 1) * 128],
                                                          rhs=QM[s][:, kc, bl], start=False, stop=(kc == nkc - 1)),
                   reads=[Sbf.r, QM[s].r], writes=[otr], inc=(kc == nkc - 1 and mc == nmc - 1))
        otv = ot.rearrange("p (m t) -> p m t", t=128)
        if mode == "F":
            op("act", lambda e: e.copy(out=oF_slice, in_=otv), reads=[otr], writes=[oF_res])
        else:
            op("dve", lambda e: e.tensor_tensor(out=osum_t[:, 0:nmc, bl], in0=otv, in1=oF_slice, op=ALU.add),
               reads=[otr, oF_res], writes=[osum_t.r])

    def post_tile(j, nmc, gain_ap, orow0):
        u = j % 2
        os_, sq_, sg_, rs_, oo_ = osum[u], sq[u], sg[u], rstd[u], oOut[u]
        dv = nmc * 128
        op("act", lambda e: e.activation(out=sq_[:, 0:nmc, :], in_=os_[:, 0:nmc, :], func=AF.Square), reads=[os_.r], writes=[sq_.r])
        pm = next_pmm()
        for mc in range(nmc):
            op("pe", lambda e, mc=mc: e.matmul(pm[:, :], lhsT=onesb[:], rhs=sq_[:, mc, :], start=(mc == 0), stop=(mc == nmc - 1)),
               reads=[onesb.r, sq_.r], writes=[pm.r], inc=(mc == nmc - 1))
        op("act", lambda e: e.activation(out=rs_[:], in_=pm[:, :], func=AF.Ln, scale=1.0 / dv, bias=epsr[:, 0:1]),
           reads=[pm.r, epsr.r], writes=[rs_.r])
        op("act", lambda e: e.activation(out=rs_[:], in_=rs_[:], func=AF.Exp, scale=-0.5), reads=[rs_.r], writes=[rs_.r])
        for mc in range(nmc):
            op("dve", lambda e, mc=mc: e.tensor_tensor(out=os_[:, mc, :], in0=os_[:, mc, :], in1=rs_[:], op=ALU.mult),
               reads=[os_.r, rs_.r], writes=[os_.r])
            op("dve", lambda e, mc=mc: e.scalar_tensor_tensor(out=oo_[:, mc, :], in0=os_[:, mc, :], scalar=gain_ap[mc], in1=sg_[:, mc, :],
                                                             op0=ALU.mult, op1=ALU.mult),
               reads=[os_.r, sg_.r, hggT.r, retgT.r], writes=[oo_.r])
        dma("sp", lambda e: e.dma_start(out=oS[orow0:orow0 + dv, j * TT:(j + 1) * TT].rearrange("(m p) t -> p m t", p=128),
                                        in_=oo_[:, 0:nmc, :]), reads=[oo_.r])

    def init_state(src_ap, nkc, dv, src_res=None):
        if src_ap is None:
            op("dve", lambda e: e.memset(Sst[:, 0:nkc, 0:dv], 0.0), writes=[Sst.r])
        else:
            op("dve", lambda e: e.tensor_copy(out=Sst[:, 0:nkc, 0:dv], in_=src_ap), reads=[src_res], writes=[Sst.r])
        Sbf = Sbfs[scur["i"]]
        op("act", lambda e: e.copy(out=Sbf[:, 0:nkc, 0:dv], in_=Sst[:, 0:nkc, 0:dv]), reads=[Sst.r], writes=[Sbf.r])

    def scan_tile_hg(s, order, dirn, vaps, vres, cds, mode, g, oF_slices=None, oF_res=None, osum_t=None):
        first = 0 if dirn == 0 else 1
        c1 = slice(first * 64, first * 64 + 64)
        c2 = slice((1 - first) * 64, (1 - first) * 64 + 64)
        mk = maskF if dirn == 0 else maskB
        for b in order:
            bl = slice(b * 128, (b + 1) * 128)
            if mode != "pre":
                at, atr = pat[:, b * 128:(b + 1) * 128], pat.res[b]
                for (cols, Ksrc) in ((c1, KA[s]), (c2, KB[s])):
                    op("pe", lambda e, cols=cols, Ksrc=Ksrc, at=at, bl=bl, b=b: e.matmul(
                        at[:, cols], lhsT=Ksrc[:, 0, bl], rhs=QT[s][:, 0, b * 128 + cols.start:b * 128 + cols.stop], start=True, stop=True),
                       reads=[Ksrc.r, QT[s].r], writes=[atr], inc=True)
                am = ATm[b]
                op("dve", lambda e, am=am, at=at: e.tensor_tensor(out=am[:], in0=at, in1=mk[:], op=ALU.mult), reads=[atr, mk.r], writes=[am.r])
            pt = next_ptr()
            op("pe", lambda e, pt=pt, bl=bl: e.transpose(pt[:, 0, :], KM[s][:, 0, bl], ident[:]), reads=[KM[s].r, ident.r], writes=[pt.r], inc=True)
            kt = kTs[b]
            op("act", lambda e, kt=kt, pt=pt: e.copy(out=kt[:, 0, :], in_=pt[:, 0, :]), reads=[pt.r], writes=[kt.r])
            op("pe", lambda e, kt=kt, b=b: e.matmul(psS[:, b * 128:(b + 1) * 128], lhsT=kt[:, 0, :], rhs=vaps[b], start=True, stop=True),
               reads=[kt.r, vres], writes=[psS.res[b]], inc=True)
            step(g, 2)
        for b in order:
            bl = slice(b * 128, (b + 1) * 128)
            cur = scur["i"]
            Sb, Sn = Sbfs[cur], Sbfs[1 - cur]
            op("dve", lambda e, b=b: e.scalar_tensor_tensor(out=Sst[:, 0, 0:128], in0=Sst[:, 0, 0:128], scalar=cds[b], in1=psS[:, b * 128:(b + 1) * 128],
                                                           op0=ALU.mult, op1=ALU.add),
               reads=[Sst.r, psS.res[b], tcb[s].r], writes=[Sst.r])
            op("act", lambda e, Sn=Sn: e.copy(out=Sn[:, 0, 0:128], in_=Sst[:, 0, 0:128]), reads=[Sst.r], writes=[Sn.r])
            if mode != "pre":
                osl = b % 2
                ot, otr = po[:, osl * 256:osl * 256 + 128], po.res[osl]
                am = ATm[b]
                op("pe", lambda e, ot=ot, am=am, b=b: e.matmul(ot, lhsT=vaps[b], rhs=am[:], start=True, stop=False),
                   reads=[vres, am.r], writes=[otr], inc=False)
                op("pe", lambda e, ot=ot, Sb=Sb, bl=bl: e.matmul(ot, lhsT=Sb[:, 0, 0:128], rhs=QM[s][:, 0, bl], start=False, stop=True),
                   reads=[Sb.r, QM[s].r], writes=[otr], inc=True)
                if mode == "F":
                    op("act", lambda e, ot=ot, b=b: e.copy(out=oF_slices[b], in_=ot), reads=[otr], writes=[oF_res])
                else:
                    op("dve", lambda e, ot=ot, b=b, bl=bl: e.tensor_tensor(out=osum_t[:, 0, bl], in0=ot, in1=oF_slices[b], op=ALU.add),
                       reads=[otr, oF_res], writes=[osum_t.r])
            scur["i"] = 1 - cur
            step(g, 2)

    NSTEP = 4

    def hg_head(ph, h):
        gbase = NT if ph == 0 else 0
        W = load_w(h * 640, 640)
        prefetch_w()

        def A_fwd(j, ctx, nxt):
            X = load_xT(gbase + j, None if nxt is None else gbase + nxt)
            visit["n"] += 1
            s = visit["n"] % NSET
            ctx["s"], ctx["X"] = s, X
            pmq = inproj_fm(W, 0, X)
            yield
            silu_from_psum(pmq, tE[s], qS[:, 0, j * TT:(j + 1) * TT], qS.res[j])
            yield
            pmf = inproj_fm(W, 128, X)
            yield
            inproj_tm(W, 384, 128, X, vS, vS.res[j], j * 4)
            yield
            yield from hg_prep(h, pmf, qS[:, 0, j * TT:(j + 1) * TT], qS.res[j], s, 0, False)

        def B_fwd(j, ctx, g):
            s = ctx["s"]
            scan_tile_hg(s, list(range(4)), 0, [vS[:, j * 4 + b, 0:128] for b in range(4)], vS.res[j],
                         [tcb[s][:, b:b + 1] for b in range(4)], "F", g,
                         oF_slices=[oF[:, 0, j * TT + b * 128:j * TT + (b + 1) * 128] for b in range(4)], oF_res=oF.res[j])

        def A_bwd(j, ctx, nxt):
            X = load_xT(gbase + j, None if nxt is None else gbase + nxt)
            visit["n"] += 1
            s = visit["n"] % NSET
            ctx["s"], ctx["X"] = s, X
            if ph == 1:
                pmg = inproj_fm(W, 512, X)
                silu_from_psum(pmg, tE[s], sg[j % 2][:, 0, :], sg[j % 2].r)
                yield
            pmf = inproj_fm(W, 256, X)
            yield
            if ph == 0:
                inproj_tm(W, 384, 128, X, vT[s], vT[s].r, 0)
                yield
                yield from hg_prep(h, pmf, None, None, s, 1, True)
            else:
                yield from hg_prep(h, pmf, qS[:, 0, j * TT:(j + 1) * TT], qS.res[j], s, 1, False)

        def B_bwd(j, ctx, g):
            s, X = ctx["s"], ctx["X"]
            cds = [tcb[s][:, b:b + 1] for b in range(4)]
            if ph == 0:
                scan_tile_hg(s, [3, 2, 1, 0], 1, [vT[s][:, b, 0:128] for b in range(4)], vT[s].r, cds, "pre", g)
            else:
                scan_tile_hg(s, [3, 2, 1, 0], 1, [vS[:, j * 4 + b, 0:128] for b in range(4)], vS.res[j], cds, "B", g,
                             oF_slices=[oF[:, 0, j * TT + b * 128:j * TT + (b + 1) * 128] for b in range(4)], oF_res=oF.res[j],
                             osum_t=osum[j % 2])
            if ph == 1:
                post_tile(j, 1, [hggT[:, h:h + 1]], h * 128)

        if ph == 1:
            init_state(None, 1, 128)
            pipeline(range(NT), A_fwd, B_fwd)
            init_state(Sinit_h[:, h, :].unsqueeze(1), 1, 128, Sinit_h.r)
        else:
            init_state(None, 1, 128)
        pipeline(reversed(range(NT)), A_bwd, B_bwd)
        if ph == 0:
            op("dve", lambda e: e.tensor_copy(out=Sinit_h[:, h, :], in_=Sst[:, 0, 0:128]), reads=[Sst.r], writes=[Sinit_h.r])

    def ret_head(ph, hr):
        gbase = NT if ph == 0 else 0
        W = load_w(OFF_RET + hr * 1024, 1024)
        prefetch_w()
        ret_tables(hr)
        gam128 = float((1.0 - 2.0 ** (-5.0 - hr)) ** 128)

        def A_fwd(j, ctx, nxt):
            X = load_xT(gbase + j, None if nxt is None else gbase + nxt)
            C_ = load_cs(gbase + j)
            visit["n"] += 1
            s = visit["n"] % NSET
            ctx["s"], ctx["X"] = s, X
            sl = slice(j * TT, (j + 1) * TT)
            pa, pb = inproj_fm(W, 0, X), inproj_fm(W, 128, X)
            yield
            ret_rot(pa, pb, C_, [qS[:, 0, sl], qS[:, 1, sl]], qS.res[j], s)
            yield
            pa, pb = inproj_fm(W, 256, X), inproj_fm(W, 384, X)
            yield
            kr = krot[s]
            ret_rot(pa, pb, C_, [kr[:, 0, :], kr[:, 1, :]], kr.r, s)
            yield
            inproj_tm(W, 512, 256, X, vS, vS.res[j], j * 4)
            yield
            yield from ret_mix(s, 0, [qS[:, 0, sl], qS[:, 1, sl]], qS.res[j], [kr[:, 0, :], kr[:, 1, :]], kr.r, False)

        def B_fwd(j, ctx, g):
            s = ctx["s"]
            for b in range(4):
                scan_block(s, b, 0, 2, 2, vS[:, j * 4 + b, 0:256], vS.res[j], gam128, "F",
                           oF_slice=oF[:, 0:2, j * TT + b * 128:j * TT + (b + 1) * 128], oF_res=oF.res[j])
                step(g, NSTEP)

        def A_bwd(j, ctx, nxt):
            X = load_xT(gbase + j, None if nxt is None else gbase + nxt)
            C_ = load_cs(gbase + j)
            visit["n"] += 1
            s = visit["n"] % NSET
            ctx["s"], ctx["X"] = s, X
            sl = slice(j * TT, (j + 1) * TT)
            if ph == 1:
                for mc in range(2):
                    pmg = inproj_fm(W, 768 + mc * 128, X)
                    silu_from_psum(pmg, tE[s], sg[j % 2][:, mc, :], sg[j % 2].r)
                yield
            pa, pb = inproj_fm(W, 256, X), inproj_fm(W, 384, X)
            yield
            kr = krot[s]
            ret_rot(pa, pb, C_, [kr[:, 0, :], kr[:, 1, :]], kr.r, s)
            yield
            if ph == 0:
                inproj_tm(W, 512, 256, X, vT[s], vT[s].r, 0)
                yield
                yield from ret_mix(s, 1, None, None, [kr[:, 0, :], kr[:, 1, :]], kr.r, True)
            else:
                yield from ret_mix(s, 1, [qS[:, 0, sl], qS[:, 1, sl]], qS.res[j], [kr[:, 0, :], kr[:, 1, :]], kr.r, False)

        def B_bwd(j, ctx, g):
            s, X = ctx["s"], ctx["X"]
            for b in reversed(range(4)):
                if ph == 0:
                    scan_block(s, b, 1, 2, 2, vT[s][:, b, 0:256], vT[s].r, gam128, "pre")
                else:
                    scan_block(s, b, 1, 2, 2, vS[:, j * 4 + b, 0:256], vS.res[j], gam128, "B",
                               oF_slice=oF[:, 0:2, j * TT + b * 128:j * TT + (b + 1) * 128], oF_res=oF.res[j], osum_t=osum[j % 2])
                step(g, NSTEP)
            if ph == 1:
                post_tile(j, 2, [retgT[:, 2 * hr:2 * hr + 1], retgT[:, 2 * hr + 1:2 * hr + 2]], D + hr * 256)

        if ph == 1:
            init_state(None, 2, 256)
            pipeline(range(NT), A_fwd, B_fwd)
            init_state(Sinit_r[:, hr, :, :], 2, 256, Sinit_r.r)
        else:
            init_state(None, 2, 256)
        pipeline(reversed(range(NT)), A_bwd, B_bwd)
        if ph == 0:
            op("dve", lambda e: e.tensor_copy(out=Sinit_r[:, hr, :, :], in_=Sst[:, 0:2, 0:256]), reads=[Sst.r], writes=[Sinit_r.r])

    for ph in (0, 1):
        for h in range(HG_H):
            hg_head(ph, h)
        for hr in range(RET_H):
            ret_head(ph, hr)

    osync = fence(("sp", "pool"))
    barrier(osync)
    ph_stack.close()

    p23 = ExitStack()
    mmbanks["l"] = [pmm[0], pmm[1], pmm[2], pat, po, psS]
    bgr = S.sb("bgr", [128, 36], F32, stack=p23)
    lng = S.sb("lng", [128, 4, D], F32, stack=p23)
    wgtS = S.sb("wgtS", [128, NB, 32], F32, stack=p23, nres=NB)
    p2 = ExitStack()
    st = p2
    Wg = S.sb("Wg", [128, NKC, 2048], BF16, stack=st)
    Wbh = S.sb("Wbh", [128, NKC, D], BF16, stack=st)
    Wbr = S.sb("Wbr", [128, NKC, D], BF16, stack=st)
    Wo = S.sb("Wo", [128, NKC, D], BF16, stack=st)
    Wgr = S.sb("Wgr", [128, NKC, 36], BF16, stack=st)
    dma("pool", lambda e: e.dma_start(out=Wg[:], in_=win_d[:, OFF_G:OFF_G + 2048].rearrange("(k p) c -> p k c", p=128)), writes=[Wg.r])
    for (Wt, src) in ((Wbh, wbh_d), (Wbr, wbr_d), (Wo, wo_d)):
        dma("pool", lambda e, Wt=Wt, src=src: e.dma_start(out=Wt[:], in_=src.rearrange("(k p) c -> p k c", p=128)), writes=[Wt.r])
    dma("pool", lambda e: e.dma_start(out=Wgr[:], in_=wgr_d.rearrange("(k p) c -> p k c", p=128)), writes=[Wgr.r])
    dma("sp", lambda e: e.dma_start(out=bgr[:], in_=bgr_d[0:1, :].partition_broadcast(128)), writes=[bgr.r])
    for i, src in enumerate((l1g_d, l1b_d, l2g_d, l2b_d)):
        dma("sp", lambda e, i=i, src=src: e.dma_start(out=lng[:, i, :], in_=src[0:1, :].partition_broadcast(128)), writes=[lng.r])

    def layer_norm(z, gi, outt, stt, mv, mul_eng="pool"):
        op("dve", lambda e: e.bn_stats(out=stt[:, 0:6], in_=z[:, 0:512]), reads=[z.r], writes=[stt.r])
        op("dve", lambda e: e.bn_stats(out=stt[:, 6:12], in_=z[:, 512:1024]), reads=[z.r], writes=[stt.r])
        op("dve", lambda e: e.bn_aggr(out=mv[:, 0:2], in_=stt[:, 0:12]), reads=[stt.r], writes=[mv.r])
        op("act", lambda e: e.activation(out=mv[:, 2:3], in_=mv[:, 1:2], func=AF.Sqrt, bias=epsr[:, 1:2]), reads=[mv.r, epsr.r], writes=[mv.r])
        op("dve", lambda e: e.reciprocal(out=mv[:, 3:4], in_=mv[:, 2:3]), reads=[mv.r], writes=[mv.r])
        op("dve", lambda e: e.tensor_scalar(out=z[:], in0=z[:], scalar1=mv[:, 0:1], scalar2=mv[:, 3:4], op0=ALU.subtract, op1=ALU.mult),
           reads=[z.r, mv.r], writes=[z.r])
        op(mul_eng, lambda e: e.tensor_tensor(out=z[:], in0=z[:], in1=lng[:, gi, :], op=ALU.mult), reads=[z.r, lng.r], writes=[z.r])
        op("dve", lambda e: e.tensor_tensor(out=outt[:], in0=z[:], in1=lng[:, gi + 1, :], op=ALU.add), reads=[z.r, lng.r], writes=[outt.r])

    with ExitStack() as st2:
        xT2 = [S.sb(f"xT2{i}", [128, NKC, TT], BF16, stack=st2) for i in range(1)] * 2
        oSt = [S.sb(f"oSt{i}", [128, 16, TT], BF16, stack=st2) for i in range(1)] * 2
        xtok = [S.sb(f"xtok{i}", [128, D], F32, stack=st2) for i in range(2)]
        sga = [S.sb(f"sga{i}", [128, TT], F32, stack=st2) for i in range(2)]
        sgb = [S.sb(f"sgb{i}", [128, TT], F32, stack=st2) for i in range(2)]
        mg = [S.sb(f"mg{i}", [128, NKC, TT], BF16, stack=st2) for i in range(1)] * 2
        zt = [S.sb(f"zt{i}", [128, D], F32, stack=st2) for i in range(2)]
        x1t = [S.sb(f"x1t{i}", [128, D], F32, stack=st2) for i in range(2)]
        x1b = [S.sb(f"x1b{i}", [128, D], BF16, stack=st2) for i in range(2)]
        x1T = [S.sb(f"x1T{i}", [128, NKC, TT], BF16, stack=st2) for i in range(1)] * 2
        stt = [S.sb(f"stt{i}", [128, 12], F32, stack=st2) for i in range(2)]
        mv = [S.sb(f"mv{i}", [128, 4], F32, stack=st2) for i in range(2)]
        rt = [S.sb(f"rt{i}", [128, 96], F32, stack=st2) for i in range(2)]
        nblk2 = 0
        nonlocal_state = {"n": 0}
        for j in range(NT):
            X, O_, M_, XT1 = xT2[j % 2], oSt[j % 2], mg[j % 2], x1T[j % 2]
            dma("sp", lambda e, X=X, j=j: e.dma_start(out=X[:], in_=xTd[j].rearrange("p (k t) -> p k t", t=TT)), reads=[xsync], writes=[X.r])
            dma("sp", lambda e, O_=O_, j=j: e.dma_start(out=O_[:], in_=oS[:, j * TT:(j + 1) * TT].rearrange("(c p) t -> p c t", p=128)),
                reads=[osync], writes=[O_.r])
            for dc in range(NKC):
                u = dc % 2
                pga = inproj_fm(Wg, dc * 128, X)
                op("act", lambda e, pga=pga, u=u: e.activation(out=sga[u][:], in_=pga[:, :], func=AF.Sigmoid), reads=[pga.r], writes=[sga[u].r])
                pgb = inproj_fm(Wg, D + dc * 128, X)
                op("act", lambda e, pgb=pgb, u=u: e.activation(out=sgb[u][:], in_=pgb[:, :], func=AF.Sigmoid), reads=[pgb.r], writes=[sgb[u].r])
                pyh = next_pmm()
                for kc in range(NKC):
                    op("pe", lambda e, pyh=pyh, kc=kc, dc=dc: e.matmul(pyh[:, :], lhsT=Wbh[:, kc, dc * 128:(dc + 1) * 128], rhs=O_[:, kc, :],
                                                                      start=(kc == 0), stop=(kc == NKC - 1)),
                       reads=[Wbh.r, O_.r], writes=[pyh.r], inc=(kc == NKC - 1))
                op("dve", lambda e, pyh=pyh, u=u: e.tensor_tensor(out=sga[u][:], in0=pyh[:, :], in1=sga[u][:], op=ALU.mult),
                   reads=[pyh.r, sga[u].r], writes=[sga[u].r])
                pyr = next_pmm()
                for kc in range(NKC):
                    op("pe", lambda e, pyr=pyr, kc=kc, dc=dc: e.matmul(pyr[:, :], lhsT=Wbr[:, kc, dc * 128:(dc + 1) * 128], rhs=O_[:, 8 + kc, :],
                                                                      start=(kc == 0), stop=(kc == NKC - 1)),
                       reads=[Wbr.r, O_.r], writes=[pyr.r], inc=(kc == NKC - 1))
                op("dve", lambda e, pyr=pyr, u=u: e.tensor_tensor(out=sgb[u][:], in0=pyr[:, :], in1=sgb[u][:], op=ALU.mult),
                   reads=[pyr.r, sgb[u].r], writes=[sgb[u].r])
                op("pool", lambda e, u=u, dc=dc: e.tensor_tensor(out=M_[:, dc, :], in0=sga[u][:], in1=sgb[u][:], op=ALU.add),
                   reads=[sga[u].r, sgb[u].r], writes=[M_.r])
            def part1(b):
                nonlocal_state["n"] += 1
                u = nonlocal_state["n"] % 2
                gb = j * 4 + b
                xk, z_, x1_, x1b_ = xtok[u], zt[u], x1t[u], x1b[u]
                dma("sp", lambda e, xk=xk, gb=gb: e.dma_start(out=xk[:], in_=x_d[gb * 128:(gb + 1) * 128, :]), writes=[xk.r])
                for hf in range(2):
                    pm = next_pmm()
                    for kc in range(NKC):
                        op("pe", lambda e, pm=pm, kc=kc, hf=hf, b=b: e.matmul(pm[:, :], lhsT=M_[:, kc, b * 128:(b + 1) * 128],
                                                                             rhs=Wo[:, kc, hf * 512:(hf + 1) * 512], start=(kc == 0), stop=(kc == NKC - 1)),
                           reads=[M_.r, Wo.r], writes=[pm.r], inc=(kc == NKC - 1))
                    op("dve", lambda e, pm=pm, hf=hf, xk=xk, z_=z_: e.scalar_tensor_tensor(out=z_[:, hf * 512:(hf + 1) * 512], in0=xk[:, hf * 512:(hf + 1) * 512],
                                                                                         scalar=ALPHA, in1=pm[:, :], op0=ALU.mult, op1=ALU.add),
                       reads=[pm.r, xk.r], writes=[z_.r])
                layer_norm(z_, 0, x1_, stt[u], mv[u])
                dma("sp", lambda e, x1_=x1_, gb=gb: e.dma_start(out=x1f[gb * 128:(gb + 1) * 128, :], in_=x1_[:]), reads=[x1_.r])
                op("act", lambda e, x1_=x1_, x1b_=x1b_: e.copy(out=x1b_[:], in_=x1_[:]), reads=[x1_.r], writes=[x1b_.r])
                dma("sp", lambda e, x1b_=x1b_, gb=gb: e.dma_start(out=x1bd[gb * 128:(gb + 1) * 128, :], in_=x1b_[:]), reads=[x1b_.r])

                return u, gb

            def part2(b, u, gb):
                x1b_ = x1b[u]
                pt = next_ptr()
                for kc in range(NKC):
                    op("pe", lambda e, pt=pt, kc=kc, x1b_=x1b_: e.transpose(pt[:, kc, :], x1b_[:, kc * 128:(kc + 1) * 128], ident[:]),
                       reads=[x1b_.r, ident.r], writes=[pt.r], inc=(kc == NKC - 1))
                op("act", lambda e, pt=pt, b=b: e.copy(out=XT1[:, :, b * 128:(b + 1) * 128], in_=pt[:]), reads=[pt.r], writes=[XT1.r])
                pl = next_pmm()
                for kc in range(NKC):
                    op("pe", lambda e, pl=pl, kc=kc, b=b: e.matmul(pl[:, 0:36], lhsT=XT1[:, kc, b * 128:(b + 1) * 128], rhs=Wgr[:, kc, :],
                                                                  start=(kc == 0), stop=(kc == NKC - 1)),
                       reads=[XT1.r, Wgr.r], writes=[pl.r], inc=(kc == NKC - 1))
                R = rt[u]
                rr_ = [R.r]
                op("dve", lambda e, pl=pl, R=R: e.tensor_tensor(out=R[:, 0:36], in0=pl[:, 0:36], in1=bgr[:], op=ALU.add), reads=[pl.r, bgr.r], writes=rr_)
                op("dve", lambda e, R=R: e.reduce_max(out=R[:, 36:37], in_=R[:, 0:4], axis=mybir.AxisListType.X), reads=rr_, writes=rr_)
                op("dve", lambda e, R=R: e.tensor_scalar(out=R[:, 40:44], in0=R[:, 0:4], scalar1=R[:, 36:37], scalar2=None, op0=ALU.is_equal), reads=rr_, writes=rr_)
                op("dve", lambda e, R=R: e.tensor_scalar(out=R[:, 37:38], in0=R[:, 36:37], scalar1=-1.0, scalar2=None, op0=ALU.mult), reads=rr_, writes=rr_)
                op("act", lambda e, R=R: e.activation(out=R[:, 44:48], in_=R[:, 0:4], func=AF.Exp, bias=R[:, 37:38]), reads=rr_, writes=rr_)
                op("dve", lambda e, R=R: e.reduce_sum(out=R[:, 38:39], in_=R[:, 44:48], axis=mybir.AxisListType.X), reads=rr_, writes=rr_)
                op("dve", lambda e, R=R: e.reciprocal(out=R[:, 39:40], in_=R[:, 38:39]), reads=rr_, writes=rr_)
                op("dve", lambda e, R=R: e.tensor_scalar(out=R[:, 48:56], in0=R[:, 4:12], scalar1=R[:, 40:41], scalar2=None, op0=ALU.mult), reads=rr_, writes=rr_)
                for g in range(1, 4):
                    op("dve", lambda e, R=R, g=g: e.scalar_tensor_tensor(out=R[:, 48:56], in0=R[:, 4 + 8 * g:12 + 8 * g], scalar=R[:, 40 + g:41 + g],
                                                                         in1=R[:, 48:56], op0=ALU.mult, op1=ALU.add), reads=rr_, writes=rr_)
                op("dve", lambda e, R=R: e.max(out=R[:, 56:64], in_=R[:, 48:56]), reads=rr_, writes=rr_)
                op("dve", lambda e, R=R: e.tensor_scalar(out=R[:, 64:72], in0=R[:, 48:56], scalar1=R[:, 56:57], scalar2=None, op0=ALU.is_equal), reads=rr_, writes=rr_)
                op("dve", lambda e, R=R: e.tensor_scalar(out=R[:, 72:80], in0=R[:, 48:56], scalar1=R[:, 57:58], scalar2=None, op0=ALU.is_equal), reads=rr_, writes=rr_)
                op("dve", lambda e, R=R: e.tensor_tensor(out=R[:, 80:81], in0=R[:, 57:58], in1=R[:, 56:57], op=ALU.subtract), reads=rr_, writes=rr_)
                op("act", lambda e, R=R: e.activation(out=R[:, 81:82], in_=R[:, 80:81], func=AF.Exp), reads=rr_, writes=rr_)
                op("dve", lambda e, R=R: e.tensor_scalar(out=R[:, 82:83], in0=R[:, 81:82], scalar1=1.0, scalar2=None, op0=ALU.add), reads=rr_, writes=rr_)
                op("dve", lambda e, R=R: e.reciprocal(out=R[:, 83:84], in_=R[:, 82:83]), reads=rr_, writes=rr_)
                op("dve", lambda e, R=R: e.tensor_tensor(out=R[:, 84:85], in0=R[:, 81:82], in1=R[:, 83:84], op=ALU.mult), reads=rr_, writes=rr_)
                op("dve", lambda e, R=R: e.tensor_scalar(out=R[:, 85:87], in0=R[:, 83:85], scalar1=R[:, 39:40], scalar2=None, op0=ALU.mult), reads=rr_, writes=rr_)
                op("dve", lambda e, R=R: e.tensor_scalar(out=R[:, 88:96], in0=R[:, 64:72], scalar1=R[:, 85:86], scalar2=None, op0=ALU.mult), reads=rr_, writes=rr_)
                op("dve", lambda e, R=R: e.scalar_tensor_tensor(out=R[:, 88:96], in0=R[:, 72:80], scalar=R[:, 86:87], in1=R[:, 88:96],
                                                                op0=ALU.mult, op1=ALU.add), reads=rr_, writes=rr_)
                for g in range(4):
                    op("dve", lambda e, R=R, g=g, gb=gb: e.tensor_scalar(out=wgtS[:, gb, g * 8:(g + 1) * 8], in0=R[:, 88:96], scalar1=R[:, 40 + g:41 + g],
                                                                         scalar2=None, op0=ALU.mult), reads=rr_, writes=[wgtS.res[gb]])


            prev = None
            for b in range(4):
                cur_ = part1(b)
                if prev is not None:
                    part2(b - 1, *prev)
                prev = cur_
            part2(3, *prev)
    msync = fence(("sp", "pool"))
    barrier(msync)
    p2.close()

    precast_step(len(pc_jobs))
    with ExitStack() as st3:
        Uup = S.sb("Uup", [128, 128], BF16, stack=st3)
        Mb = S.sb("Mb", [128, NB, NEXP], BF16, stack=st3)
        Mf = S.sb("Mf", [128, NB, NEXP], F32, stack=st3)
        rankS = S.sb("rankS", [128, NB, NEXP], F32, stack=st3)
        Dm = S.sb("Dm", [128, NB, NEXP], F32, stack=st3)
        Eq = S.sb("Eq", [128, NB, NEXP], F32, stack=st3)
        cntS = S.sb("cntS", [128, NEXP], F32, stack=st3)
        thr = S.sb("thr", [128, NEXP], F32, stack=st3)
        thri = S.sb("thri", [128, NEXP], I32, stack=st3)
        cmp1 = S.sb("cmp1", [128, NEXP, NEXP], F32, stack=st3)
        nblk = S.sb("nblk", [128, NEXP], F32, stack=st3)
        pend = S.sb("pend", [128, NEXP], F32, stack=st3)
        pstr = S.sb("pstr", [128, NEXP], F32, stack=st3)
        bidx = S.sb("bidx", [128, NBLK], F32, stack=st3)
        bidxi = S.sb("bidxi", [128, NBLK], I32, stack=st3)
        cmp2 = S.sb("cmp2", [128, NBLK, NEXP], F32, stack=st3)
        bef = S.sb("bef", [128, NBLK], F32, stack=st3)
        pidx = S.sb("pidx", [128, 1], F32, stack=st3)
        pidxi = S.sb("pidxi", [128, 1], I32, stack=st3)
        idxW = S.sb("idxW", [128, NBLK], I32, stack=st3)
        dBf = S.sb("dBf", [128, NB], F32, stack=st3)
        dAf = S.sb("dAf", [128, NB], F32, stack=st3)
        wBf = S.sb("wBf", [128, NB], F32, stack=st3)
        wAf = S.sb("wAf", [128, NB], F32, stack=st3)
        dAi = S.sb("dAi", [128, NB], I32, stack=st3)
        dBi = S.sb("dBi", [128, NB], I32, stack=st3)
        zero = S.sb("zero", [128, 4, D], BF16, stack=st3)
        xblk = [S.sb(f"xblk{i}", [128, D], BF16, stack=st3) for i in range(2)]
        Wt = [S.sb(f"Wt{i}", [128, 12288], BF16, stack=st3) for i in range(3)]
        xsb = [S.sb(f"xsb{i}", [128, D], BF16, stack=st3) for i in range(2)]
        xsT = [S.sb(f"xsT{i}", [128, NKC, 128], BF16, stack=st3) for i in range(2)]
        sl_ = [S.sb(f"sl{i}", [128, 4, 128], F32, stack=st3) for i in range(2)]
        gT = [S.sb(f"gT{i}", [128, 4, 128], BF16, stack=st3) for i in range(2)]
        ysb = [S.sb(f"ysb{i}", [128, D], F32, stack=st3) for i in range(2)]
        yA = [S.sb(f"yA{i}", [128, D], F32, stack=st3) for i in range(3)]
        yB = [S.sb(f"yB{i}", [128, D], F32, stack=st3) for i in range(3)]
        x1l = [S.sb(f"x1l{i}", [128, D], F32, stack=st3) for i in range(3)]
        stt3 = [S.sb(f"stt3{i}", [128, 12], F32, stack=st3) for i in range(4)]
        mv3 = [S.sb(f"mv3{i}", [128, 4], F32, stack=st3) for i in range(4)]
        wres = [wgtS.res[b] for b in range(NB)]
        op("pool", lambda e: e.memset(Uup[:], 1.0), writes=[Uup.r])
        op("pool", lambda e: e.affine_select(out=Uup[:], in_=Uup[:], pattern=[[1, 128]], compare_op=ALU.is_ge, fill=0.0, base=-1,
                                             channel_multiplier=-1), reads=[Uup.r], writes=[Uup.r])
        op("pool", lambda e: e.iota(thri[:], pattern=[[128, NEXP]], base=0, channel_multiplier=0), writes=[thri.r])
        op("dve", lambda e: e.tensor_copy(out=thr[:], in_=thri[:]), reads=[thri.r], writes=[thr.r])
        op("pool", lambda e: e.iota(bidxi[:], pattern=[[1, NBLK]], base=0, channel_multiplier=0), writes=[bidxi.r])
        op("dve", lambda e: e.tensor_copy(out=bidx[:], in_=bidxi[:]), reads=[bidxi.r], writes=[bidx.r])
        op("pool", lambda e: e.iota(pidxi[:], pattern=[[0, 1]], base=0, channel_multiplier=1), writes=[pidxi.r])
        op("dve", lambda e: e.tensor_copy(out=pidx[:], in_=pidxi[:]), reads=[pidxi.r], writes=[pidx.r])
        op("pool", lambda e: e.memset(zero[:], 0.0), writes=[zero.r])
        ztoks = []
        for i0 in range(0, NBLK, 4):
            ztoks.append(dma("sp", lambda e, i0=i0: e.dma_start(out=xs_d[i0 * 128:(i0 + 4) * 128, :].rearrange("(i p) d -> p i d", p=128), in_=zero[:]),
                             reads=[zero.r]))
        flat = lambda t: t[:].rearrange("p b e -> p (b e)")
        op("dve", lambda e: e.tensor_scalar(out=flat(Mb), in0=flat(wgtS), scalar1=0.0, scalar2=None, op0=ALU.is_gt), reads=wres, writes=[Mb.r])
        op("dve", lambda e: e.tensor_scalar(out=flat(Mf), in0=flat(wgtS), scalar1=0.0, scalar2=None, op0=ALU.is_gt), reads=wres, writes=[Mf.r])
        for b in range(NB):
            pm = next_pmm()
            for b2 in range(b):
                op("pe", lambda e, pm=pm, b2=b2: e.matmul(pm[:, 0:NEXP], lhsT=onesb[:], rhs=Mb[:, b2, :], start=(b2 == 0), stop=False),
                   reads=[onesb.r, Mb.r], writes=[pm.r], inc=False)
            op("pe", lambda e, pm=pm, b=b: e.matmul(pm[:, 0:NEXP], lhsT=Uup[:], rhs=Mb[:, b, :], start=(b == 0), stop=True),
               reads=[Uup.r, Mb.r], writes=[pm.r], inc=True)
            op("act", lambda e, pm=pm, b=b: e.copy(out=rankS[:, b, :], in_=pm[:, 0:NEXP]), reads=[pm.r], writes=[rankS.r])
        pm = next_pmm()
        for b2 in range(NB):
            op("pe", lambda e, pm=pm, b2=b2: e.matmul(pm[:, 0:NEXP], lhsT=onesb[:], rhs=Mb[:, b2, :], start=(b2 == 0), stop=(b2 == NB - 1)),
               reads=[onesb.r, Mb.r], writes=[pm.r], inc=(b2 == NB - 1))
        op("act", lambda e, pm=pm: e.copy(out=cntS[:], in_=pm[:, 0:NEXP]), reads=[pm.r], writes=[cntS.r])
        op("dve", lambda e: e.tensor_tensor(out=cmp1[:], in0=bc(cntS[:], NEXP), in1=thr[:].unsqueeze(1).to_broadcast([128, NEXP, NEXP]), op=ALU.is_gt),
           reads=[cntS.r, thr.r], writes=[cmp1.r])
        op("dve", lambda e: e.reduce_sum(out=nblk[:], in_=cmp1[:], axis=mybir.AxisListType.X), reads=[cmp1.r], writes=[nblk.r])
        op("dve", lambda e: e.tensor_tensor_scan(out=pend[:], data0=ones[:, 0:NEXP], data1=nblk[:], initial=0.0, op0=ALU.mult, op1=ALU.add),
           reads=[ones.r, nblk.r], writes=[pend.r])
        op("dve", lambda e: e.tensor_tensor(out=pstr[:], in0=pend[:], in1=nblk[:], op=ALU.subtract), reads=[pend.r, nblk.r], writes=[pstr.r])
        op("dve", lambda e: e.tensor_scalar(out=pstr[:], in0=pstr[:], scalar1=128.0, scalar2=None, op0=ALU.mult), reads=[pstr.r], writes=[pstr.r])
        op("dve", lambda e: e.tensor_tensor(out=cmp2[:], in0=pend[:].unsqueeze(1).to_broadcast([128, NBLK, NEXP]), in1=bc(bidx[:], NEXP), op=ALU.is_le),
           reads=[pend.r, bidx.r], writes=[cmp2.r])
        op("dve", lambda e: e.reduce_sum(out=bef[:], in_=cmp2[:], axis=mybir.AxisListType.X), reads=[cmp2.r], writes=[bef.r])
        op("dve", lambda e: e.tensor_scalar(out=bef[:], in0=bef[:], scalar1=float(NEXP - 1), scalar2=128.0, op0=ALU.min, op1=ALU.mult), reads=[bef.r], writes=[bef.r])
        op("dve", lambda e: e.tensor_scalar(out=idxW[:], in0=bef[:], scalar1=pidx[:, 0:1], scalar2=None, op0=ALU.add), reads=[bef.r, pidx.r], writes=[idxW.r])
        op("dve", lambda e: e.tensor_tensor(out=Dm[:], in0=rankS[:], in1=pstr[:].unsqueeze(1).to_broadcast([128, NB, NEXP]), op=ALU.add),
           reads=[rankS.r, pstr.r], writes=[Dm.r])
        op("dve", lambda e: e.tensor_tensor(out=Dm[:], in0=Dm[:], in1=Mf[:], op=ALU.mult), reads=[Dm.r, Mf.r], writes=[Dm.r])
        op("dve", lambda e: e.reduce_max(out=dBf[:], in_=Dm[:], axis=mybir.AxisListType.X), reads=[Dm.r], writes=[dBf.r])
        op("dve", lambda e: e.reduce_sum(out=dAf[:], in_=Dm[:], axis=mybir.AxisListType.X), reads=[Dm.r], writes=[dAf.r])
        op("dve", lambda e: e.tensor_tensor(out=dAf[:], in0=dAf[:], in1=dBf[:], op=ALU.subtract), reads=[dAf.r, dBf.r], writes=[dAf.r])
        op("dve", lambda e: e.tensor_tensor(out=Eq[:], in0=Dm[:], in1=bc(dBf[:], NEXP), op=ALU.is_equal), reads=[Dm.r, dBf.r], writes=[Eq.r])
        op("dve", lambda e: e.tensor_tensor(out=flat(Eq), in0=flat(Eq), in1=flat(wgtS), op=ALU.mult), reads=[Eq.r] + wres, writes=[Eq.r])
        op("dve", lambda e: e.reduce_sum(out=wBf[:], in_=Eq[:], axis=mybir.AxisListType.X), reads=[Eq.r], writes=[wBf.r])
        op("dve", lambda e: e.reduce_sum(out=wAf[:], in_=wgtS[:], axis=mybir.AxisListType.X), reads=wres, writes=[wAf.r])
        op("dve", lambda e: e.tensor_tensor(out=wAf[:], in0=wAf[:], in1=wBf[:], op=ALU.subtract), reads=[wAf.r, wBf.r], writes=[wAf.r])
        op("dve", lambda e: e.tensor_copy(out=dAi[:], in_=dAf[:]), reads=[dAf.r], writes=[dAi.r])
        op("dve", lambda e: e.tensor_copy(out=dBi[:], in_=dBf[:]), reads=[dBf.r], writes=[dBi.r])
        zres = Res()
        S.wait_all("pool", ztoks[-S.NDS:])
        zres.w = ("pool", S.cnt["pool"])
        stoks = []
        for b in range(NB):
            xb_ = xblk[b % 2]
            dma("sp", lambda e, xb_=xb_, b=b: e.dma_start(out=xb_[:], in_=x1bd[b * 128:(b + 1) * 128, :]), reads=[msync], writes=[xb_.r])
            for di in (dAi, dBi):
                stoks.append(dma("pool", lambda e, xb_=xb_, b=b, di=di: e.indirect_dma_start(
                    out=xs_d[:, :], out_offset=bass.IndirectOffsetOnAxis(ap=di[:, b:b + 1], axis=0), in_=xb_[:, :], in_offset=None),
                    reads=[xb_.r, di.r, zres]))
        sres = Res()
        S.wait_all("sp", stoks[-S.NDS:])
        sres.w = ("sp", S.cnt["sp"])
        banks = [pmm[0], pmm[1], pmm[2], pat, po, psS]
        bstate = {"i": 0}

        def next_bank():
            bstate["i"] += 1
            return banks[bstate["i"] % len(banks)]

        ytoks = []

        def stage1(i):
            u = i % 2
            W_, xs_, xT_, s_, g_ = Wt[i % 3], xsb[u], xsT[u], sl_[u], gT[u]
            dma("pool", lambda e: e.indirect_dma_start(out=W_[:, :], out_offset=None, in_=WS[:, :],
                                                       in_offset=bass.IndirectOffsetOnAxis(ap=idxW[:, i:i + 1], axis=0)),
                reads=[idxW.r, msync], writes=[W_.r])
            dma("sp", lambda e: e.dma_start(out=xs_[:], in_=xs_d[i * 128:(i + 1) * 128, :]), reads=[sres], writes=[xs_.r])
            pt = next_ptr()
            for kc in range(NKC):
                op("pe", lambda e, kc=kc: e.transpose(pt[:, kc, :], xs_[:, kc * 128:(kc + 1) * 128], ident[:]),
                   reads=[xs_.r, ident.r], writes=[pt.r], inc=(kc == NKC - 1))
            op("act", lambda e: e.copy(out=xT_[:], in_=pt[:]), reads=[pt.r], writes=[xT_.r])
            W1 = W_[:, 0:4096].rearrange("p (k f) -> p k f", f=DEXP)
            W3 = W_[:, 4096:8192].rearrange("p (k f) -> p k f", f=DEXP)
            p1, p3 = next_bank(), next_bank()
            for (pp, Wm) in ((p1, W1), (p3, W3)):
                for fc in range(4):
                    for kc in range(NKC):
                        op("pe", lambda e, pp=pp, Wm=Wm, fc=fc, kc=kc: e.matmul(pp[:, fc * 128:(fc + 1) * 128], lhsT=Wm[:, kc, fc * 128:(fc + 1) * 128],
                                                                              rhs=xT_[:, kc, :], start=(kc == 0), stop=(kc == NKC - 1)),
                           reads=[W_.r, xT_.r], writes=[pp.r], inc=(kc == NKC - 1 and fc == 3))
            op("act", lambda e: e.activation(out=s_[:].rearrange("p a b -> p (a b)"), in_=p1[:, :], func=AF.Silu), reads=[p1.r], writes=[s_.r])
            op("dve", lambda e: e.tensor_tensor(out=g_[:].rearrange("p a b -> p (a b)"), in0=p3[:, :], in1=s_[:].rearrange("p a b -> p (a b)"), op=ALU.mult),
               reads=[p3.r, s_.r], writes=[g_.r])

        def stage2(i):
            u = i % 2
            W_, g_, y_ = Wt[i % 3], gT[u], ysb[u]
            W2 = W_[:, 8192:12288].rearrange("p (k f) -> p k f", f=D)
            for h2 in range(2):
                py = next_bank()
                for fc in range(4):
                    op("pe", lambda e, py=py, fc=fc, h2=h2: e.matmul(py[:, :], lhsT=g_[:, fc, :], rhs=W2[:, fc, h2 * 512:(h2 + 1) * 512],
                                                                   start=(fc == 0), stop=(fc == 3)),
                       reads=[g_.r, W_.r], writes=[py.r], inc=(fc == 3))
                if h2 == 0:
                    op("act", lambda e, py=py: e.copy(out=y_[:, 0:512], in_=py[:, :]), reads=[py.r], writes=[y_.r])
                else:
                    op("dve", lambda e, py=py: e.tensor_copy(out=y_[:, 512:1024], in_=py[:, :]), reads=[py.r], writes=[y_.r])
            ytoks.append(dma("sp", lambda e: e.dma_start(out=ys_d[i * 128:(i + 1) * 128, :], in_=y_[:]), reads=[y_.r]))

        stage1(0)
        for i in range(NBLK):
            if i + 1 < NBLK:
                stage1(i + 1)
            stage2(i)
        yres = Res()
        S.wait_all("pool", ytoks[-S.NDS:])
        yres.w = ("pool", S.cnt["pool"])
        for b in range(NB):
            u = b % 3
            dma("sp", lambda e, u=u, b=b: e.dma_start(out=x1l[u][:], in_=x1f[b * 128:(b + 1) * 128, :]), reads=[msync], writes=[x1l[u].r])
            for (yt, di) in ((yA[u], dAi), (yB[u], dBi)):
                dma("pool", lambda e, yt=yt, di=di, b=b: e.indirect_dma_start(out=yt[:, :], out_offset=None, in_=ys_d[:, :],
                                                                            in_offset=bass.IndirectOffsetOnAxis(ap=di[:, b:b + 1], axis=0)),
                    reads=[di.r, yres], writes=[yt.r])
            z_ = x1l[u]
            op("dve", lambda e, u=u, b=b, z_=z_: e.scalar_tensor_tensor(out=yA[u][:], in0=yA[u][:], scalar=wAf[:, b:b + 1], in1=yB[u][:], op0=ALU.mult, op1=ALU.bypass)
               if False else e.tensor_scalar(out=yA[u][:], in0=yA[u][:], scalar1=wAf[:, b:b + 1], scalar2=None, op0=ALU.mult),
               reads=[yA[u].r, wAf.r], writes=[yA[u].r])
            op("dve", lambda e, u=u, b=b: e.scalar_tensor_tensor(out=yA[u][:], in0=yB[u][:], scalar=wBf[:, b:b + 1], in1=yA[u][:], op0=ALU.mult, op1=ALU.add),
               reads=[yA[u].r, yB[u].r, wBf.r], writes=[yA[u].r])
            op("dve", lambda e, u=u, z_=z_: e.scalar_tensor_tensor(out=z_[:], in0=z_[:], scalar=ALPHA, in1=yA[u][:], op0=ALU.mult, op1=ALU.add),
               reads=[z_.r, yA[u].r], writes=[z_.r])
            layer_norm(z_, 2, yB[u], stt3[u], mv3[u], mul_eng="dve")
            dma("sp", lambda e, u=u, b=b: e.dma_start(out=out_d[b * 128:(b + 1) * 128, :], in_=yB[u][:]), reads=[yB[u].r])
        fence(("sp", "pool"))
    p23.close()
    S.emit()
    S.stack.close()
    return nc


def _ret_expo():
    i = np.arange(128)
    ii = i % 64
    c = i // 64
    f = np.zeros((5, 128), np.float64)
    f[0] = ii + 1
    f[1] = i + 1
    f[2] = -(ii + 1)
    f[3] = np.where(c == 0, 63 - ii, -(ii + 1))
    f[4] = 127 - i
    b = f[:, ::-1]
    tab = np.concatenate([f, b], 0).astype(np.float32)
    return np.ascontiguousarray(np.broadcast_to(tab.reshape(1, 1280), (128, 1280)))


def _win_perm(swap):
    K = 1024
    off = {n: i * K for i, n in enumerate(["hq", "hff", "hfb", "hi", "hg", "rq", "rk", "rv", "rg", "ga", "gb"])}
    ff, fb = ("hfb", "hff") if swap else ("hff", "hfb")
    cols = []
    for h in range(HG_H):
        for n in ("hq", ff, fb, "hi", "hg"):
            cols.append(off[n] + h * 128 + np.arange(128))
    for r in range(RET_H):
        perm = np.concatenate([np.arange(0, 256, 2), np.arange(1, 256, 2)])
        cols.append(off["rq"] + r * 256 + perm)
        cols.append(off["rk"] + r * 256 + perm)
        cols.append(off["rv"] + r * 256 + np.arange(256))
        cols.append(off["rg"] + r * 256 + np.arange(256))
    cols.append(off["ga"] + np.arange(K))
    cols.append(off["gb"] + np.arange(K))
    return np.concatenate(cols)


_NC_CACHE = {}


def kernel(x, positions, w_in, hg_lb_logits, hg_norm_g, ret_norm_g, w_branch_hg, w_branch_ret, w_out, ln1_g, ln1_b,
           w_group, b_group, w_router, b_router, w1, w3, w2, ln2_g, ln2_b, _debug=False):
    x = np.asarray(x, np.float32)
    B, L, _ = x.shape
    T = L // 2
    ncores = 2 * B
    key = (T, _debug)
    if key not in _NC_CACHE:
        _NC_CACHE[key] = build(T, _debug)
    nc = _NC_CACHE[key]
    positions = np.asarray(positions, np.int32)
    w_in0 = np.asarray(w_in, np.float32)[0]
    wins = [np.ascontiguousarray(w_in0[:, _win_perm(False)]), np.ascontiguousarray(w_in0[:, _win_perm(True)])]
    invf = (1.0 / (np.float32(10000.0) ** np.linspace(0.0, 1.0, 128, dtype=np.float32))).astype(np.float32).reshape(128, 1)
    f32 = lambda a: np.ascontiguousarray(np.asarray(a, np.float32))
    common = {
        "lbl": f32(hg_lb_logits), "hgg": f32(hg_norm_g), "retg": f32(ret_norm_g),
        "wbh": f32(w_branch_hg)[0], "wbr": f32(w_branch_ret)[0], "wo": f32(w_out)[0],
        "l1g": f32(ln1_g), "l1b": f32(ln1_b), "l2g": f32(ln2_g), "l2b": f32(ln2_b),
        "wgr": np.ascontiguousarray(np.concatenate([f32(w_group)[0], f32(w_router)[0]], 1)),
        "bgr": np.ascontiguousarray(np.concatenate([f32(b_group), f32(b_router)], 1)),
        "w1": f32(w1)[0], "w3": f32(w3)[0], "w2": f32(w2)[0],
        "invf": invf, "rexpo": _ret_expo(),
    }
    in_maps = []
    for c in range(ncores):
        b, half = c // 2, c % 2
        xb, pb = x[b], positions[b]
        if half == 1:
            xb, pb = xb[::-1], pb[::-1]
        m = dict(common)
        m["x"] = np.ascontiguousarray(xb)
        m["pos"] = np.ascontiguousarray(pb).reshape(1, L)
        m["w_in"] = wins[half]
        in_maps.append(m)
    res = run_bass_kernel_spmd(nc, in_maps, core_ids=list(range(ncores)))
    out = np.empty((B, L, D), np.float32)
    for c in range(ncores):
        b, half = c // 2, c % 2
        o = np.asarray(res.results[c]["out"])
        if half == 0:
            out[b, :T] = o
        else:
            out[b, T:] = o[::-1]
    if _debug:
        return out, res.results
    return out
```

```python
from contextlib import ExitStack
import math
import numpy as np
import concourse.bass as bass
import concourse.mybir as mybir
from concourse.bass_utils import run_bass_kernel_spmd

F32 = mybir.dt.float32
BF16 = mybir.dt.bfloat16
I32 = mybir.dt.int32
ALU = mybir.AluOpType
AF = mybir.ActivationFunctionType

D = 1024
NKC = 8
TT = 512
HG_H = 8
RET_H = 4
NEXP = 32
DEXP = 512
ALPHA = 2.0 ** 0.25
LN_EPS = 1e-5
RMS_EPS = 1e-6
OFF_RET = HG_H * 640
OFF_G = OFF_RET + RET_H * 1024
TWO_PI = 2.0 * math.pi
CW1 = 6.28125
CW2 = TWO_PI - CW1


class Res:
    __slots__ = ("w", "r")

    def __init__(self):
        self.w = None
        self.r = {}


class Tile:
    def __init__(self, t, nres=1):
        self.t = t
        self.res = [Res() for _ in range(nres)]

    @property
    def r(self):
        return self.res[0]

    def __getitem__(self, k):
        return self.t[k]


class Sched:
    ENG = ("pe", "act", "dve", "pool", "sp")
    NDS = 8

    def __init__(self, nc):
        self.nc = nc
        self.ops = {e: [] for e in self.ENG}
        self.cnt = {e: 0 for e in self.ENG}
        self.waited = {e: {} for e in self.ENG}
        self.dcount = {e: 0 for e in self.ENG}
        self.stack = ExitStack()

    def sb(self, name, shape, dtype, nres=1, stack=None):
        t = (stack or self.stack).enter_context(self.nc.sbuf_tensor("sb_" + name, list(shape), dtype))
        return Tile(t, nres)

    def ps(self, name, shape, dtype, nres=1):
        t = self.stack.enter_context(self.nc.psum_tensor("ps_" + name, list(shape), dtype))
        return Tile(t, nres)

    def _collect(self, eng, reads, writes, extra=()):
        deps = {}

        def add(tok):
            if tok is None:
                return
            k, v = tok
            if deps.get(k, 0) < v:
                deps[k] = v

        for r in reads:
            add(r.w)
        for w in writes:
            add(w.w)
            for k, v in w.r.items():
                add((k, v))
        for t in extra:
            add(t)
        waits = []
        for k, v in deps.items():
            if k == eng and eng == "pe":
                continue
            if self.waited[eng].get(k, 0) >= v:
                continue
            self.waited[eng][k] = v
            waits.append((k, v))
        return waits

    def _record(self, tok, reads, writes):
        k, v = tok
        for r in reads:
            if r.r.get(k, 0) < v:
                r.r[k] = v
        for w in writes:
            w.w = tok
            w.r = {}

    def op(self, eng, fn, reads=(), writes=(), inc=True):
        assert inc or eng == "pe"
        waits = self._collect(eng, reads, writes)
        if inc:
            self.cnt[eng] += 1
            tok = (eng, self.cnt[eng])
            incinfo = (eng, 1)
        else:
            tok = (eng, self.cnt[eng] + 1)
            incinfo = None
        self.ops[eng].append((waits, fn, incinfo))
        self._record(tok, reads, writes)
        return tok

    def dma(self, q, fn, reads=(), writes=()):
        n = self.dcount[q]
        self.dcount[q] += 1
        slot, rnd = n % self.NDS, n // self.NDS
        key = ("dma", q, slot)
        extra = [(key, 16 * rnd)] if rnd > 0 else []
        waits = self._collect(q, reads, writes, extra)
        tok = (key, 16 * (rnd + 1))
        self.ops[q].append((waits, fn, (key, 16)))
        self._record(tok, reads, writes)
        return tok

    def wait_all(self, eng, toks):
        waits = self._collect(eng, (), (), toks)
        self.cnt[eng] += 1
        self.ops[eng].append((waits, None, (eng, 1)))

    def emit(self):
        nc = self.nc
        keys = set()
        for e in self.ENG:
            for waits, fn, incinfo in self.ops[e]:
                for k, v in waits:
                    keys.add(k)
                if incinfo:
                    keys.add(incinfo[0])
        sems = {}
        for k in sorted(keys, key=str):
            nm = k if isinstance(k, str) else f"d_{k[1]}_{k[2]}"
            sems[k] = self.stack.enter_context(nc.semaphore("s_" + nm))

        def run(name, e):
            for waits, fn, incinfo in self.ops[name]:
                for k, v in waits:
                    e.wait_ge(sems[k], v)
                ins = e.nop() if fn is None else fn(e)
                if incinfo:
                    ins.then_inc(sems[incinfo[0]], incinfo[1])

        with nc.Block() as block:
            @block.tensor
            def _(e):
                run("pe", e)

            @block.scalar
            def _(e):
                run("act", e)

            @block.vector
            def _(e):
                run("dve", e)

            @block.gpsimd
            def _(e):
                run("pool", e)

            @block.sync
            def _(e):
                run("sp", e)


def bc(ap, n):
    return ap.unsqueeze(ap.ndim).to_broadcast(list(ap.shape) + [n])


def build(T, debug=False):
    assert T % TT == 0
    NT = T // TT
    NB = T // 128
    nc = bass.Bass("TRN2", target_bir_lowering=False)
    dt_in = lambda name, shape, dt=F32: nc.dram_tensor(name, list(shape), dt, kind="ExternalInput").ap()
    x_d = dt_in("x", [2 * T, D])
    pos_d = dt_in("pos", [1, 2 * T], I32)
    win_d = dt_in("w_in", [D, 11264])
    lbl_d = dt_in("lbl", [2, D])
    hgg_d = dt_in("hgg", [1, D])
    retg_d = dt_in("retg", [1, D])
    wbh_d = dt_in("wbh", [D, D])
    wbr_d = dt_in("wbr", [D, D])
    wo_d = dt_in("wo", [D, D])
    l1g_d = dt_in("l1g", [1, D])
    l1b_d = dt_in("l1b", [1, D])
    l2g_d = dt_in("l2g", [1, D])
    l2b_d = dt_in("l2b", [1, D])
    wgr_d = dt_in("wgr", [D, 36])
    bgr_d = dt_in("bgr", [1, 36])
    w1_d = dt_in("w1", [NEXP, D, DEXP])
    w3_d = dt_in("w3", [NEXP, D, DEXP])
    w2_d = dt_in("w2", [NEXP, DEXP, D])
    invf_d = dt_in("invf", [128, 1])
    rexpo_d = dt_in("rexpo", [128, 10 * 128])
    out_d = nc.dram_tensor("out", [T, D], F32, kind="ExternalOutput").ap()
    dk = "ExternalOutput" if debug else "Internal"
    xTd = nc.dram_tensor("xTd", [2 * NT, 128, NKC * TT], BF16, kind="Internal").ap()
    csd = nc.dram_tensor("csd", [2 * NT, 128, 2 * TT], F32, kind="Internal").ap()
    oS = nc.dram_tensor("oS", [2 * D, T], BF16, kind=dk).ap()
    x1f = nc.dram_tensor("x1f", [T, D], F32, kind=dk).ap()
    x1Td = nc.dram_tensor("x1Td", [NT, 128, NKC * TT], BF16, kind="Internal").ap()
    NBLK = (2 * T) // 128 + NEXP
    x1bd = nc.dram_tensor("x1bd", [T, D], BF16, kind="Internal").ap()
    xs_d = nc.dram_tensor("xs_d", [NBLK * 128, D], BF16, kind="Internal").ap()
    ys_d = nc.dram_tensor("ys_d", [NBLK * 128, D], F32, kind="Internal").ap()
    WS = nc.dram_tensor("WS", [NEXP * 128, 12288], BF16, kind="Internal").ap()

    S = Sched(nc)
    op, dma = S.op, S.dma

    pmm = [S.ps(f"pmm{i}", [128, 512], F32) for i in range(3)]
    ptr = [S.ps(f"ptr{i}", [128, NKC, 128], BF16) for i in range(2)]
    pat = S.ps("pat", [128, 512], F32, nres=4)
    po = S.ps("po", [128, 512], F32, nres=2)
    psS = S.ps("psS", [128, 512], F32, nres=4)
    pat.res = [pat.res[0]] * 4
    po.res = [po.res[0]] * 2
    psS.res = [psS.res[0]] * 4
    cnt = {"mm": 0, "tr": 0}

    mmbanks = {"l": pmm}

    def next_pmm():
        cnt["mm"] += 1
        l = mmbanks["l"]
        return l[cnt["mm"] % len(l)]

    def next_ptr():
        cnt["tr"] += 1
        return ptr[cnt["tr"] % 2]

    ident = S.sb("ident", [128, 128], BF16)
    maskF = S.sb("maskF", [128, 128], F32)
    maskB = S.sb("maskB", [128, 128], F32)
    ones = S.sb("ones", [128, 512], F32)
    onesb = S.sb("onesb", [128, 128], BF16)
    lbT = S.sb("lbT", [128, 4, HG_H], F32)
    lraw = S.sb("lraw", [128, 2, HG_H], F32)
    hggT = S.sb("hggT", [128, HG_H], F32)
    retgT = S.sb("retgT", [128, 8], F32)
    invf = S.sb("invf", [128, 1], F32)
    epsr = S.sb("epsr", [128, 2], F32)

    op("pool", lambda e: e.memset(ident[:], 1.0), writes=[ident.r])
    op("pool", lambda e: e.affine_select(out=ident[:], in_=ident[:], pattern=[[-1, 128]], compare_op=ALU.is_equal,
                                         fill=0.0, base=0, channel_multiplier=1), reads=[ident.r], writes=[ident.r])
    op("pool", lambda e: e.memset(maskF[:], 1.0), writes=[maskF.r])
    op("pool", lambda e: e.affine_select(out=maskF[:], in_=maskF[:], pattern=[[1, 128]], compare_op=ALU.is_ge,
                                         fill=0.0, base=0, channel_multiplier=-1), reads=[maskF.r], writes=[maskF.r])
    op("pool", lambda e: e.memset(maskB[:], 1.0), writes=[maskB.r])
    op("pool", lambda e: e.affine_select(out=maskB[:], in_=maskB[:], pattern=[[-1, 128]], compare_op=ALU.is_ge,
                                         fill=0.0, base=0, channel_multiplier=1), reads=[maskB.r], writes=[maskB.r])
    op("dve", lambda e: e.memset(ones[:], 1.0), writes=[ones.r])
    op("dve", lambda e: e.memset(onesb[:], 1.0), writes=[onesb.r])
    op("dve", lambda e: e.memset(epsr[:, 0:1], RMS_EPS), writes=[epsr.r])
    op("dve", lambda e: e.memset(epsr[:, 1:2], LN_EPS), writes=[epsr.r])
    dma("sp", lambda e: e.dma_start(out=lraw[:], in_=lbl_d.rearrange("r (h p) -> p r h", p=128), allow_slow_non_contiguous=True), writes=[lraw.r])
    dma("sp", lambda e: e.dma_start(out=hggT[:], in_=hgg_d.rearrange("r (h p) -> p (r h)", p=128), allow_slow_non_contiguous=True), writes=[hggT.r])
    dma("sp", lambda e: e.dma_start(out=retgT[:], in_=retg_d.rearrange("r (h p) -> p (r h)", p=128), allow_slow_non_contiguous=True), writes=[retgT.r])
    dma("sp", lambda e: e.dma_start(out=invf[:], in_=invf_d), writes=[invf.r])
    op("dve", lambda e: e.tensor_tensor(out=lbT[:, 3, :], in0=lraw[:, 1, :], in1=lraw[:, 0, :], op=ALU.subtract),
       reads=[lraw.r], writes=[lbT.r])
    op("act", lambda e: e.activation(out=lbT[:, 0, :], in_=lbT[:, 3, :], func=AF.Sigmoid), reads=[lbT.r], writes=[lbT.r])
    op("dve", lambda e: e.tensor_scalar(out=lbT[:, 1, :], in0=lbT[:, 0, :], scalar1=-1.0, scalar2=1.0, op0=ALU.mult, op1=ALU.add),
       reads=[lbT.r], writes=[lbT.r])
    op("dve", lambda e: e.tensor_scalar(out=lbT[:, 2, :], in0=lbT[:, 1, :], scalar1=-1.0, scalar2=None, op0=ALU.mult),
       reads=[lbT.r], writes=[lbT.r])

    with ExitStack() as st:
        xb = [S.sb(f"xb{i}", [128, 4, D], BF16, stack=st) for i in range(2)]
        xf = [S.sb(f"xf{i}", [128, 4, D], F32, stack=st) for i in range(2)]

        def xload(jj):
            xff = xf[jj % 2]
            dma("sp", lambda e: e.dma_start(out=xff[:], in_=x_d[jj * TT:(jj + 1) * TT, :].rearrange("(b p) d -> p b d", p=128)), writes=[xff.r])

        xload(0)
        xTt = [S.sb(f"xTt{i}", [128, NKC, TT], BF16, stack=st) for i in range(2)]
        GX = 4
        posw = S.sb("posw", [128, GX * TT], I32, stack=st)
        angw = S.sb("angw", [128, GX * TT], F32, stack=st)
        kfw = S.sb("kfw", [128, GX * TT], F32, stack=st)
        kiw = S.sb("kiw", [128, GX * TT], I32, stack=st)
        rrw = S.sb("rrw", [128, GX * TT], F32, stack=st)
        yyw = S.sb("yyw", [128, GX * TT], F32, stack=st)
        mmw = S.sb("mmw", [128, GX * TT], F32, stack=st)
        csw = S.sb("csw", [128, 2, GX * TT], F32, stack=st)
        nblk = 0
        for j in range(2 * NT):
            xt = xTt[j % 2]
            xbb = xb[j % 2]
            xff = xf[j % 2]
            if j + 1 < 2 * NT:
                xload(j + 1)
            for b in range(4):
                if b < 3:
                    op("act", lambda e, xff=xff, xbb=xbb, b=b: e.copy(out=xbb[:, b, :], in_=xff[:, b, :]), reads=[xff.r], writes=[xbb.r])
                else:
                    op("dve", lambda e, xff=xff, xbb=xbb, b=b: e.tensor_copy(out=xbb[:, b, :], in_=xff[:, b, :]), reads=[xff.r], writes=[xbb.r])
            for b in range(4):
                pt = next_ptr()
                for kc in range(NKC):
                    op("pe", lambda e, pt=pt, xbb=xbb, kc=kc, b=b: e.transpose(pt[:, kc, :], xbb[:, b, kc * 128:(kc + 1) * 128], ident[:]),
                       reads=[xbb.r, ident.r], writes=[pt.r], inc=(kc == NKC - 1))
                eng = "act" if b % 2 == 0 else "dve"
                if eng == "act":
                    op("act", lambda e, pt=pt, xt=xt, b=b: e.copy(out=xt[:, :, b * 128:(b + 1) * 128], in_=pt[:]),
                       reads=[pt.r], writes=[xt.r])
                else:
                    op("dve", lambda e, pt=pt, xt=xt, b=b: e.tensor_copy(out=xt[:, :, b * 128:(b + 1) * 128], in_=pt[:]),
                       reads=[pt.r], writes=[xt.r])
            dma("sp", lambda e, xt=xt, j=j: e.dma_start(out=xTd[j].rearrange("p (k t) -> p k t", t=TT), in_=xt[:]), reads=[xt.r])
            if j % GX == 0:
                gw = min(GX, 2 * NT - j) * TT
                pi_, an, kf_, ki_, r_, y_, m_ = posw, angw, kfw, kiw, rrw, yyw, mmw
                dma("sp", lambda e, j=j, gw=gw: e.dma_start(out=posw[:, 0:gw], in_=pos_d[0:1, j * TT:j * TT + gw].partition_broadcast(128)),
                    writes=[posw.r])
                op("dve", lambda e, gw=gw: e.tensor_copy(out=angw[:, 0:gw], in_=posw[:, 0:gw]), reads=[posw.r], writes=[angw.r])
                op("dve", lambda e, gw=gw: e.tensor_scalar(out=angw[:, 0:gw], in0=angw[:, 0:gw], scalar1=invf[:, 0:1], scalar2=None, op0=ALU.mult),
                   reads=[angw.r, invf.r], writes=[angw.r])
                op("dve", lambda e, gw=gw: e.tensor_scalar(out=kiw[:, 0:gw], in0=angw[:, 0:gw], scalar1=1.0 / TWO_PI, scalar2=None, op0=ALU.mult),
                   reads=[angw.r], writes=[kiw.r])
                op("dve", lambda e, gw=gw: e.tensor_copy(out=kfw[:, 0:gw], in_=kiw[:, 0:gw]), reads=[kiw.r], writes=[kfw.r])
                op("dve", lambda e, gw=gw: e.scalar_tensor_tensor(out=rrw[:, 0:gw], in0=kfw[:, 0:gw], scalar=-CW1, in1=angw[:, 0:gw], op0=ALU.mult, op1=ALU.add),
                   reads=[kfw.r, angw.r], writes=[rrw.r])
                op("dve", lambda e, gw=gw: e.scalar_tensor_tensor(out=rrw[:, 0:gw], in0=kfw[:, 0:gw], scalar=-CW2, in1=rrw[:, 0:gw], op0=ALU.mult, op1=ALU.add),
                   reads=[kfw.r, rrw.r], writes=[rrw.r])
                for which, shift in ((1, 0.0), (0, math.pi / 2)):
                    op("pool", lambda e, shift=shift, gw=gw: e.tensor_scalar(out=yyw[:, 0:gw], in0=rrw[:, 0:gw], scalar1=shift, scalar2=None, op0=ALU.add),
                       reads=[rrw.r], writes=[yyw.r])
                    op("dve", lambda e, gw=gw: e.tensor_scalar(out=mmw[:, 0:gw], in0=yyw[:, 0:gw], scalar1=math.pi, scalar2=-TWO_PI, op0=ALU.is_gt, op1=ALU.mult),
                       reads=[yyw.r], writes=[mmw.r])
                    op("dve", lambda e, gw=gw: e.tensor_tensor(out=yyw[:, 0:gw], in0=yyw[:, 0:gw], in1=mmw[:, 0:gw], op=ALU.add), reads=[yyw.r, mmw.r], writes=[yyw.r])
                    op("dve", lambda e, gw=gw: e.tensor_scalar(out=mmw[:, 0:gw], in0=yyw[:, 0:gw], scalar1=-math.pi, scalar2=TWO_PI, op0=ALU.is_lt, op1=ALU.mult),
                       reads=[yyw.r], writes=[mmw.r])
                    op("dve", lambda e, gw=gw: e.tensor_tensor(out=yyw[:, 0:gw], in0=yyw[:, 0:gw], in1=mmw[:, 0:gw], op=ALU.add), reads=[yyw.r, mmw.r], writes=[yyw.r])
                    op("dve", lambda e, gw=gw: e.tensor_scalar(out=yyw[:, 0:gw], in0=yyw[:, 0:gw], scalar1=-3.1415925, scalar2=3.1415925, op0=ALU.max, op1=ALU.min),
                       reads=[yyw.r], writes=[yyw.r])
                    op("act", lambda e, which=which, gw=gw: e.activation(out=csw[:, which, 0:gw], in_=yyw[:, 0:gw], func=AF.Sin), reads=[yyw.r], writes=[csw.r])
            jo = (j % GX) * TT
            dma("sp", lambda e, j=j, jo=jo: e.dma_start(out=csd[j].rearrange("p (k t) -> p k t", t=TT), in_=csw[:, :, jo:jo + TT]), reads=[csw.r])
    def fence(queues=("sp",)):
        toks = []
        for q in queues:
            n = S.dcount[q]
            for sl in range(S.NDS):
                if n > sl:
                    toks.append((("dma", q, sl), 16 * ((n - 1 - ((n - 1 - sl) % S.NDS)) // S.NDS + 1)))
        waits = S._collect("sp", (), (), toks)
        S.cnt["sp"] += 1
        S.ops["sp"].append((waits, None, ("sp", 1)))
        r = Res()
        r.w = ("sp", S.cnt["sp"])
        return r

    def barrier(extra_res):
        toks = [(eng, S.cnt[eng]) for eng in ("pe", "act", "dve", "pool")] + [extra_res.w]
        for eng in S.ENG:
            waits = S._collect(eng, (), (), toks)
            if waits:
                S.cnt[eng] += 1
                S.ops[eng].append((waits, None, (eng, 1)))

    xsync = fence(("sp", "pool"))
    barrier(xsync)

    ph_stack = ExitStack()
    st = ph_stack
    rexpo = S.sb("rexpo", [128, 10, 128], F32, stack=st)
    rtab = S.sb("rtab", [128, 10, 128], F32, stack=st)
    Sinit_h = S.sb("Sinit_h", [128, HG_H, 128], F32, stack=st)
    Sinit_r = S.sb("Sinit_r", [128, RET_H, 2, 256], F32, stack=st)
    dma("sp", lambda e: e.dma_start(out=rexpo[:], in_=rexpo_d.rearrange("p (k i) -> p k i", i=128)), writes=[rexpo.r])
    Wh = [S.sb(f"Wh{i}", [128, NKC, 1024], BF16, stack=st) for i in range(2)]
    xTt = [S.sb(f"xTl{i}", [128, NKC, TT], BF16, stack=st) for i in range(2)]
    cst = [S.sb(f"cst{i}", [128, 2, TT], F32, stack=st) for i in range(1)]
    qS = S.sb("qS", [128, 2, T], BF16, stack=st, nres=NT)
    vS = S.sb("vS", [128, NB, 256], BF16, stack=st, nres=NT)
    oF = S.sb("oF", [128, 2, T], BF16, stack=st, nres=NT)
    Sst = S.sb("Sst", [128, 2, 256], F32, stack=st)
    Sbfs = [S.sb(f"Sbf{i}", [128, 2, 256], BF16, stack=st) for i in range(2)]
    scur = {"i": 0}
    NSET = 2
    tA = [S.sb(f"tA{i}", [128, TT], F32, stack=st) for i in range(NSET)]
    tB = [S.sb(f"tB{i}", [128, TT], F32, stack=st) for i in range(NSET)]
    tC = [S.sb(f"tC{i}", [128, TT], F32, stack=st) for i in range(NSET)]
    tG = [S.sb(f"tG{i}", [128, TT], F32, stack=st) for i in range(NSET)]
    tE = [S.sb(f"tE{i}", [128, TT], F32, stack=st) for i in range(NSET)]
    tF = [S.sb(f"tF{i}", [128, TT], F32, stack=st) for i in range(NSET)]
    tBc = [S.sb(f"tBc{i}", [128, 8], F32, stack=st) for i in range(NSET)]
    tcd = [S.sb(f"tcd{i}", [128, 8], F32, stack=st) for i in range(NSET)]
    tcb = [S.sb(f"tcb{i}", [128, 4], F32, stack=st) for i in range(NSET)]
    QT = [S.sb(f"QT{i}", [128, 2, TT], BF16, stack=st) for i in range(NSET)]
    QM = [S.sb(f"QM{i}", [128, 2, TT], BF16, stack=st) for i in range(NSET)]
    KA = [S.sb(f"KA{i}", [128, 2, TT], BF16, stack=st) for i in range(NSET)]
    KB = [S.sb(f"KB{i}", [128, 2, TT], BF16, stack=st) for i in range(NSET)]
    KM = [S.sb(f"KM{i}", [128, 2, TT], BF16, stack=st) for i in range(NSET)]
    vT = [S.sb(f"vT{i}", [128, 4, 256], BF16, stack=st) for i in range(NSET)]
    krot = [S.sb(f"krot{i}", [128, 2, TT], BF16, stack=st) for i in range(NSET)]
    ATm = [S.sb(f"ATm{i}", [128, 128], BF16, stack=st) for i in range(4)]
    kTs = [S.sb(f"kTs{i}", [128, 2, 128], BF16, stack=st) for i in range(4)]
    osum = [S.sb(f"osum{i}", [128, 2, TT], F32, stack=st) for i in range(2)]
    sq = [S.sb(f"sq{i}", [128, 2, TT], BF16, stack=st) for i in range(1)] * 2
    sg = [S.sb(f"sg{i}", [128, 2, TT], BF16, stack=st) for i in range(2)]
    rstd = [S.sb(f"rstd{i}", [128, TT], F32, stack=st) for i in range(1)] * 2
    oOut = [S.sb(f"oOut{i}", [128, 2, TT], BF16, stack=st) for i in range(2)]
    visit = {"n": 0, "blk": 0, "wh": 0, "xl": 0}

    wsched = []
    for _ph in (0, 1):
        wsched += [(h * 640, 640) for h in range(HG_H)] + [(OFF_RET + hr * 1024, 1024) for hr in range(RET_H)]
    wstate = {"issued": 0, "cur": -1}

    def _issue_w():
        i = wstate["issued"]
        if i >= len(wsched):
            return
        c0, ncols = wsched[i]
        W = Wh[i % 2]
        dma("pool", lambda e: e.dma_start(out=W[:, :, 0:ncols], in_=win_d[:, c0:c0 + ncols].rearrange("(k p) c -> p k c", p=128)),
            writes=[W.r])
        wstate["issued"] += 1

    def load_w(c0, ncols):
        wstate["cur"] += 1
        i = wstate["cur"]
        assert wsched[i] == (c0, ncols)
        while wstate["issued"] <= i:
            _issue_w()
        return Wh[i % 2]

    def prefetch_w():
        if wstate["issued"] <= wstate["cur"] + 1:
            _issue_w()

    pc_jobs = []
    for ex in range(NEXP):
        pc_jobs.append((w1_d[ex].rearrange("(k p) f -> p k f", p=128), ex, 0, 4096, DEXP))
        pc_jobs.append((w3_d[ex].rearrange("(k p) f -> p k f", p=128), ex, 4096, 4096, DEXP))
        pc_jobs.append((w2_d[ex].rearrange("(k p) f -> p k f", p=128), ex, 8192, 4096, D))
    pc_state = {"i": 0}

    def precast_step(n=1):
        for _ in range(n):
            i = pc_state["i"]
            if i >= len(pc_jobs):
                return
            src, ex, c0, w, inner = pc_jobs[i]
            pc_state["i"] += 1
            dma("pool", lambda e, src=src, ex=ex, c0=c0, w=w, inner=inner: e.dma_start(
                out=WS[ex * 128:(ex + 1) * 128, c0:c0 + w].rearrange("p (k f) -> p k f", f=inner), in_=src))

    xpend = {}

    def prefetch_xT(gt):
        if gt is None or gt in xpend:
            return
        visit["xl"] += 1
        X = xTt[visit["xl"] % 2]
        dma("sp", lambda e: e.dma_start(out=X[:], in_=xTd[gt].rearrange("p (k t) -> p k t", t=TT)), reads=[xsync], writes=[X.r])
        xpend[gt] = X

    def load_xT(gt, nxt=None):
        prefetch_xT(gt)
        X = xpend.pop(gt)
        prefetch_xT(nxt)
        precast_step(1)
        return X

    def load_cs(gt):
        C = cst[0]
        dma("sp", lambda e: e.dma_start(out=C[:], in_=csd[gt].rearrange("p (k t) -> p k t", t=TT)), reads=[xsync], writes=[C.r])
        return C

    def inproj_fm(W, c0, X):
        pm = next_pmm()
        for kc in range(NKC):
            op("pe", lambda e, kc=kc: e.matmul(pm[:, :], lhsT=W[:, kc, c0:c0 + 128], rhs=X[:, kc, :], start=(kc == 0), stop=(kc == NKC - 1)),
               reads=[W.r, X.r], writes=[pm.r], inc=(kc == NKC - 1))
        return pm

    def inproj_tm(W, c0, dv, X, dst, dst_res, dst_b0):
        nb_per = 512 // dv
        for g in range(4 // nb_per):
            pm = next_pmm()
            for bb in range(nb_per):
                b = g * nb_per + bb
                for kc in range(NKC):
                    op("pe", lambda e, kc=kc, b=b, bb=bb, pm=pm: e.matmul(pm[:, bb * dv:(bb + 1) * dv], lhsT=X[:, kc, b * 128:(b + 1) * 128],
                                                             rhs=W[:, kc, c0:c0 + dv], start=(kc == 0), stop=(kc == NKC - 1)),
                       reads=[W.r, X.r], writes=[pm.r], inc=(kc == NKC - 1 and bb == nb_per - 1))
            b0 = dst_b0 + g * nb_per
            op("act", lambda e, pm=pm, b0=b0: e.copy(out=dst[:, b0:b0 + nb_per, 0:dv], in_=pm[:, :].rearrange("p (b v) -> p b v", v=dv)),
               reads=[pm.r], writes=[dst_res])

    def v4(ap):
        return ap.rearrange("p (b c i) -> p b c i", c=2, i=64)

    def v8(ap):
        return ap.rearrange("p (c i) -> p c i", i=64)

    def hg_prep(h, pm_f, qsrc, qres, s, dirn, prescan):
        A, B, C, G, E, Fh, Bc, cd, cb = tA[s], tB[s], tC[s], tG[s], tE[s], tF[s], tBc[s], tcd[s], tcb[s]
        first = 0 if dirn == 0 else 1
        second = 1 - first
        lb_h, oml_h, noml_h = lbT[:, 0, h:h + 1], lbT[:, 1, h:h + 1], lbT[:, 2, h:h + 1]
        op("act", lambda e: e.activation(out=B[:], in_=pm_f[:, :], func=AF.Exp, scale=-1.0), reads=[pm_f.r], writes=[B.r])
        yield
        op("act", lambda e: e.activation(out=B[:], in_=B[:], func=AF.Ln, bias=1.0), reads=[B.r], writes=[B.r])
        op("act", lambda e: e.activation(out=A[:], in_=B[:], func=AF.Exp, scale=-1.0), reads=[B.r], writes=[A.r])
        yield
        op("act", lambda e: e.activation(out=B[:], in_=A[:], func=AF.Ln, scale=oml_h, bias=lb_h), reads=[A.r, lbT.r], writes=[B.r])
        op("dve", lambda e: e.tensor_scalar(out=C[:], in0=A[:], scalar1=noml_h, scalar2=oml_h, op0=ALU.mult, op1=ALU.add),
           reads=[A.r, lbT.r], writes=[C.r])
        yield
        op("dve", lambda e: e.tensor_tensor_scan(out=G[:], data0=ones[:], data1=B[:], initial=0.0, op0=ALU.mult, op1=ALU.add),
           reads=[ones.r, B.r], writes=[G.r])
        yield
        if dirn == 0:
            op("pool", lambda e: e.memset(Bc[:, 0:1], 0.0), writes=[Bc.r])
            op("pool", lambda e: e.tensor_copy(out=Bc[:, 1:8], in_=G[:, 63:511:64]), reads=[G.r], writes=[Bc.r])
            op("dve", lambda e: e.tensor_tensor(out=v8(A[:]), in0=v8(G[:]), in1=bc(Bc[:, 0:8], 64), op=ALU.subtract),
               reads=[G.r, Bc.r], writes=[A.r])
        else:
            op("dve", lambda e: e.tensor_tensor(out=A[:], in0=B[:], in1=G[:], op=ALU.subtract), reads=[B.r, G.r], writes=[A.r])
            op("dve", lambda e: e.tensor_tensor(out=v8(A[:]), in0=v8(A[:]), in1=bc(G[:, 63:512:64], 64), op=ALU.add),
               reads=[A.r, G.r], writes=[A.r])
        yield
        op("act", lambda e: e.activation(out=E[:], in_=A[:], func=AF.Exp), reads=[A.r], writes=[E.r])
        op("act", lambda e: e.activation(out=Fh[:], in_=A[:], func=AF.Exp, scale=-1.0), reads=[A.r], writes=[Fh.r])
        yield
        op("dve", lambda e: e.tensor_tensor(out=C[:], in0=C[:], in1=Fh[:], op=ALU.mult), reads=[C.r, Fh.r], writes=[C.r])
        far = 63 if dirn == 0 else 0
        op("pool", lambda e: e.tensor_copy(out=cd[:, 0:8], in_=E[:, far:512:64]), reads=[E.r], writes=[cd.r])
        cd4 = cd[:, 0:8].rearrange("p (b c) -> p b c", c=2)
        op("pool", lambda e: e.tensor_tensor(out=cb[:, 0:4], in0=cd4[:, :, 0], in1=cd4[:, :, 1], op=ALU.mult), reads=[cd.r], writes=[cb.r])
        yield
        op("dve", lambda e: e.tensor_tensor(out=v8(Fh[:]), in0=v8(C[:]), in1=bc(cd[:, 0:8], 64), op=ALU.mult),
           reads=[C.r, cd.r], writes=[Fh.r])
        yield
        km = KM[s]
        F4, C4, km4 = v4(Fh[:]), v4(C[:]), v4(km[:, 0, :])
        op("pool", lambda e: e.tensor_tensor(out=km4[:, :, first, :], in0=F4[:, :, first, :], in1=bc(cd4[:, :, second], 64), op=ALU.mult),
           reads=[Fh.r, cd.r], writes=[km.r])
        op("pool", lambda e: e.tensor_copy(out=km4[:, :, second, :], in_=F4[:, :, second, :]), reads=[Fh.r], writes=[km.r])
        yield
        if prescan:
            return
        qt, qm, ka, kb = QT[s], QM[s], KA[s], KB[s]
        op("dve", lambda e: e.tensor_tensor(out=qt[:, 0, :], in0=qsrc, in1=E[:], op=ALU.mult), reads=[qres, E.r], writes=[qt.r])
        op("pool", lambda e: e.tensor_copy(out=ka[:, 0, :], in_=C[:]), reads=[C.r], writes=[ka.r])
        yield
        kb4, qm4, qt4 = v4(kb[:, 0, :]), v4(qm[:, 0, :]), v4(qt[:, 0, :])
        op("pool", lambda e: e.tensor_copy(out=kb4[:, :, first, :], in_=F4[:, :, first, :]), reads=[Fh.r], writes=[kb.r])
        op("pool", lambda e: e.tensor_copy(out=kb4[:, :, second, :], in_=C4[:, :, second, :]), reads=[C.r], writes=[kb.r])
        yield
        op("pool", lambda e: e.tensor_copy(out=qm4[:, :, first, :], in_=qt4[:, :, first, :]), reads=[qt.r], writes=[qm.r])
        op("pool", lambda e: e.tensor_tensor(out=qm4[:, :, second, :], in0=qt4[:, :, second, :], in1=bc(cd4[:, :, first], 64), op=ALU.mult),
           reads=[qt.r, cd.r], writes=[qm.r])
        yield

    def silu_from_psum(pm, tmp, dst_ap, dst_res):
        op("act", lambda e: e.activation(out=tmp[:], in_=pm[:, :], func=AF.Exp, scale=-1.0), reads=[pm.r], writes=[tmp.r])
        op("act", lambda e: e.activation(out=tmp[:], in_=tmp[:], func=AF.Ln, bias=1.0), reads=[tmp.r], writes=[tmp.r])
        op("act", lambda e: e.activation(out=tmp[:], in_=tmp[:], func=AF.Exp, scale=-1.0), reads=[tmp.r], writes=[tmp.r])
        op("dve", lambda e: e.tensor_tensor(out=dst_ap, in0=pm[:, :], in1=tmp[:], op=ALU.mult), reads=[pm.r, tmp.r], writes=[dst_res])

    def step(g, n):
        if g is None:
            return
        for _ in range(n):
            try:
                next(g)
            except StopIteration:
                return

    def exhaust(g):
        if g is None:
            return
        for _ in g:
            pass

    def pipeline(tiles, make_A, do_B):
        tiles = list(tiles)
        ctxs = [dict() for _ in tiles]
        nx = lambda i: tiles[i + 1] if i + 1 < len(tiles) else None
        exhaust(make_A(tiles[0], ctxs[0], nx(0)))
        for i, j in enumerate(tiles):
            g = make_A(tiles[i + 1], ctxs[i + 1], nx(i + 1)) if i + 1 < len(tiles) else None
            do_B(j, ctxs[i], g)
            exhaust(g)

    def ret_tables(hr):
        lg = math.log(1.0 - 2.0 ** (-5.0 - hr))
        for k in range(10):
            kind = k % 5
            bias = math.log(0.0625) if kind >= 2 else 0.0
            op("act", lambda e, k=k, bias=bias: e.activation(out=rtab[:, k, :], in_=rexpo[:, k, :], func=AF.Exp, scale=lg, bias=bias),
               reads=[rexpo.r], writes=[rtab.r])

    def ret_rot(pmA, pmB, C_, dst, dst_res, s):
        t1, t2 = tA[s], tB[s]
        op("dve", lambda e: e.tensor_tensor(out=t1[:], in0=pmA[:, :], in1=C_[:, 0, :], op=ALU.mult), reads=[pmA.r, C_.r], writes=[t1.r])
        op("dve", lambda e: e.tensor_tensor(out=t2[:], in0=pmB[:, :], in1=C_[:, 1, :], op=ALU.mult), reads=[pmB.r, C_.r], writes=[t2.r])
        op("pool", lambda e: e.tensor_tensor(out=dst[0], in0=t1[:], in1=t2[:], op=ALU.subtract), reads=[t1.r, t2.r], writes=[dst_res])
        t3, t4 = tC[s], tG[s]
        op("dve", lambda e: e.tensor_tensor(out=t3[:], in0=pmA[:, :], in1=C_[:, 1, :], op=ALU.mult), reads=[pmA.r, C_.r], writes=[t3.r])
        op("dve", lambda e: e.tensor_tensor(out=t4[:], in0=pmB[:, :], in1=C_[:, 0, :], op=ALU.mult), reads=[pmB.r, C_.r], writes=[t4.r])
        op("pool", lambda e: e.tensor_tensor(out=dst[1], in0=t3[:], in1=t4[:], op=ALU.add), reads=[t3.r, t4.r], writes=[dst_res])

    def ret_mix(s, dirn, qsrc, qres, ksrc, kres, prescan):
        base = 5 * dirn

        def tb(kind):
            return rtab[:, base + kind, :].unsqueeze(1).to_broadcast([128, 4, 128])

        def b4(ap):
            return ap.rearrange("p (b i) -> p b i", i=128)

        for kc in range(2):
            eng = "dve" if kc == 0 else "pool"
            op(eng, lambda e, kc=kc: e.tensor_tensor(out=b4(KM[s][:, kc, :]), in0=b4(ksrc[kc]), in1=tb(4), op=ALU.mult),
               reads=[kres, rtab.r], writes=[KM[s].r])
            yield
            if prescan:
                continue
            op("dve", lambda e, kc=kc: e.tensor_tensor(out=b4(QT[s][:, kc, :]), in0=b4(qsrc[kc]), in1=tb(0), op=ALU.mult),
               reads=[qres, rtab.r], writes=[QT[s].r])
            op("pool", lambda e, kc=kc: e.tensor_tensor(out=b4(QM[s][:, kc, :]), in0=b4(qsrc[kc]), in1=tb(1), op=ALU.mult),
               reads=[qres, rtab.r], writes=[QM[s].r])
            op("dve", lambda e, kc=kc: e.tensor_tensor(out=b4(KA[s][:, kc, :]), in0=b4(ksrc[kc]), in1=tb(2), op=ALU.mult),
               reads=[kres, rtab.r], writes=[KA[s].r])
            op("pool", lambda e, kc=kc: e.tensor_tensor(out=b4(KB[s][:, kc, :]), in0=b4(ksrc[kc]), in1=tb(3), op=ALU.mult),
               reads=[kres, rtab.r], writes=[KB[s].r])
            yield

    def scan_block(s, b, dirn, nkc, nmc, vap, vres, cdb, mode, oF_slice=None, oF_res=None, osum_t=None):
        dv = nmc * 128
        bl = slice(b * 128, (b + 1) * 128)
        visit["blk"] += 1
        nb = visit["blk"]
        cur = scur["i"]
        Sbf, Sn = Sbfs[cur], Sbfs[1 - cur]
        first = 0 if dirn == 0 else 1
        pt = next_ptr()
        for kc in range(nkc):
            op("pe", lambda e, kc=kc: e.transpose(pt[:, kc, :], KM[s][:, kc, bl], ident[:]), reads=[KM[s].r, ident.r], writes=[pt.r],
               inc=(kc == nkc - 1))
        kt = kTs[nb % 4]
        op("act", lambda e: e.copy(out=kt[:, 0:nkc, :], in_=pt[:, 0:nkc, :]), reads=[pt.r], writes=[kt.r])
        spv = [psS[:, 0:256], psS[:, 256:512]]
        sprl = list(psS.res)
        for kc in range(nkc):
            op("pe", lambda e, kc=kc: e.matmul(spv[kc], lhsT=kt[:, kc, :], rhs=vap, start=True, stop=True),
               reads=[kt.r, vres], writes=sprl, inc=(kc == nkc - 1))
        for kc in range(nkc):
            sc = cdb[kc] if isinstance(cdb, (list, tuple)) else cdb
            op("dve", lambda e, kc=kc, sc=sc: e.scalar_tensor_tensor(out=Sst[:, kc, 0:dv], in0=Sst[:, kc, 0:dv], scalar=sc, in1=spv[kc],
                                                                   op0=ALU.mult, op1=ALU.add),
               reads=[Sst.r] + sprl + ([tcb[s].r] if not isinstance(sc, float) else []), writes=[Sst.r])
        op("act", lambda e: e.copy(out=Sn[:, 0:nkc, 0:dv], in_=Sst[:, 0:nkc, 0:dv]), reads=[Sst.r], writes=[Sn.r])
        scur["i"] = 1 - cur
        if mode == "pre":
            return
        c1 = slice(first * 64, first * 64 + 64)
        c2 = slice((1 - first) * 64, (1 - first) * 64 + 64)
        asl = nb % 4
        at = pat[:, asl * 128:(asl + 1) * 128]
        atr = pat.res[asl]
        for (cols, Ksrc) in ((c1, KA[s]), (c2, KB[s])):
            for kc in range(nkc):
                op("pe", lambda e, cols=cols, Ksrc=Ksrc, kc=kc: e.matmul(
                    at[:, cols], lhsT=Ksrc[:, kc, bl], rhs=QT[s][:, kc, b * 128 + cols.start:b * 128 + cols.stop],
                    start=(kc == 0), stop=(kc == nkc - 1)),
                   reads=[Ksrc.r, QT[s].r], writes=[atr], inc=(kc == nkc - 1))
        am = ATm[nb % 4]
        mk = maskF if dirn == 0 else maskB
        op("dve", lambda e: e.tensor_tensor(out=am[:], in0=at, in1=mk[:], op=ALU.mult), reads=[atr, mk.r], writes=[am.r])
        osl = nb % 2
        ot = po[:, osl * 256:osl * 256 + dv]
        otr = po.res[osl]
        for mc in range(nmc):
            op("pe", lambda e, mc=mc: e.matmul(ot[:, mc * 128:(mc + 1) * 128], lhsT=vap[:, mc * 128:(mc + 1) * 128], rhs=am[:],
                                               start=True, stop=False), reads=[vres, am.r], writes=[otr], inc=False)
            for kc in range(nkc):
                op("pe", lambda e, mc=mc, kc=kc: e.matmul(ot[:, mc * 128:(mc + 1) * 128], lhsT=Sbf[:, kc, mc * 128:(mc + 1) * 128],
                                                          rhs=QM[s][:, kc, bl], start=False, stop=(kc == nkc - 1)),
                   reads=[Sbf.r, QM[s].r], writes=[otr], inc=(kc == nkc - 1 and mc == nmc - 1))
        otv = ot.rearrange("p (m t) -> p m t", t=128)
        if mode == "F":
            op("act", lambda e: e.copy(out=oF_slice, in_=otv), reads=[otr], writes=[oF_res])
        else:
            op("dve", lambda e: e.tensor_tensor(out=osum_t[:, 0:nmc, bl], in0=otv, in1=oF_slice, op=ALU.add),
               reads=[otr, oF_res], writes=[osum_t.r])

    def post_tile(j, nmc, gain_ap, orow0):
        u = j % 2
        os_, sq_, sg_, rs_, oo_ = osum[u], sq[u], sg[u], rstd[u], oOut[u]
        dv = nmc * 128
        op("act", lambda e: e.activation(out=sq_[:, 0:nmc, :], in_=os_[:, 0:nmc, :], func=AF.Square), reads=[os_.r], writes=[sq_.r])
        pm = next_pmm()
        for mc in range(nmc):
            op("pe", lambda e, mc=mc: e.matmul(pm[:, :], lhsT=onesb[:], rhs=sq_[:, mc, :], start=(mc == 0), stop=(mc == nmc - 1)),
               reads=[onesb.r, sq_.r], writes=[pm.r], inc=(mc == nmc - 1))
        op("act", lambda e: e.activation(out=rs_[:], in_=pm[:, :], func=AF.Ln, scale=1.0 / dv, bias=epsr[:, 0:1]),
           reads=[pm.r, epsr.r], writes=[rs_.r])
        op("act", lambda e: e.activation(out=rs_[:], in_=rs_[:], func=AF.Exp, scale=-0.5), reads=[rs_.r], writes=[rs_.r])
        for mc in range(nmc):
            op("dve", lambda e, mc=mc: e.tensor_tensor(out=os_[:, mc, :], in0=os_[:, mc, :], in1=rs_[:], op=ALU.mult),
               reads=[os_.r, rs_.r], writes=[os_.r])
            op("dve", lambda e, mc=mc: e.scalar_tensor_tensor(out=oo_[:, mc, :], in0=os_[:, mc, :], scalar=gain_ap[mc], in1=sg_[:, mc, :],
                                                             op0=ALU.mult, op1=ALU.mult),
               reads=[os_.r, sg_.r, hggT.r, retgT.r], writes=[oo_.r])
        dma("sp", lambda e: e.dma_start(out=oS[orow0:orow0 + dv, j * TT:(j + 1) * TT].rearrange("(m p) t -> p m t", p=128),
                                        in_=oo_[:, 0:nmc, :]), reads=[oo_.r])

    def init_state(src_ap, nkc, dv, src_res=None):
        if src_ap is None:
            op("dve", lambda e: e.memset(Sst[:, 0:nkc, 0:dv], 0.0), writes=[Sst.r])
        else:
            op("dve", lambda e: e.tensor_copy(out=Sst[:, 0:nkc, 0:dv], in_=src_ap), reads=[src_res], writes=[Sst.r])
        Sbf = Sbfs[scur["i"]]
        op("act", lambda e: e.copy(out=Sbf[:, 0:nkc, 0:dv], in_=Sst[:, 0:nkc, 0:dv]), reads=[Sst.r], writes=[Sbf.r])

    def scan_tile_hg(s, order, dirn, vaps, vres, cds, mode, g, oF_slices=None, oF_res=None, osum_t=None):
        first = 0 if dirn == 0 else 1
        c1 = slice(first * 64, first * 64 + 64)
        c2 = slice((1 - first) * 64, (1 - first) * 64 + 64)
        mk = maskF if dirn == 0 else maskB
        for b in order:
            bl = slice(b * 128, (b + 1) * 128)
            if mode != "pre":
                at, atr = pat[:, b * 128:(b + 1) * 128], pat.res[b]
                for (cols, Ksrc) in ((c1, KA[s]), (c2, KB[s])):
                    op("pe", lambda e, cols=cols, Ksrc=Ksrc, at=at, bl=bl, b=b: e.matmul(
                        at[:, cols], lhsT=Ksrc[:, 0, bl], rhs=QT[s][:, 0, b * 128 + cols.start:b * 128 + cols.stop], start=True, stop=True),
                       reads=[Ksrc.r, QT[s].r], writes=[atr], inc=True)
                am = ATm[b]
                op("dve", lambda e, am=am, at=at: e.tensor_tensor(out=am[:], in0=at, in1=mk[:], op=ALU.mult), reads=[atr, mk.r], writes=[am.r])
            pt = next_ptr()
            op("pe", lambda e, pt=pt, bl=bl: e.transpose(pt[:, 0, :], KM[s][:, 0, bl], ident[:]), reads=[KM[s].r, ident.r], writes=[pt.r], inc=True)
            kt = kTs[b]
            op("act", lambda e, kt=kt, pt=pt: e.copy(out=kt[:, 0, :], in_=pt[:, 0, :]), reads=[pt.r], writes=[kt.r])
            op("pe", lambda e, kt=kt, b=b: e.matmul(psS[:, b * 128:(b + 1) * 128], lhsT=kt[:, 0, :], rhs=vaps[b], start=True, stop=True),
               reads=[kt.r, vres], writes=[psS.res[b]], inc=True)
            step(g, 2)
        for b in order:
            bl = slice(b * 128, (b + 1) * 128)
            cur = scur["i"]
            Sb, Sn = Sbfs[cur], Sbfs[1 - cur]
            op("dve", lambda e, b=b: e.scalar_tensor_tensor(out=Sst[:, 0, 0:128], in0=Sst[:, 0, 0:128], scalar=cds[b], in1=psS[:, b * 128:(b + 1) * 128],
                                                           op0=ALU.mult, op1=ALU.add),
               reads=[Sst.r, psS.res[b], tcb[s].r], writes=[Sst.r])
            op("act", lambda e, Sn=Sn: e.copy(out=Sn[:, 0, 0:128], in_=Sst[:, 0, 0:128]), reads=[Sst.r], writes=[Sn.r])
            if mode != "pre":
                osl = b % 2
                ot, otr = po[:, osl * 256:osl * 256 + 128], po.res[osl]
                am = ATm[b]
                op("pe", lambda e, ot=ot, am=am, b=b: e.matmul(ot, lhsT=vaps[b], rhs=am[:], start=True, stop=False),
                   reads=[vres, am.r], writes=[otr], inc=False)
                op("pe", lambda e, ot=ot, Sb=Sb, bl=bl: e.matmul(ot, lhsT=Sb[:, 0, 0:128], rhs=QM[s][:, 0, bl], start=False, stop=True),
                   reads=[Sb.r, QM[s].r], writes=[otr], inc=True)
                if mode == "F":
                    op("act", lambda e, ot=ot, b=b: e.copy(out=oF_slices[b], in_=ot), reads=[otr], writes=[oF_res])
                else:
                    op("dve", lambda e, ot=ot, b=b, bl=bl: e.tensor_tensor(out=osum_t[:, 0, bl], in0=ot, in1=oF_slices[b], op=ALU.add),
                       reads=[otr, oF_res], writes=[osum_t.r])
            scur["i"] = 1 - cur
            step(g, 2)

    NSTEP = 4

    def hg_head(ph, h):
        gbase = NT if ph == 0 else 0
        W = load_w(h * 640, 640)
        prefetch_w()

        def A_fwd(j, ctx, nxt):
            X = load_xT(gbase + j, None if nxt is None else gbase + nxt)
            visit["n"] += 1
            s = visit["n"] % NSET
            ctx["s"], ctx["X"] = s, X
            pmq = inproj_fm(W, 0, X)
            yield
            silu_from_psum(pmq, tE[s], qS[:, 0, j * TT:(j + 1) * TT], qS.res[j])
            yield
            pmf = inproj_fm(W, 128, X)
            yield
            inproj_tm(W, 384, 128, X, vS, vS.res[j], j * 4)
            yield
            yield from hg_prep(h, pmf, qS[:, 0, j * TT:(j + 1) * TT], qS.res[j], s, 0, False)

        def B_fwd(j, ctx, g):
            s = ctx["s"]
            scan_tile_hg(s, list(range(4)), 0, [vS[:, j * 4 + b, 0:128] for b in range(4)], vS.res[j],
                         [tcb[s][:, b:b + 1] for b in range(4)], "F", g,
                         oF_slices=[oF[:, 0, j * TT + b * 128:j * TT + (b + 1) * 128] for b in range(4)], oF_res=oF.res[j])

        def A_bwd(j, ctx, nxt):
            X = load_xT(gbase + j, None if nxt is None else gbase + nxt)
            visit["n"] += 1
            s = visit["n"] % NSET
            ctx["s"], ctx["X"] = s, X
            if ph == 1:
                pmg = inproj_fm(W, 512, X)
                silu_from_psum(pmg, tE[s], sg[j % 2][:, 0, :], sg[j % 2].r)
                yield
            pmf = inproj_fm(W, 256, X)
            yield
            if ph == 0:
                inproj_tm(W, 384, 128, X, vT[s], vT[s].r, 0)
                yield
                yield from hg_prep(h, pmf, None, None, s, 1, True)
            else:
                yield from hg_prep(h, pmf, qS[:, 0, j * TT:(j + 1) * TT], qS.res[j], s, 1, False)

        def B_bwd(j, ctx, g):
            s, X = ctx["s"], ctx["X"]
            cds = [tcb[s][:, b:b + 1] for b in range(4)]
            if ph == 0:
                scan_tile_hg(s, [3, 2, 1, 0], 1, [vT[s][:, b, 0:128] for b in range(4)], vT[s].r, cds, "pre", g)
            else:
                scan_tile_hg(s, [3, 2, 1, 0], 1, [vS[:, j * 4 + b, 0:128] for b in range(4)], vS.res[j], cds, "B", g,
                             oF_slices=[oF[:, 0, j * TT + b * 128:j * TT + (b + 1) * 128] for b in range(4)], oF_res=oF.res[j],
                             osum_t=osum[j % 2])
            if ph == 1:
                post_tile(j, 1, [hggT[:, h:h + 1]], h * 128)

        if ph == 1:
            init_state(None, 1, 128)
            pipeline(range(NT), A_fwd, B_fwd)
            init_state(Sinit_h[:, h, :].unsqueeze(1), 1, 128, Sinit_h.r)
        else:
            init_state(None, 1, 128)
        pipeline(reversed(range(NT)), A_bwd, B_bwd)
        if ph == 0:
            op("dve", lambda e: e.tensor_copy(out=Sinit_h[:, h, :], in_=Sst[:, 0, 0:128]), reads=[Sst.r], writes=[Sinit_h.r])

    def ret_head(ph, hr):
        gbase = NT if ph == 0 else 0
        W = load_w(OFF_RET + hr * 1024, 1024)
        prefetch_w()
        ret_tables(hr)
        gam128 = float((1.0 - 2.0 ** (-5.0 - hr)) ** 128)

        def A_fwd(j, ctx, nxt):
            X = load_xT(gbase + j, None if nxt is None else gbase + nxt)
            C_ = load_cs(gbase + j)
            visit["n"] += 1
            s = visit["n"] % NSET
            ctx["s"], ctx["X"] = s, X
            sl = slice(j * TT, (j + 1) * TT)
            pa, pb = inproj_fm(W, 0, X), inproj_fm(W, 128, X)
            yield
            ret_rot(pa, pb, C_, [qS[:, 0, sl], qS[:, 1, sl]], qS.res[j], s)
            yield
            pa, pb = inproj_fm(W, 256, X), inproj_fm(W, 384, X)
            yield
            kr = krot[s]
            ret_rot(pa, pb, C_, [kr[:, 0, :], kr[:, 1, :]], kr.r, s)
            yield
            inproj_tm(W, 512, 256, X, vS, vS.res[j], j * 4)
            yield
            yield from ret_mix(s, 0, [qS[:, 0, sl], qS[:, 1, sl]], qS.res[j], [kr[:, 0, :], kr[:, 1, :]], kr.r, False)

        def B_fwd(j, ctx, g):
            s = ctx["s"]
            for b in range(4):
                scan_block(s, b, 0, 2, 2, vS[:, j * 4 + b, 0:256], vS.res[j], gam128, "F",
                           oF_slice=oF[:, 0:2, j * TT + b * 128:j * TT + (b + 1) * 128], oF_res=oF.res[j])
                step(g, NSTEP)

        def A_bwd(j, ctx, nxt):
            X = load_xT(gbase + j, None if nxt is None else gbase + nxt)
            C_ = load_cs(gbase + j)
            visit["n"] += 1
            s = visit["n"] % NSET
            ctx["s"], ctx["X"] = s, X
            sl = slice(j * TT, (j + 1) * TT)
            if ph == 1:
                for mc in range(2):
                    pmg = inproj_fm(W, 768 + mc * 128, X)
                    silu_from_psum(pmg, tE[s], sg[j % 2][:, mc, :], sg[j % 2].r)
                yield
            pa, pb = inproj_fm(W, 256, X), inproj_fm(W, 384, X)
            yield
            kr = krot[s]
            ret_rot(pa, pb, C_, [kr[:, 0, :], kr[:, 1, :]], kr.r, s)
            yield
            if ph == 0:
                inproj_tm(W, 512, 256, X, vT[s], vT[s].r, 0)
                yield
                yield from ret_mix(s, 1, None, None, [kr[:, 0, :], kr[:, 1, :]], kr.r, True)
            else:
                yield from ret_mix(s, 1, [qS[:, 0, sl], qS[:, 1, sl]], qS.res[j], [kr[:, 0, :], kr[:, 1, :]], kr.r, False)

        def B_bwd(j, ctx, g):
            s, X = ctx["s"], ctx["X"]
            for b in reversed(range(4)):
                if ph == 0:
                    scan_block(s, b, 1, 2, 2, vT[s][:, b, 0:256], vT[s].r, gam128, "pre")
                else:
                    scan_block(s, b, 1, 2, 2, vS[:, j * 4 + b, 0:256], vS.res[j], gam128, "B",
                               oF_slice=oF[:, 0:2, j * TT + b * 128:j * TT + (b + 1) * 128], oF_res=oF.res[j], osum_t=osum[j % 2])
                step(g, NSTEP)
            if ph == 1:
                post_tile(j, 2, [retgT[:, 2 * hr:2 * hr + 1], retgT[:, 2 * hr + 1:2 * hr + 2]], D + hr * 256)

        if ph == 1:
            init_state(None, 2, 256)
            pipeline(range(NT), A_fwd, B_fwd)
            init_state(Sinit_r[:, hr, :, :], 2, 256, Sinit_r.r)
        else:
            init_state(None, 2, 256)
        pipeline(reversed(range(NT)), A_bwd, B_bwd)
        if ph == 0:
            op("dve", lambda e: e.tensor_copy(out=Sinit_r[:, hr, :, :], in_=Sst[:, 0:2, 0:256]), reads=[Sst.r], writes=[Sinit_r.r])

    for ph in (0, 1):
        for h in range(HG_H):
            hg_head(ph, h)
        for hr in range(RET_H):
            ret_head(ph, hr)

    osync = fence(("sp", "pool"))
    barrier(osync)
    ph_stack.close()

    p23 = ExitStack()
    mmbanks["l"] = [pmm[0], pmm[1], pmm[2], pat, po, psS]
    bgr = S.sb("bgr", [128, 36], F32, stack=p23)
    lng = S.sb("lng", [128, 4, D], F32, stack=p23)
    wgtS = S.sb("wgtS", [128, NB, 32], F32, stack=p23, nres=NB)
    p2 = ExitStack()
    st = p2
    Wg = S.sb("Wg", [128, NKC, 2048], BF16, stack=st)
    Wbh = S.sb("Wbh", [128, NKC, D], BF16, stack=st)
    Wbr = S.sb("Wbr", [128, NKC, D], BF16, stack=st)
    Wo = S.sb("Wo", [128, NKC, D], BF16, stack=st)
    Wgr = S.sb("Wgr", [128, NKC, 36], BF16, stack=st)
    dma("pool", lambda e: e.dma_start(out=Wg[:], in_=win_d[:, OFF_G:OFF_G + 2048].rearrange("(k p) c -> p k c", p=128)), writes=[Wg.r])
    for (Wt, src) in ((Wbh, wbh_d), (Wbr, wbr_d), (Wo, wo_d)):
        dma("pool", lambda e, Wt=Wt, src=src: e.dma_start(out=Wt[:], in_=src.rearrange("(k p) c -> p k c", p=128)), writes=[Wt.r])
    dma("pool", lambda e: e.dma_start(out=Wgr[:], in_=wgr_d.rearrange("(k p) c -> p k c", p=128)), writes=[Wgr.r])
    dma("sp", lambda e: e.dma_start(out=bgr[:], in_=bgr_d[0:1, :].partition_broadcast(128)), writes=[bgr.r])
    for i, src in enumerate((l1g_d, l1b_d, l2g_d, l2b_d)):
        dma("sp", lambda e, i=i, src=src: e.dma_start(out=lng[:, i, :], in_=src[0:1, :].partition_broadcast(128)), writes=[lng.r])

    def layer_norm(z, gi, outt, stt, mv, mul_eng="pool"):
        op("dve", lambda e: e.bn_stats(out=stt[:, 0:6], in_=z[:, 0:512]), reads=[z.r], writes=[stt.r])
        op("dve", lambda e: e.bn_stats(out=stt[:, 6:12], in_=z[:, 512:1024]), reads=[z.r], writes=[stt.r])
        op("dve", lambda e: e.bn_aggr(out=mv[:, 0:2], in_=stt[:, 0:12]), reads=[stt.r], writes=[mv.r])
        op("act", lambda e: e.activation(out=mv[:, 2:3], in_=mv[:, 1:2], func=AF.Sqrt, bias=epsr[:, 1:2]), reads=[mv.r, epsr.r], writes=[mv.r])
        op("dve", lambda e: e.reciprocal(out=mv[:, 3:4], in_=mv[:, 2:3]), reads=[mv.r], writes=[mv.r])
        op("dve", lambda e: e.tensor_scalar(out=z[:], in0=z[:], scalar1=mv[:, 0:1], scalar2=mv[:, 3:4], op0=ALU.subtract, op1=ALU.mult),
           reads=[z.r, mv.r], writes=[z.r])
        op(mul_eng, lambda e: e.tensor_tensor(out=z[:], in0=z[:], in1=lng[:, gi, :], op=ALU.mult), reads=[z.r, lng.r], writes=[z.r])
        op("dve", lambda e: e.tensor_tensor(out=outt[:], in0=z[:], in1=lng[:, gi + 1, :], op=ALU.add), reads=[z.r, lng.r], writes=[outt.r])

    with ExitStack() as st2:
        xT2 = [S.sb(f"xT2{i}", [128, NKC, TT], BF16, stack=st2) for i in range(1)] * 2
        oSt = [S.sb(f"oSt{i}", [128, 16, TT], BF16, stack=st2) for i in range(1)] * 2
        xtok = [S.sb(f"xtok{i}", [128, D], F32, stack=st2) for i in range(2)]
        sga = [S.sb(f"sga{i}", [128, TT], F32, stack=st2) for i in range(2)]
        sgb = [S.sb(f"sgb{i}", [128, TT], F32, stack=st2) for i in range(2)]
        mg = [S.sb(f"mg{i}", [128, NKC, TT], BF16, stack=st2) for i in range(1)] * 2
        zt = [S.sb(f"zt{i}", [128, D], F32, stack=st2) for i in range(2)]
        x1t = [S.sb(f"x1t{i}", [128, D], F32, stack=st2) for i in range(2)]
        x1b = [S.sb(f"x1b{i}", [128, D], BF16, stack=st2) for i in range(2)]
        x1T = [S.sb(f"x1T{i}", [128, NKC, TT], BF16, stack=st2) for i in range(1)] * 2
        stt = [S.sb(f"stt{i}", [128, 12], F32, stack=st2) for i in range(2)]
        mv = [S.sb(f"mv{i}", [128, 4], F32, stack=st2) for i in range(2)]
        rt = [S.sb(f"rt{i}", [128, 96], F32, stack=st2) for i in range(2)]
        nblk2 = 0
        nonlocal_state = {"n": 0}
        for j in range(NT):
            X, O_, M_, XT1 = xT2[j % 2], oSt[j % 2], mg[j % 2], x1T[j % 2]
            dma("sp", lambda e, X=X, j=j: e.dma_start(out=X[:], in_=xTd[j].rearrange("p (k t) -> p k t", t=TT)), reads=[xsync], writes=[X.r])
            dma("sp", lambda e, O_=O_, j=j: e.dma_start(out=O_[:], in_=oS[:, j * TT:(j + 1) * TT].rearrange("(c p) t -> p c t", p=128)),
                reads=[osync], writes=[O_.r])
            for dc in range(NKC):
                u = dc % 2
                pga = inproj_fm(Wg, dc * 128, X)
                op("act", lambda e, pga=pga, u=u: e.activation(out=sga[u][:], in_=pga[:, :], func=AF.Sigmoid), reads=[pga.r], writes=[sga[u].r])
                pgb = inproj_fm(Wg, D + dc * 128, X)
                op("act", lambda e, pgb=pgb, u=u: e.activation(out=sgb[u][:], in_=pgb[:, :], func=AF.Sigmoid), reads=[pgb.r], writes=[sgb[u].r])
                pyh = next_pmm()
                for kc in range(NKC):
                    op("pe", lambda e, pyh=pyh, kc=kc, dc=dc: e.matmul(pyh[:, :], lhsT=Wbh[:, kc, dc * 128:(dc + 1) * 128], rhs=O_[:, kc, :],
                                                                      start=(kc == 0), stop=(kc == NKC - 1)),
                       reads=[Wbh.r, O_.r], writes=[pyh.r], inc=(kc == NKC - 1))
                op("dve", lambda e, pyh=pyh, u=u: e.tensor_tensor(out=sga[u][:], in0=pyh[:, :], in1=sga[u][:], op=ALU.mult),
                   reads=[pyh.r, sga[u].r], writes=[sga[u].r])
                pyr = next_pmm()
                for kc in range(NKC):
                    op("pe", lambda e, pyr=pyr, kc=kc, dc=dc: e.matmul(pyr[:, :], lhsT=Wbr[:, kc, dc * 128:(dc + 1) * 128], rhs=O_[:, 8 + kc, :],
                                                                      start=(kc == 0), stop=(kc == NKC - 1)),
                       reads=[Wbr.r, O_.r], writes=[pyr.r], inc=(kc == NKC - 1))
                op("dve", lambda e, pyr=pyr, u=u: e.tensor_tensor(out=sgb[u][:], in0=pyr[:, :], in1=sgb[u][:], op=ALU.mult),
                   reads=[pyr.r, sgb[u].r], writes=[sgb[u].r])
                op("pool", lambda e, u=u, dc=dc: e.tensor_tensor(out=M_[:, dc, :], in0=sga[u][:], in1=sgb[u][:], op=ALU.add),
                   reads=[sga[u].r, sgb[u].r], writes=[M_.r])
            def part1(b):
                nonlocal_state["n"] += 1
                u = nonlocal_state["n"] % 2
                gb = j * 4 + b
                xk, z_, x1_, x1b_ = xtok[u], zt[u], x1t[u], x1b[u]
                dma("sp", lambda e, xk=xk, gb=gb: e.dma_start(out=xk[:], in_=x_d[gb * 128:(gb + 1) * 128, :]), writes=[xk.r])
                for hf in range(2):
                    pm = next_pmm()
                    for kc in range(NKC):
                        op("pe", lambda e, pm=pm, kc=kc, hf=hf, b=b: e.matmul(pm[:, :], lhsT=M_[:, kc, b * 128:(b + 1) * 128],
                                                                             rhs=Wo[:, kc, hf * 512:(hf + 1) * 512], start=(kc == 0), stop=(kc == NKC - 1)),
                           reads=[M_.r, Wo.r], writes=[pm.r], inc=(kc == NKC - 1))
                    op("dve", lambda e, pm=pm, hf=hf, xk=xk, z_=z_: e.scalar_tensor_tensor(out=z_[:, hf * 512:(hf + 1) * 512], in0=xk[:, hf * 512:(hf + 1) * 512],
                                                                                         scalar=ALPHA, in1=pm[:, :], op0=ALU.mult, op1=ALU.add),
                       reads=[pm.r, xk.r], writes=[z_.r])
                layer_norm(z_, 0, x1_, stt[u], mv[u])
                dma("sp", lambda e, x1_=x1_, gb=gb: e.dma_start(out=x1f[gb * 128:(gb + 1) * 128, :], in_=x1_[:]), reads=[x1_.r])
                op("act", lambda e, x1_=x1_, x1b_=x1b_: e.copy(out=x1b_[:], in_=x1_[:]), reads=[x1_.r], writes=[x1b_.r])
                dma("sp", lambda e, x1b_=x1b_, gb=gb: e.dma_start(out=x1bd[gb * 128:(gb + 1) * 128, :], in_=x1b_[:]), reads=[x1b_.r])

                return u, gb

            def part2(b, u, gb):
                x1b_ = x1b[u]
                pt = next_ptr()
                for kc in range(NKC):
                    op("pe", lambda e, pt=pt, kc=kc, x1b_=x1b_: e.transpose(pt[:, kc, :], x1b_[:, kc * 128:(kc + 1) * 128], ident[:]),
                       reads=[x1b_.r, ident.r], writes=[pt.r], inc=(kc == NKC - 1))
                op("act", lambda e, pt=pt, b=b: e.copy(out=XT1[:, :, b * 128:(b + 1) * 128], in_=pt[:]), reads=[pt.r], writes=[XT1.r])
                pl = next_pmm()
                for kc in range(NKC):
                    op("pe", lambda e, pl=pl, kc=kc, b=b: e.matmul(pl[:, 0:36], lhsT=XT1[:, kc, b * 128:(b + 1) * 128], rhs=Wgr[:, kc, :],
                                                                  start=(kc == 0), stop=(kc == NKC - 1)),
                       reads=[XT1.r, Wgr.r], writes=[pl.r], inc=(kc == NKC - 1))
                R = rt[u]
                rr_ = [R.r]
                op("dve", lambda e, pl=pl, R=R: e.tensor_tensor(out=R[:, 0:36], in0=pl[:, 0:36], in1=bgr[:], op=ALU.add), reads=[pl.r, bgr.r], writes=rr_)
                op("dve", lambda e, R=R: e.reduce_max(out=R[:, 36:37], in_=R[:, 0:4], axis=mybir.AxisListType.X), reads=rr_, writes=rr_)
                op("dve", lambda e, R=R: e.tensor_scalar(out=R[:, 40:44], in0=R[:, 0:4], scalar1=R[:, 36:37], scalar2=None, op0=ALU.is_equal), reads=rr_, writes=rr_)
                op("dve", lambda e, R=R: e.tensor_scalar(out=R[:, 37:38], in0=R[:, 36:37], scalar1=-1.0, scalar2=None, op0=ALU.mult), reads=rr_, writes=rr_)
                op("act", lambda e, R=R: e.activation(out=R[:, 44:48], in_=R[:, 0:4], func=AF.Exp, bias=R[:, 37:38]), reads=rr_, writes=rr_)
                op("dve", lambda e, R=R: e.reduce_sum(out=R[:, 38:39], in_=R[:, 44:48], axis=mybir.AxisListType.X), reads=rr_, writes=rr_)
                op("dve", lambda e, R=R: e.reciprocal(out=R[:, 39:40], in_=R[:, 38:39]), reads=rr_, writes=rr_)
                op("dve", lambda e, R=R: e.tensor_scalar(out=R[:, 48:56], in0=R[:, 4:12], scalar1=R[:, 40:41], scalar2=None, op0=ALU.mult), reads=rr_, writes=rr_)
                for g in range(1, 4):
                    op("dve", lambda e, R=R, g=g: e.scalar_tensor_tensor(out=R[:, 48:56], in0=R[:, 4 + 8 * g:12 + 8 * g], scalar=R[:, 40 + g:41 + g],
                                                                         in1=R[:, 48:56], op0=ALU.mult, op1=ALU.add), reads=rr_, writes=rr_)
                op("dve", lambda e, R=R: e.max(out=R[:, 56:64], in_=R[:, 48:56]), reads=rr_, writes=rr_)
                op("dve", lambda e, R=R: e.tensor_scalar(out=R[:, 64:72], in0=R[:, 48:56], scalar1=R[:, 56:57], scalar2=None, op0=ALU.is_equal), reads=rr_, writes=rr_)
                op("dve", lambda e, R=R: e.tensor_scalar(out=R[:, 72:80], in0=R[:, 48:56], scalar1=R[:, 57:58], scalar2=None, op0=ALU.is_equal), reads=rr_, writes=rr_)
                op("dve", lambda e, R=R: e.tensor_tensor(out=R[:, 80:81], in0=R[:, 57:58], in1=R[:, 56:57], op=ALU.subtract), reads=rr_, writes=rr_)
                op("act", lambda e, R=R: e.activation(out=R[:, 81:82], in_=R[:, 80:81], func=AF.Exp), reads=rr_, writes=rr_)
                op("dve", lambda e, R=R: e.tensor_scalar(out=R[:, 82:83], in0=R[:, 81:82], scalar1=1.0, scalar2=None, op0=ALU.add), reads=rr_, writes=rr_)
                op("dve", lambda e, R=R: e.reciprocal(out=R[:, 83:84], in_=R[:, 82:83]), reads=rr_, writes=rr_)
                op("dve", lambda e, R=R: e.tensor_tensor(out=R[:, 84:85], in0=R[:, 81:82], in1=R[:, 83:84], op=ALU.mult), reads=rr_, writes=rr_)
                op("dve", lambda e, R=R: e.tensor_scalar(out=R[:, 85:87], in0=R[:, 83:85], scalar1=R[:, 39:40], scalar2=None, op0=ALU.mult), reads=rr_, writes=rr_)
                op("dve", lambda e, R=R: e.tensor_scalar(out=R[:, 88:96], in0=R[:, 64:72], scalar1=R[:, 85:86], scalar2=None, op0=ALU.mult), reads=rr_, writes=rr_)
                op("dve", lambda e, R=R: e.scalar_tensor_tensor(out=R[:, 88:96], in0=R[:, 72:80], scalar=R[:, 86:87], in1=R[:, 88:96],
                                                                op0=ALU.mult, op1=ALU.add), reads=rr_, writes=rr_)
                for g in range(4):
                    op("dve", lambda e, R=R, g=g, gb=gb: e.tensor_scalar(out=wgtS[:, gb, g * 8:(g + 1) * 8], in0=R[:, 88:96], scalar1=R[:, 40 + g:41 + g],
                                                                         scalar2=None, op0=ALU.mult), reads=rr_, writes=[wgtS.res[gb]])


            prev = None
            for b in range(4):
                cur_ = part1(b)
                if prev is not None:
                    part2(b - 1, *prev)
                prev = cur_
            part2(3, *prev)
    msync = fence(("sp", "pool"))
    barrier(msync)
    p2.close()

    precast_step(len(pc_jobs))
    with ExitStack() as st3:
        Uup = S.sb("Uup", [128, 128], BF16, stack=st3)
        Mb = S.sb("Mb", [128, NB, NEXP], BF16, stack=st3)
        Mf = S.sb("Mf", [128, NB, NEXP], F32, stack=st3)
        rankS = S.sb("rankS", [128, NB, NEXP], F32, stack=st3)
        Dm = S.sb("Dm", [128, NB, NEXP], F32, stack=st3)
        Eq = S.sb("Eq", [128, NB, NEXP], F32, stack=st3)
        cntS = S.sb("cntS", [128, NEXP], F32, stack=st3)
        thr = S.sb("thr", [128, NEXP], F32, stack=st3)
        thri = S.sb("thri", [128, NEXP], I32, stack=st3)
        cmp1 = S.sb("cmp1", [128, NEXP, NEXP], F32, stack=st3)
        nblk = S.sb("nblk", [128, NEXP], F32, stack=st3)
        pend = S.sb("pend", [128, NEXP], F32, stack=st3)
        pstr = S.sb("pstr", [128, NEXP], F32, stack=st3)
        bidx = S.sb("bidx", [128, NBLK], F32, stack=st3)
        bidxi = S.sb("bidxi", [128, NBLK], I32, stack=st3)
        cmp2 = S.sb("cmp2", [128, NBLK, NEXP], F32, stack=st3)
        bef = S.sb("bef", [128, NBLK], F32, stack=st3)
        pidx = S.sb("pidx", [128, 1], F32, stack=st3)
        pidxi = S.sb("pidxi", [128, 1], I32, stack=st3)
        idxW = S.sb("idxW", [128, NBLK], I32, stack=st3)
        dBf = S.sb("dBf", [128, NB], F32, stack=st3)
        dAf = S.sb("dAf", [128, NB], F32, stack=st3)
        wBf = S.sb("wBf", [128, NB], F32, stack=st3)
        wAf = S.sb("wAf", [128, NB], F32, stack=st3)
        dAi = S.sb("dAi", [128, NB], I32, stack=st3)
        dBi = S.sb("dBi", [128, NB], I32, stack=st3)
        zero = S.sb("zero", [128, 4, D], BF16, stack=st3)
        xblk = [S.sb(f"xblk{i}", [128, D], BF16, stack=st3) for i in range(2)]
        Wt = [S.sb(f"Wt{i}", [128, 12288], BF16, stack=st3) for i in range(3)]
        xsb = [S.sb(f"xsb{i}", [128, D], BF16, stack=st3) for i in range(2)]
        xsT = [S.sb(f"xsT{i}", [128, NKC, 128], BF16, stack=st3) for i in range(2)]
        sl_ = [S.sb(f"sl{i}", [128, 4, 128], F32, stack=st3) for i in range(2)]
        gT = [S.sb(f"gT{i}", [128, 4, 128], BF16, stack=st3) for i in range(2)]
        ysb = [S.sb(f"ysb{i}", [128, D], F32, stack=st3) for i in range(2)]
        yA = [S.sb(f"yA{i}", [128, D], F32, stack=st3) for i in range(3)]
        yB = [S.sb(f"yB{i}", [128, D], F32, stack=st3) for i in range(3)]
        x1l = [S.sb(f"x1l{i}", [128, D], F32, stack=st3) for i in range(3)]
        stt3 = [S.sb(f"stt3{i}", [128, 12], F32, stack=st3) for i in range(4)]
        mv3 = [S.sb(f"mv3{i}", [128, 4], F32, stack=st3) for i in range(4)]
        wres = [wgtS.res[b] for b in range(NB)]
        op("pool", lambda e: e.memset(Uup[:], 1.0), writes=[Uup.r])
        op("pool", lambda e: e.affine_select(out=Uup[:], in_=Uup[:], pattern=[[1, 128]], compare_op=ALU.is_ge, fill=0.0, base=-1,
                                             channel_multiplier=-1), reads=[Uup.r], writes=[Uup.r])
        op("pool", lambda e: e.iota(thri[:], pattern=[[128, NEXP]], base=0, channel_multiplier=0), writes=[thri.r])
        op("dve", lambda e: e.tensor_copy(out=thr[:], in_=thri[:]), reads=[thri.r], writes=[thr.r])
        op("pool", lambda e: e.iota(bidxi[:], pattern=[[1, NBLK]], base=0, channel_multiplier=0), writes=[bidxi.r])
        op("dve", lambda e: e.tensor_copy(out=bidx[:], in_=bidxi[:]), reads=[bidxi.r], writes=[bidx.r])
        op("pool", lambda e: e.iota(pidxi[:], pattern=[[0, 1]], base=0, channel_multiplier=1), writes=[pidxi.r])
        op("dve", lambda e: e.tensor_copy(out=pidx[:], in_=pidxi[:]), reads=[pidxi.r], writes=[pidx.r])
        op("pool", lambda e: e.memset(zero[:], 0.0), writes=[zero.r])
        ztoks = []
        for i0 in range(0, NBLK, 4):
            ztoks.append(dma("sp", lambda e, i0=i0: e.dma_start(out=xs_d[i0 * 128:(i0 + 4) * 128, :].rearrange("(i p) d -> p i d", p=128), in_=zero[:]),
                             reads=[zero.r]))
        flat = lambda t: t[:].rearrange("p b e -> p (b e)")
        op("dve", lambda e: e.tensor_scalar(out=flat(Mb), in0=flat(wgtS), scalar1=0.0, scalar2=None, op0=ALU.is_gt), reads=wres, writes=[Mb.r])
        op("dve", lambda e: e.tensor_scalar(out=flat(Mf), in0=flat(wgtS), scalar1=0.0, scalar2=None, op0=ALU.is_gt), reads=wres, writes=[Mf.r])
        for b in range(NB):
            pm = next_pmm()
            for b2 in range(b):
                op("pe", lambda e, pm=pm, b2=b2: e.matmul(pm[:, 0:NEXP], lhsT=onesb[:], rhs=Mb[:, b2, :], start=(b2 == 0), stop=False),
                   reads=[onesb.r, Mb.r], writes=[pm.r], inc=False)
            op("pe", lambda e, pm=pm, b=b: e.matmul(pm[:, 0:NEXP], lhsT=Uup[:], rhs=Mb[:, b, :], start=(b == 0), stop=True),
               reads=[Uup.r, Mb.r], writes=[pm.r], inc=True)
            op("act", lambda e, pm=pm, b=b: e.copy(out=rankS[:, b, :], in_=pm[:, 0:NEXP]), reads=[pm.r], writes=[rankS.r])
        pm = next_pmm()
        for b2 in range(NB):
            op("pe", lambda e, pm=pm, b2=b2: e.matmul(pm[:, 0:NEXP], lhsT=onesb[:], rhs=Mb[:, b2, :], start=(b2 == 0), stop=(b2 == NB - 1)),
               reads=[onesb.r, Mb.r], writes=[pm.r], inc=(b2 == NB - 1))
        op("act", lambda e, pm=pm: e.copy(out=cntS[:], in_=pm[:, 0:NEXP]), reads=[pm.r], writes=[cntS.r])
        op("dve", lambda e: e.tensor_tensor(out=cmp1[:], in0=bc(cntS[:], NEXP), in1=thr[:].unsqueeze(1).to_broadcast([128, NEXP, NEXP]), op=ALU.is_gt),
           reads=[cntS.r, thr.r], writes=[cmp1.r])
        op("dve", lambda e: e.reduce_sum(out=nblk[:], in_=cmp1[:], axis=mybir.AxisListType.X), reads=[cmp1.r], writes=[nblk.r])
        op("dve", lambda e: e.tensor_tensor_scan(out=pend[:], data0=ones[:, 0:NEXP], data1=nblk[:], initial=0.0, op0=ALU.mult, op1=ALU.add),
           reads=[ones.r, nblk.r], writes=[pend.r])
        op("dve", lambda e: e.tensor_tensor(out=pstr[:], in0=pend[:], in1=nblk[:], op=ALU.subtract), reads=[pend.r, nblk.r], writes=[pstr.r])
        op("dve", lambda e: e.tensor_scalar(out=pstr[:], in0=pstr[:], scalar1=128.0, scalar2=None, op0=ALU.mult), reads=[pstr.r], writes=[pstr.r])
        op("dve", lambda e: e.tensor_tensor(out=cmp2[:], in0=pend[:].unsqueeze(1).to_broadcast([128, NBLK, NEXP]), in1=bc(bidx[:], NEXP), op=ALU.is_le),
           reads=[pend.r, bidx.r], writes=[cmp2.r])
        op("dve", lambda e: e.reduce_sum(out=bef[:], in_=cmp2[:], axis=mybir.AxisListType.X), reads=[cmp2.r], writes=[bef.r])
        op("dve", lambda e: e.tensor_scalar(out=bef[:], in0=bef[:], scalar1=float(NEXP - 1), scalar2=128.0, op0=ALU.min, op1=ALU.mult), reads=[bef.r], writes=[bef.r])
        op("dve", lambda e: e.tensor_scalar(out=idxW[:], in0=bef[:], scalar1=pidx[:, 0:1], scalar2=None, op0=ALU.add), reads=[bef.r, pidx.r], writes=[idxW.r])
        op("dve", lambda e: e.tensor_tensor(out=Dm[:], in0=rankS[:], in1=pstr[:].unsqueeze(1).to_broadcast([128, NB, NEXP]), op=ALU.add),
           reads=[rankS.r, pstr.r], writes=[Dm.r])
        op("dve", lambda e: e.tensor_tensor(out=Dm[:], in0=Dm[:], in1=Mf[:], op=ALU.mult), reads=[Dm.r, Mf.r], writes=[Dm.r])
        op("dve", lambda e: e.reduce_max(out=dBf[:], in_=Dm[:], axis=mybir.AxisListType.X), reads=[Dm.r], writes=[dBf.r])
        op("dve", lambda e: e.reduce_sum(out=dAf[:], in_=Dm[:], axis=mybir.AxisListType.X), reads=[Dm.r], writes=[dAf.r])
        op("dve", lambda e: e.tensor_tensor(out=dAf[:], in0=dAf[:], in1=dBf[:], op=ALU.subtract), reads=[dAf.r, dBf.r], writes=[dAf.r])
        op("dve", lambda e: e.tensor_tensor(out=Eq[:], in0=Dm[:], in1=bc(dBf[:], NEXP), op=ALU.is_equal), reads=[Dm.r, dBf.r], writes=[Eq.r])
        op("dve", lambda e: e.tensor_tensor(out=flat(Eq), in0=flat(Eq), in1=flat(wgtS), op=ALU.mult), reads=[Eq.r] + wres, writes=[Eq.r])
        op("dve", lambda e: e.reduce_sum(out=wBf[:], in_=Eq[:], axis=mybir.AxisListType.X), reads=[Eq.r], writes=[wBf.r])
        op("dve", lambda e: e.reduce_sum(out=wAf[:], in_=wgtS[:], axis=mybir.AxisListType.X), reads=wres, writes=[wAf.r])
        op("dve", lambda e: e.tensor_tensor(out=wAf[:], in0=wAf[:], in1=wBf[:], op=ALU.subtract), reads=[wAf.r, wBf.r], writes=[wAf.r])
        op("dve", lambda e: e.tensor_copy(out=dAi[:], in_=dAf[:]), reads=[dAf.r], writes=[dAi.r])
        op("dve", lambda e: e.tensor_copy(out=dBi[:], in_=dBf[:]), reads=[dBf.r], writes=[dBi.r])
        zres = Res()
        S.wait_all("pool", ztoks[-S.NDS:])
        zres.w = ("pool", S.cnt["pool"])
        stoks = []
        for b in range(NB):
            xb_ = xblk[b % 2]
            dma("sp", lambda e, xb_=xb_, b=b: e.dma_start(out=xb_[:], in_=x1bd[b * 128:(b + 1) * 128, :]), reads=[msync], writes=[xb_.r])
            for di in (dAi, dBi):
                stoks.append(dma("pool", lambda e, xb_=xb_, b=b, di=di: e.indirect_dma_start(
                    out=xs_d[:, :], out_offset=bass.IndirectOffsetOnAxis(ap=di[:, b:b + 1], axis=0), in_=xb_[:, :], in_offset=None),
                    reads=[xb_.r, di.r, zres]))
        sres = Res()
        S.wait_all("sp", stoks[-S.NDS:])
        sres.w = ("sp", S.cnt["sp"])
        banks = [pmm[0], pmm[1], pmm[2], pat, po, psS]
        bstate = {"i": 0}

        def next_bank():
            bstate["i"] += 1
            return banks[bstate["i"] % len(banks)]

        ytoks = []

        def stage1(i):
            u = i % 2
            W_, xs_, xT_, s_, g_ = Wt[i % 3], xsb[u], xsT[u], sl_[u], gT[u]
            dma("pool", lambda e: e.indirect_dma_start(out=W_[:, :], out_offset=None, in_=WS[:, :],
                                                       in_offset=bass.IndirectOffsetOnAxis(ap=idxW[:, i:i + 1], axis=0)),
                reads=[idxW.r, msync], writes=[W_.r])
            dma("sp", lambda e: e.dma_start(out=xs_[:], in_=xs_d[i * 128:(i + 1) * 128, :]), reads=[sres], writes=[xs_.r])
            pt = next_ptr()
            for kc in range(NKC):
                op("pe", lambda e, kc=kc: e.transpose(pt[:, kc, :], xs_[:, kc * 128:(kc + 1) * 128], ident[:]),
                   reads=[xs_.r, ident.r], writes=[pt.r], inc=(kc == NKC - 1))
            op("act", lambda e: e.copy(out=xT_[:], in_=pt[:]), reads=[pt.r], writes=[xT_.r])
            W1 = W_[:, 0:4096].rearrange("p (k f) -> p k f", f=DEXP)
            W3 = W_[:, 4096:8192].rearrange("p (k f) -> p k f", f=DEXP)
            p1, p3 = next_bank(), next_bank()
            for (pp, Wm) in ((p1, W1), (p3, W3)):
                for fc in range(4):
                    for kc in range(NKC):
                        op("pe", lambda e, pp=pp, Wm=Wm, fc=fc, kc=kc: e.matmul(pp[:, fc * 128:(fc + 1) * 128], lhsT=Wm[:, kc, fc * 128:(fc + 1) * 128],
                                                                              rhs=xT_[:, kc, :], start=(kc == 0), stop=(kc == NKC - 1)),
                           reads=[W_.r, xT_.r], writes=[pp.r], inc=(kc == NKC - 1 and fc == 3))
            op("act", lambda e: e.activation(out=s_[:].rearrange("p a b -> p (a b)"), in_=p1[:, :], func=AF.Silu), reads=[p1.r], writes=[s_.r])
            op("dve", lambda e: e.tensor_tensor(out=g_[:].rearrange("p a b -> p (a b)"), in0=p3[:, :], in1=s_[:].rearrange("p a b -> p (a b)"), op=ALU.mult),
               reads=[p3.r, s_.r], writes=[g_.r])

        def stage2(i):
            u = i % 2
            W_, g_, y_ = Wt[i % 3], gT[u], ysb[u]
            W2 = W_[:, 8192:12288].rearrange("p (k f) -> p k f", f=D)
            for h2 in range(2):
                py = next_bank()
                for fc in range(4):
                    op("pe", lambda e, py=py, fc=fc, h2=h2: e.matmul(py[:, :], lhsT=g_[:, fc, :], rhs=W2[:, fc, h2 * 512:(h2 + 1) * 512],
                                                                   start=(fc == 0), stop=(fc == 3)),
                       reads=[g_.r, W_.r], writes=[py.r], inc=(fc == 3))
                if h2 == 0:
                    op("act", lambda e, py=py: e.copy(out=y_[:, 0:512], in_=py[:, :]), reads=[py.r], writes=[y_.r])
                else:
                    op("dve", lambda e, py=py: e.tensor_copy(out=y_[:, 512:1024], in_=py[:, :]), reads=[py.r], writes=[y_.r])
            ytoks.append(dma("sp", lambda e: e.dma_start(out=ys_d[i * 128:(i + 1) * 128, :], in_=y_[:]), reads=[y_.r]))

        stage1(0)
        for i in range(NBLK):
            if i + 1 < NBLK:
                stage1(i + 1)
            stage2(i)
        yres = Res()
        S.wait_all("pool", ytoks[-S.NDS:])
        yres.w = ("pool", S.cnt["pool"])
        for b in range(NB):
            u = b % 3
            dma("sp", lambda e, u=u, b=b: e.dma_start(out=x1l[u][:], in_=x1f[b * 128:(b + 1) * 128, :]), reads=[msync], writes=[x1l[u].r])
            for (yt, di) in ((yA[u], dAi), (yB[u], dBi)):
                dma("pool", lambda e, yt=yt, di=di, b=b: e.indirect_dma_start(out=yt[:, :], out_offset=None, in_=ys_d[:, :],
                                                                            in_offset=bass.IndirectOffsetOnAxis(ap=di[:, b:b + 1], axis=0)),
                    reads=[di.r, yres], writes=[yt.r])
            z_ = x1l[u]
            op("dve", lambda e, u=u, b=b, z_=z_: e.scalar_tensor_tensor(out=yA[u][:], in0=yA[u][:], scalar=wAf[:, b:b + 1], in1=yB[u][:], op0=ALU.mult, op1=ALU.bypass)
               if False else e.tensor_scalar(out=yA[u][:], in0=yA[u][:], scalar1=wAf[:, b:b + 1], scalar2=None, op0=ALU.mult),
               reads=[yA[u].r, wAf.r], writes=[yA[u].r])
            op("dve", lambda e, u=u, b=b: e.scalar_tensor_tensor(out=yA[u][:], in0=yB[u][:], scalar=wBf[:, b:b + 1], in1=yA[u][:], op0=ALU.mult, op1=ALU.add),
               reads=[yA[u].r, yB[u].r, wBf.r], writes=[yA[u].r])
            op("dve", lambda e, u=u, z_=z_: e.scalar_tensor_tensor(out=z_[:], in0=z_[:], scalar=ALPHA, in1=yA[u][:], op0=ALU.mult, op1=ALU.add),
               reads=[z_.r, yA[u].r], writes=[z_.r])
            layer_norm(z_, 2, yB[u], stt3[u], mv3[u], mul_eng="dve")
            dma("sp", lambda e, u=u, b=b: e.dma_start(out=out_d[b * 128:(b + 1) * 128, :], in_=yB[u][:]), reads=[yB[u].r])
        fence(("sp", "pool"))
    p23.close()
    S.emit()
    S.stack.close()
    return nc


def _ret_expo():
    i = np.arange(128)
    ii = i % 64
    c = i // 64
    f = np.zeros((5, 128), np.float64)
    f[0] = ii + 1
    f[1] = i + 1
    f[2] = -(ii + 1)
    f[3] = np.where(c == 0, 63 - ii, -(ii + 1))
    f[4] = 127 - i
    b = f[:, ::-1]
    tab = np.concatenate([f, b], 0).astype(np.float32)
    return np.ascontiguousarray(np.broadcast_to(tab.reshape(1, 1280), (128, 1280)))


def _win_perm(swap):
    K = 1024
    off = {n: i * K for i, n in enumerate(["hq", "hff", "hfb", "hi", "hg", "rq", "rk", "rv", "rg", "ga", "gb"])}
    ff, fb = ("hfb", "hff") if swap else ("hff", "hfb")
    cols = []
    for h in range(HG_H):
        for n in ("hq", ff, fb, "hi", "hg"):
            cols.append(off[n] + h * 128 + np.arange(128))
    for r in range(RET_H):
        perm = np.concatenate([np.arange(0, 256, 2), np.arange(1, 256, 2)])
        cols.append(off["rq"] + r * 256 + perm)
        cols.append(off["rk"] + r * 256 + perm)
        cols.append(off["rv"] + r * 256 + np.arange(256))
        cols.append(off["rg"] + r * 256 + np.arange(256))
    cols.append(off["ga"] + np.arange(K))
    cols.append(off["gb"] + np.arange(K))
    return np.concatenate(cols)


_NC_CACHE = {}


def kernel(x, positions, w_in, hg_lb_logits, hg_norm_g, ret_norm_g, w_branch_hg, w_branch_ret, w_out, ln1_g, ln1_b,
           w_group, b_group, w_router, b_router, w1, w3, w2, ln2_g, ln2_b, _debug=False):
    x = np.asarray(x, np.float32)
    B, L, _ = x.shape
    T = L // 2
    ncores = 2 * B
    key = (T, _debug)
    if key not in _NC_CACHE:
        _NC_CACHE[key] = build(T, _debug)
    nc = _NC_CACHE[key]
    positions = np.asarray(positions, np.int32)
    w_in0 = np.asarray(w_in, np.float32)[0]
    wins = [np.ascontiguousarray(w_in0[:, _win_perm(False)]), np.ascontiguousarray(w_in0[:, _win_perm(True)])]
    invf = (1.0 / (np.float32(10000.0) ** np.linspace(0.0, 1.0, 128, dtype=np.float32))).astype(np.float32).reshape(128, 1)
    f32 = lambda a: np.ascontiguousarray(np.asarray(a, np.float32))
    common = {
        "lbl": f32(hg_lb_logits), "hgg": f32(hg_norm_g), "retg": f32(ret_norm_g),
        "wbh": f32(w_branch_hg)[0], "wbr": f32(w_branch_ret)[0], "wo": f32(w_out)[0],
        "l1g": f32(ln1_g), "l1b": f32(ln1_b), "l2g": f32(ln2_g), "l2b": f32(ln2_b),
        "wgr": np.ascontiguousarray(np.concatenate([f32(w_group)[0], f32(w_router)[0]], 1)),
        "bgr": np.ascontiguousarray(np.concatenate([f32(b_group), f32(b_router)], 1)),
        "w1": f32(w1)[0], "w3": f32(w3)[0], "w2": f32(w2)[0],
        "invf": invf, "rexpo": _ret_expo(),
    }
    in_maps = []
    for c in range(ncores):
        b, half = c // 2, c % 2
        xb, pb = x[b], positions[b]
        if half == 1:
            xb, pb = xb[::-1], pb[::-1]
        m = dict(common)
        m["x"] = np.ascontiguousarray(xb)
        m["pos"] = np.ascontiguousarray(pb).reshape(1, L)
        m["w_in"] = wins[half]
        in_maps.append(m)
    res = run_bass_kernel_spmd(nc, in_maps, core_ids=list(range(ncores)))
    out = np.empty((B, L, D), np.float32)
    for c in range(ncores):
        b, half = c // 2, c % 2
        o = np.asarray(res.results[c]["out"])
        if half == 0:
            out[b, :T] = o
        else:
            out[b, T:] = o[::-1]
    if _debug:
        return out, res.results
    return out
```

```python
from contextlib import ExitStack
import math
import numpy as np
import concourse.bass as bass
import concourse.mybir as mybir
from concourse.bass_utils import run_bass_kernel_spmd

F32 = mybir.dt.float32
BF16 = mybir.dt.bfloat16
I32 = mybir.dt.int32
ALU = mybir.AluOpType
AF = mybir.ActivationFunctionType

D = 1024
NKC = 8
TT = 512
HG_H = 8
RET_H = 4
NEXP = 32
DEXP = 512
ALPHA = 2.0 ** 0.25
LN_EPS = 1e-5
RMS_EPS = 1e-6
OFF_RET = HG_H * 640
OFF_G = OFF_RET + RET_H * 1024
TWO_PI = 2.0 * math.pi
CW1 = 6.28125
CW2 = TWO_PI - CW1


class Res:
    __slots__ = ("w", "r")

    def __init__(self):
        self.w = None
        self.r = {}


class Tile:
    def __init__(self, t, nres=1):
        self.t = t
        self.res = [Res() for _ in range(nres)]

    @property
    def r(self):
        return self.res[0]

    def __getitem__(self, k):
        return self.t[k]


class Sched:
    ENG = ("pe", "act", "dve", "pool", "sp")
    NDS = 8

    def __init__(self, nc):
        self.nc = nc
        self.ops = {e: [] for e in self.ENG}
        self.cnt = {e: 0 for e in self.ENG}
        self.waited = {e: {} for e in self.ENG}
        self.dcount = {e: 0 for e in self.ENG}
        self.stack = ExitStack()

    def sb(self, name, shape, dtype, nres=1, stack=None):
        t = (stack or self.stack).enter_context(self.nc.sbuf_tensor("sb_" + name, list(shape), dtype))
        return Tile(t, nres)

    def ps(self, name, shape, dtype, nres=1):
        t = self.stack.enter_context(self.nc.psum_tensor("ps_" + name, list(shape), dtype))
        return Tile(t, nres)

    def _collect(self, eng, reads, writes, extra=()):
        deps = {}

        def add(tok):
            if tok is None:
                return
            k, v = tok
            if deps.get(k, 0) < v:
                deps[k] = v

        for r in reads:
            add(r.w)
        for w in writes:
            add(w.w)
            for k, v in w.r.items():
                add((k, v))
        for t in extra:
            add(t)
        waits = []
        for k, v in deps.items():
            if k == eng and eng == "pe":
                continue
            if self.waited[eng].get(k, 0) >= v:
                continue
            self.waited[eng][k] = v
            waits.append((k, v))
        return waits

    def _record(self, tok, reads, writes):
        k, v = tok
        for r in reads:
            if r.r.get(k, 0) < v:
                r.r[k] = v
        for w in writes:
            w.w = tok
            w.r = {}

    def op(self, eng, fn, reads=(), writes=(), inc=True):
        assert inc or eng == "pe"
        waits = self._collect(eng, reads, writes)
        if inc:
            self.cnt[eng] += 1
            tok = (eng, self.cnt[eng])
            incinfo = (eng, 1)
        else:
            tok = (eng, self.cnt[eng] + 1)
            incinfo = None
        self.ops[eng].append((waits, fn, incinfo))
        self._record(tok, reads, writes)
        return tok

    def dma(self, q, fn, reads=(), writes=()):
        n = self.dcount[q]
        self.dcount[q] += 1
        slot, rnd = n % self.NDS, n // self.NDS
        key = ("dma", q, slot)
        extra = [(key, 16 * rnd)] if rnd > 0 else []
        waits = self._collect(q, reads, writes, extra)
        tok = (key, 16 * (rnd + 1))
        self.ops[q].append((waits, fn, (key, 16)))
        self._record(tok, reads, writes)
        return tok

    def wait_all(self, eng, toks):
        waits = self._collect(eng, (), (), toks)
        self.cnt[eng] += 1
        self.ops[eng].append((waits, None, (eng, 1)))

    def emit(self):
        nc = self.nc
        keys = set()
        for e in self.ENG:
            for waits, fn, incinfo in self.ops[e]:
                for k, v in waits:
                    keys.add(k)
                if incinfo:
                    keys.add(incinfo[0])
        sems = {}
        for k in sorted(keys, key=str):
            nm = k if isinstance(k, str) else f"d_{k[1]}_{k[2]}"
            sems[k] = self.stack.enter_context(nc.semaphore("s_" + nm))

        def run(name, e):
            for waits, fn, incinfo in self.ops[name]:
                for k, v in waits:
                    e.wait_ge(sems[k], v)
                ins = e.nop() if fn is None else fn(e)
                if incinfo:
                    ins.then_inc(sems[incinfo[0]], incinfo[1])

        with nc.Block() as block:
            @block.tensor
            def _(e):
                run("pe", e)

            @block.scalar
            def _(e):
                run("act", e)

            @block.vector
            def _(e):
                run("dve", e)

            @block.gpsimd
            def _(e):
                run("pool", e)

            @block.sync
            def _(e):
                run("sp", e)


def bc(ap, n):
    return ap.unsqueeze(ap.ndim).to_broadcast(list(ap.shape) + [n])


def build(T, debug=False):
    assert T % TT == 0
    NT = T // TT
    NB = T // 128
    nc = bass.Bass("TRN2", target_bir_lowering=False)
    dt_in = lambda name, shape, dt=F32: nc.dram_tensor(name, list(shape), dt, kind="ExternalInput").ap()
    x_d = dt_in("x", [2 * T, D])
    pos_d = dt_in("pos", [1, 2 * T], I32)
    win_d = dt_in("w_in", [D, 11264])
    lbl_d = dt_in("lbl", [2, D])
    hgg_d = dt_in("hgg", [1, D])
    retg_d = dt_in("retg", [1, D])
    wbh_d = dt_in("wbh", [D, D])
    wbr_d = dt_in("wbr", [D, D])
    wo_d = dt_in("wo", [D, D])
    l1g_d = dt_in("l1g", [1, D])
    l1b_d = dt_in("l1b", [1, D])
    l2g_d = dt_in("l2g", [1, D])
    l2b_d = dt_in("l2b", [1, D])
    wgr_d = dt_in("wgr", [D, 36])
    bgr_d = dt_in("bgr", [1, 36])
    w1_d = dt_in("w1", [NEXP, D, DEXP])
    w3_d = dt_in("w3", [NEXP, D, DEXP])
    w2_d = dt_in("w2", [NEXP, DEXP, D])
    invf_d = dt_in("invf", [128, 1])
    rexpo_d = dt_in("rexpo", [128, 10 * 128])
    out_d = nc.dram_tensor("out", [T, D], F32, kind="ExternalOutput").ap()
    dk = "ExternalOutput" if debug else "Internal"
    xTd = nc.dram_tensor("xTd", [2 * NT, 128, NKC * TT], BF16, kind="Internal").ap()
    csd = nc.dram_tensor("csd", [2 * NT, 128, 2 * TT], F32, kind="Internal").ap()
    oS = nc.dram_tensor("oS", [2 * D, T], BF16, kind=dk).ap()
    x1f = nc.dram_tensor("x1f", [T, D], F32, kind=dk).ap()
    x1Td = nc.dram_tensor("x1Td", [NT, 128, NKC * TT], BF16, kind="Internal").ap()
    NBLK = (2 * T) // 128 + NEXP
    x1bd = nc.dram_tensor("x1bd", [T, D], BF16, kind="Internal").ap()
    xs_d = nc.dram_tensor("xs_d", [NBLK * 128, D], BF16, kind="Internal").ap()
    ys_d = nc.dram_tensor("ys_d", [NBLK * 128, D], F32, kind="Internal").ap()
    WS = nc.dram_tensor("WS", [NEXP * 128, 12288], BF16, kind="Internal").ap()

    S = Sched(nc)
    op, dma = S.op, S.dma

    pmm = [S.ps(f"pmm{i}", [128, 512], F32) for i in range(3)]
    ptr = [S.ps(f"ptr{i}", [128, NKC, 128], BF16) for i in range(2)]
    pat = S.ps("pat", [128, 512], F32, nres=4)
    po = S.ps("po", [128, 512], F32, nres=2)
    psS = S.ps("psS", [128, 512], F32, nres=4)
    pat.res = [pat.res[0]] * 4
    po.res = [po.res[0]] * 2
    psS.res = [psS.res[0]] * 4
    cnt = {"mm": 0, "tr": 0}

    mmbanks = {"l": pmm}

    def next_pmm():
        cnt["mm"] += 1
        l = mmbanks["l"]
        return l[cnt["mm"] % len(l)]

    def next_ptr():
        cnt["tr"] += 1
        return ptr[cnt["tr"] % 2]

    ident = S.sb("ident", [128, 128], BF16)
    maskF = S.sb("maskF", [128, 128], F32)
    maskB = S.sb("maskB", [128, 128], F32)
    ones = S.sb("ones", [128, 512], F32)
    onesb = S.sb("onesb", [128, 128], BF16)
    lbT = S.sb("lbT", [128, 4, HG_H], F32)
    lraw = S.sb("lraw", [128, 2, HG_H], F32)
    hggT = S.sb("hggT", [128, HG_H], F32)
    retgT = S.sb("retgT", [128, 8], F32)
    invf = S.sb("invf", [128, 1], F32)
    epsr = S.sb("epsr", [128, 2], F32)

    op("pool", lambda e: e.memset(ident[:], 1.0), writes=[ident.r])
    op("pool", lambda e: e.affine_select(out=ident[:], in_=ident[:], pattern=[[-1, 128]], compare_op=ALU.is_equal,
                                         fill=0.0, base=0, channel_multiplier=1), reads=[ident.r], writes=[ident.r])
    op("pool", lambda e: e.memset(maskF[:], 1.0), writes=[maskF.r])
    op("pool", lambda e: e.affine_select(out=maskF[:], in_=maskF[:], pattern=[[1, 128]], compare_op=ALU.is_ge,
                                         fill=0.0, base=0, channel_multiplier=-1), reads=[maskF.r], writes=[maskF.r])
    op("pool", lambda e: e.memset(maskB[:], 1.0), writes=[maskB.r])
    op("pool", lambda e: e.affine_select(out=maskB[:], in_=maskB[:], pattern=[[-1, 128]], compare_op=ALU.is_ge,
                                         fill=0.0, base=0, channel_multiplier=1), reads=[maskB.r], writes=[maskB.r])
    op("dve", lambda e: e.memset(ones[:], 1.0), writes=[ones.r])
    op("dve", lambda e: e.memset(onesb[:], 1.0), writes=[onesb.r])
    op("dve", lambda e: e.memset(epsr[:, 0:1], RMS_EPS), writes=[epsr.r])
    op("dve", lambda e: e.memset(epsr[:, 1:2], LN_EPS), writes=[epsr.r])
    dma("sp", lambda e: e.dma_start(out=lraw[:], in_=lbl_d.rearrange("r (h p) -> p r h", p=128), allow_slow_non_contiguous=True), writes=[lraw.r])
    dma("sp", lambda e: e.dma_start(out=hggT[:], in_=hgg_d.rearrange("r (h p) -> p (r h)", p=128), allow_slow_non_contiguous=True), writes=[hggT.r])
    dma("sp", lambda e: e.dma_start(out=retgT[:], in_=retg_d.rearrange("r (h p) -> p (r h)", p=128), allow_slow_non_contiguous=True), writes=[retgT.r])
    dma("sp", lambda e: e.dma_start(out=invf[:], in_=invf_d), writes=[invf.r])
    op("dve", lambda e: e.tensor_tensor(out=lbT[:, 3, :], in0=lraw[:, 1, :], in1=lraw[:, 0, :], op=ALU.subtract),
       reads=[lraw.r], writes=[lbT.r])
    op("act", lambda e: e.activation(out=lbT[:, 0, :], in_=lbT[:, 3, :], func=AF.Sigmoid), reads=[lbT.r], writes=[lbT.r])
    op("dve", lambda e: e.tensor_scalar(out=lbT[:, 1, :], in0=lbT[:, 0, :], scalar1=-1.0, scalar2=1.0, op0=ALU.mult, op1=ALU.add),
       reads=[lbT.r], writes=[lbT.r])
    op("dve", lambda e: e.tensor_scalar(out=lbT[:, 2, :], in0=lbT[:, 1, :], scalar1=-1.0, scalar2=None, op0=ALU.mult),
       reads=[lbT.r], writes=[lbT.r])

    with ExitStack() as st:
        xb = [S.sb(f"xb{i}", [128, 4, D], BF16, stack=st) for i in range(2)]
        xf = [S.sb(f"xf{i}", [128, 4, D], F32, stack=st) for i in range(2)]

        def xload(jj):
            xff = xf[jj % 2]
            dma("sp", lambda e: e.dma_start(out=xff[:], in_=x_d[jj * TT:(jj + 1) * TT, :].rearrange("(b p) d -> p b d", p=128)), writes=[xff.r])

        xload(0)
        xTt = [S.sb(f"xTt{i}", [128, NKC, TT], BF16, stack=st) for i in range(2)]
        posi = [S.sb(f"posi{i}", [128, TT], I32, stack=st) for i in range(2)]
        ang = [S.sb(f"ang{i}", [128, TT], F32, stack=st) for i in range(2)]
        kf = [S.sb(f"kf{i}", [128, TT], F32, stack=st) for i in range(2)]
        ki = [S.sb(f"ki{i}", [128, TT], I32, stack=st) for i in range(2)]
        rr = [S.sb(f"rr{i}", [128, TT], F32, stack=st) for i in range(2)]
        yy = [S.sb(f"yy{i}", [128, TT], F32, stack=st) for i in range(2)]
        mm_ = [S.sb(f"mm{i}", [128, TT], F32, stack=st) for i in range(2)]
        cs = [S.sb(f"cs{i}", [128, 2, TT], F32, stack=st) for i in range(2)]
        nblk = 0
        for j in range(2 * NT):
            xt = xTt[j % 2]
            xbb = xb[j % 2]
            xff = xf[j % 2]
            if j + 1 < 2 * NT:
                xload(j + 1)
            for b in range(4):
                if b < 3:
                    op("act", lambda e, xff=xff, xbb=xbb, b=b: e.copy(out=xbb[:, b, :], in_=xff[:, b, :]), reads=[xff.r], writes=[xbb.r])
                else:
                    op("dve", lambda e, xff=xff, xbb=xbb, b=b: e.tensor_copy(out=xbb[:, b, :], in_=xff[:, b, :]), reads=[xff.r], writes=[xbb.r])
            for b in range(4):
                pt = next_ptr()
                for kc in range(NKC):
                    op("pe", lambda e, pt=pt, xbb=xbb, kc=kc, b=b: e.transpose(pt[:, kc, :], xbb[:, b, kc * 128:(kc + 1) * 128], ident[:]),
                       reads=[xbb.r, ident.r], writes=[pt.r], inc=(kc == NKC - 1))
                eng = "act" if b % 2 == 0 else "dve"
                if eng == "act":
                    op("act", lambda e, pt=pt, xt=xt, b=b: e.copy(out=xt[:, :, b * 128:(b + 1) * 128], in_=pt[:]),
                       reads=[pt.r], writes=[xt.r])
                else:
                    op("dve", lambda e, pt=pt, xt=xt, b=b: e.tensor_copy(out=xt[:, :, b * 128:(b + 1) * 128], in_=pt[:]),
                       reads=[pt.r], writes=[xt.r])
            dma("sp", lambda e, xt=xt, j=j: e.dma_start(out=xTd[j].rearrange("p (k t) -> p k t", t=TT), in_=xt[:]), reads=[xt.r])
            s = j % 2
            pi_, an, kf_, ki_, r_, y_, m_, c_ = posi[s], ang[s], kf[s], ki[s], rr[s], yy[s], mm_[s], cs[s]
            dma("sp", lambda e, pi_=pi_, j=j: e.dma_start(out=pi_[:], in_=pos_d[0:1, j * TT:(j + 1) * TT].partition_broadcast(128)),
                writes=[pi_.r])
            op("dve", lambda e, pi_=pi_, an=an: e.tensor_copy(out=an[:], in_=pi_[:]), reads=[pi_.r], writes=[an.r])
            op("dve", lambda e, an=an: e.tensor_scalar(out=an[:], in0=an[:], scalar1=invf[:, 0:1], scalar2=None, op0=ALU.mult),
               reads=[an.r, invf.r], writes=[an.r])
            op("dve", lambda e, an=an, ki_=ki_: e.tensor_scalar(out=ki_[:], in0=an[:], scalar1=1.0 / TWO_PI, scalar2=None, op0=ALU.mult),
               reads=[an.r], writes=[ki_.r])
            op("dve", lambda e, kf_=kf_, ki_=ki_: e.tensor_copy(out=kf_[:], in_=ki_[:]), reads=[ki_.r], writes=[kf_.r])
            op("dve", lambda e, r_=r_, kf_=kf_, an=an: e.scalar_tensor_tensor(out=r_[:], in0=kf_[:], scalar=-CW1, in1=an[:], op0=ALU.mult, op1=ALU.add),
               reads=[kf_.r, an.r], writes=[r_.r])
            op("dve", lambda e, r_=r_, kf_=kf_: e.scalar_tensor_tensor(out=r_[:], in0=kf_[:], scalar=-CW2, in1=r_[:], op0=ALU.mult, op1=ALU.add),
               reads=[kf_.r, r_.r], writes=[r_.r])
            for which, shift in ((1, 0.0), (0, math.pi / 2)):
                op("pool", lambda e, y_=y_, r_=r_, shift=shift: e.tensor_scalar(out=y_[:], in0=r_[:], scalar1=shift, scalar2=None, op0=ALU.add),
                   reads=[r_.r], writes=[y_.r])
                op("dve", lambda e, y_=y_, m_=m_: e.tensor_scalar(out=m_[:], in0=y_[:], scalar1=math.pi, scalar2=-TWO_PI, op0=ALU.is_gt, op1=ALU.mult),
                   reads=[y_.r], writes=[m_.r])
                op("dve", lambda e, y_=y_, m_=m_: e.tensor_tensor(out=y_[:], in0=y_[:], in1=m_[:], op=ALU.add), reads=[y_.r, m_.r], writes=[y_.r])
                op("dve", lambda e, y_=y_, m_=m_: e.tensor_scalar(out=m_[:], in0=y_[:], scalar1=-math.pi, scalar2=TWO_PI, op0=ALU.is_lt, op1=ALU.mult),
                   reads=[y_.r], writes=[m_.r])
                op("dve", lambda e, y_=y_, m_=m_: e.tensor_tensor(out=y_[:], in0=y_[:], in1=m_[:], op=ALU.add), reads=[y_.r, m_.r], writes=[y_.r])
                op("dve", lambda e, y_=y_: e.tensor_scalar(out=y_[:], in0=y_[:], scalar1=-3.1415925, scalar2=3.1415925, op0=ALU.max, op1=ALU.min),
                   reads=[y_.r], writes=[y_.r])
                op("act", lambda e, y_=y_, c_=c_, which=which: e.activation(out=c_[:, which, :], in_=y_[:], func=AF.Sin), reads=[y_.r], writes=[c_.r])
            dma("sp", lambda e, c_=c_, j=j: e.dma_start(out=csd[j].rearrange("p (k t) -> p k t", t=TT), in_=c_[:]), reads=[c_.r])
    def fence(queues=("sp",)):
        toks = []
        for q in queues:
            n = S.dcount[q]
            for sl in range(S.NDS):
                if n > sl:
                    toks.append((("dma", q, sl), 16 * ((n - 1 - ((n - 1 - sl) % S.NDS)) // S.NDS + 1)))
        waits = S._collect("sp", (), (), toks)
        S.cnt["sp"] += 1
        S.ops["sp"].append((waits, None, ("sp", 1)))
        r = Res()
        r.w = ("sp", S.cnt["sp"])
        return r

    def barrier(extra_res):
        toks = [(eng, S.cnt[eng]) for eng in ("pe", "act", "dve", "pool")] + [extra_res.w]
        for eng in S.ENG:
            waits = S._collect(eng, (), (), toks)
            if waits:
                S.cnt[eng] += 1
                S.ops[eng].append((waits, None, (eng, 1)))

    xsync = fence(("sp", "pool"))
    barrier(xsync)

    ph_stack = ExitStack()
    st = ph_stack
    rexpo = S.sb("rexpo", [128, 10, 128], F32, stack=st)
    rtab = S.sb("rtab", [128, 10, 128], F32, stack=st)
    Sinit_h = S.sb("Sinit_h", [128, HG_H, 128], F32, stack=st)
    Sinit_r = S.sb("Sinit_r", [128, RET_H, 2, 256], F32, stack=st)
    dma("sp", lambda e: e.dma_start(out=rexpo[:], in_=rexpo_d.rearrange("p (k i) -> p k i", i=128)), writes=[rexpo.r])
    Wh = [S.sb(f"Wh{i}", [128, NKC, 1024], BF16, stack=st) for i in range(2)]
    xTt = [S.sb(f"xTl{i}", [128, NKC, TT], BF16, stack=st) for i in range(2)]
    cst = [S.sb(f"cst{i}", [128, 2, TT], F32, stack=st) for i in range(1)]
    qS = S.sb("qS", [128, 2, T], BF16, stack=st, nres=NT)
    vS = S.sb("vS", [128, NB, 256], BF16, stack=st, nres=NT)
    oF = S.sb("oF", [128, 2, T], BF16, stack=st, nres=NT)
    Sst = S.sb("Sst", [128, 2, 256], F32, stack=st)
    Sbfs = [S.sb(f"Sbf{i}", [128, 2, 256], BF16, stack=st) for i in range(2)]
    scur = {"i": 0}
    NSET = 2
    tA = [S.sb(f"tA{i}", [128, TT], F32, stack=st) for i in range(NSET)]
    tB = [S.sb(f"tB{i}", [128, TT], F32, stack=st) for i in range(NSET)]
    tC = [S.sb(f"tC{i}", [128, TT], F32, stack=st) for i in range(NSET)]
    tG = [S.sb(f"tG{i}", [128, TT], F32, stack=st) for i in range(NSET)]
    tE = [S.sb(f"tE{i}", [128, TT], F32, stack=st) for i in range(NSET)]
    tF = [S.sb(f"tF{i}", [128, TT], F32, stack=st) for i in range(NSET)]
    tBc = [S.sb(f"tBc{i}", [128, 8], F32, stack=st) for i in range(NSET)]
    tcd = [S.sb(f"tcd{i}", [128, 8], F32, stack=st) for i in range(NSET)]
    tcb = [S.sb(f"tcb{i}", [128, 4], F32, stack=st) for i in range(NSET)]
    QT = [S.sb(f"QT{i}", [128, 2, TT], BF16, stack=st) for i in range(NSET)]
    QM = [S.sb(f"QM{i}", [128, 2, TT], BF16, stack=st) for i in range(NSET)]
    KA = [S.sb(f"KA{i}", [128, 2, TT], BF16, stack=st) for i in range(NSET)]
    KB = [S.sb(f"KB{i}", [128, 2, TT], BF16, stack=st) for i in range(NSET)]
    KM = [S.sb(f"KM{i}", [128, 2, TT], BF16, stack=st) for i in range(NSET)]
    vT = [S.sb(f"vT{i}", [128, 4, 256], BF16, stack=st) for i in range(NSET)]
    krot = [S.sb(f"krot{i}", [128, 2, TT], BF16, stack=st) for i in range(NSET)]
    ATm = [S.sb(f"ATm{i}", [128, 128], BF16, stack=st) for i in range(4)]
    kTs = [S.sb(f"kTs{i}", [128, 2, 128], BF16, stack=st) for i in range(4)]
    osum = [S.sb(f"osum{i}", [128, 2, TT], F32, stack=st) for i in range(2)]
    sq = [S.sb(f"sq{i}", [128, 2, TT], BF16, stack=st) for i in range(1)] * 2
    sg = [S.sb(f"sg{i}", [128, 2, TT], BF16, stack=st) for i in range(2)]
    rstd = [S.sb(f"rstd{i}", [128, TT], F32, stack=st) for i in range(1)] * 2
    oOut = [S.sb(f"oOut{i}", [128, 2, TT], BF16, stack=st) for i in range(2)]
    visit = {"n": 0, "blk": 0, "wh": 0, "xl": 0}

    wsched = []
    for _ph in (0, 1):
        wsched += [(h * 640, 640) for h in range(HG_H)] + [(OFF_RET + hr * 1024, 1024) for hr in range(RET_H)]
    wstate = {"issued": 0, "cur": -1}

    def _issue_w():
        i = wstate["issued"]
        if i >= len(wsched):
            return
        c0, ncols = wsched[i]
        W = Wh[i % 2]
        dma("pool", lambda e: e.dma_start(out=W[:, :, 0:ncols], in_=win_d[:, c0:c0 + ncols].rearrange("(k p) c -> p k c", p=128)),
            writes=[W.r])
        wstate["issued"] += 1

    def load_w(c0, ncols):
        wstate["cur"] += 1
        i = wstate["cur"]
        assert wsched[i] == (c0, ncols)
        while wstate["issued"] <= i:
            _issue_w()
        return Wh[i % 2]

    def prefetch_w():
        if wstate["issued"] <= wstate["cur"] + 1:
            _issue_w()

    pc_jobs = []
    for ex in range(NEXP):
        pc_jobs.append((w1_d[ex].rearrange("(k p) f -> p k f", p=128), ex, 0, 4096, DEXP))
        pc_jobs.append((w3_d[ex].rearrange("(k p) f -> p k f", p=128), ex, 4096, 4096, DEXP))
        pc_jobs.append((w2_d[ex].rearrange("(k p) f -> p k f", p=128), ex, 8192, 4096, D))
    pc_state = {"i": 0}

    def precast_step(n=1):
        for _ in range(n):
            i = pc_state["i"]
            if i >= len(pc_jobs):
                return
            src, ex, c0, w, inner = pc_jobs[i]
            pc_state["i"] += 1
            dma("pool", lambda e, src=src, ex=ex, c0=c0, w=w, inner=inner: e.dma_start(
                out=WS[ex * 128:(ex + 1) * 128, c0:c0 + w].rearrange("p (k f) -> p k f", f=inner), in_=src))

    xpend = {}

    def prefetch_xT(gt):
        if gt is None or gt in xpend:
            return
        visit["xl"] += 1
        X = xTt[visit["xl"] % 2]
        dma("sp", lambda e: e.dma_start(out=X[:], in_=xTd[gt].rearrange("p (k t) -> p k t", t=TT)), reads=[xsync], writes=[X.r])
        xpend[gt] = X

    def load_xT(gt, nxt=None):
        prefetch_xT(gt)
        X = xpend.pop(gt)
        prefetch_xT(nxt)
        precast_step(1)
        return X

    def load_cs(gt):
        C = cst[0]
        dma("sp", lambda e: e.dma_start(out=C[:], in_=csd[gt].rearrange("p (k t) -> p k t", t=TT)), reads=[xsync], writes=[C.r])
        return C

    def inproj_fm(W, c0, X):
        pm = next_pmm()
        for kc in range(NKC):
            op("pe", lambda e, kc=kc: e.matmul(pm[:, :], lhsT=W[:, kc, c0:c0 + 128], rhs=X[:, kc, :], start=(kc == 0), stop=(kc == NKC - 1)),
               reads=[W.r, X.r], writes=[pm.r], inc=(kc == NKC - 1))
        return pm

    def inproj_tm(W, c0, dv, X, dst, dst_res, dst_b0):
        nb_per = 512 // dv
        for g in range(4 // nb_per):
            pm = next_pmm()
            for bb in range(nb_per):
                b = g * nb_per + bb
                for kc in range(NKC):
                    op("pe", lambda e, kc=kc, b=b, bb=bb, pm=pm: e.matmul(pm[:, bb * dv:(bb + 1) * dv], lhsT=X[:, kc, b * 128:(b + 1) * 128],
                                                             rhs=W[:, kc, c0:c0 + dv], start=(kc == 0), stop=(kc == NKC - 1)),
                       reads=[W.r, X.r], writes=[pm.r], inc=(kc == NKC - 1 and bb == nb_per - 1))
            b0 = dst_b0 + g * nb_per
            op("act", lambda e, pm=pm, b0=b0: e.copy(out=dst[:, b0:b0 + nb_per, 0:dv], in_=pm[:, :].rearrange("p (b v) -> p b v", v=dv)),
               reads=[pm.r], writes=[dst_res])

    def v4(ap):
        return ap.rearrange("p (b c i) -> p b c i", c=2, i=64)

    def v8(ap):
        return ap.rearrange("p (c i) -> p c i", i=64)

    def hg_prep(h, pm_f, qsrc, qres, s, dirn, prescan):
        A, B, C, G, E, Fh, Bc, cd, cb = tA[s], tB[s], tC[s], tG[s], tE[s], tF[s], tBc[s], tcd[s], tcb[s]
        first = 0 if dirn == 0 else 1
        second = 1 - first
        lb_h, oml_h, noml_h = lbT[:, 0, h:h + 1], lbT[:, 1, h:h + 1], lbT[:, 2, h:h + 1]
        op("act", lambda e: e.activation(out=B[:], in_=pm_f[:, :], func=AF.Exp, scale=-1.0), reads=[pm_f.r], writes=[B.r])
        yield
        op("act", lambda e: e.activation(out=B[:], in_=B[:], func=AF.Ln, bias=1.0), reads=[B.r], writes=[B.r])
        op("act", lambda e: e.activation(out=A[:], in_=B[:], func=AF.Exp, scale=-1.0), reads=[B.r], writes=[A.r])
        yield
        op("act", lambda e: e.activation(out=B[:], in_=A[:], func=AF.Ln, scale=oml_h, bias=lb_h), reads=[A.r, lbT.r], writes=[B.r])
        op("dve", lambda e: e.tensor_scalar(out=C[:], in0=A[:], scalar1=noml_h, scalar2=oml_h, op0=ALU.mult, op1=ALU.add),
           reads=[A.r, lbT.r], writes=[C.r])
        yield
        op("dve", lambda e: e.tensor_tensor_scan(out=G[:], data0=ones[:], data1=B[:], initial=0.0, op0=ALU.mult, op1=ALU.add),
           reads=[ones.r, B.r], writes=[G.r])
        yield
        if dirn == 0:
            op("pool", lambda e: e.memset(Bc[:, 0:1], 0.0), writes=[Bc.r])
            op("pool", lambda e: e.tensor_copy(out=Bc[:, 1:8], in_=G[:, 63:511:64]), reads=[G.r], writes=[Bc.r])
            op("dve", lambda e: e.tensor_tensor(out=v8(A[:]), in0=v8(G[:]), in1=bc(Bc[:, 0:8], 64), op=ALU.subtract),
               reads=[G.r, Bc.r], writes=[A.r])
        else:
            op("dve", lambda e: e.tensor_tensor(out=A[:], in0=B[:], in1=G[:], op=ALU.subtract), reads=[B.r, G.r], writes=[A.r])
            op("dve", lambda e: e.tensor_tensor(out=v8(A[:]), in0=v8(A[:]), in1=bc(G[:, 63:512:64], 64), op=ALU.add),
               reads=[A.r, G.r], writes=[A.r])
        yield
        op("act", lambda e: e.activation(out=E[:], in_=A[:], func=AF.Exp), reads=[A.r], writes=[E.r])
        op("act", lambda e: e.activation(out=Fh[:], in_=A[:], func=AF.Exp, scale=-1.0), reads=[A.r], writes=[Fh.r])
        yield
        op("dve", lambda e: e.tensor_tensor(out=C[:], in0=C[:], in1=Fh[:], op=ALU.mult), reads=[C.r, Fh.r], writes=[C.r])
        far = 63 if dirn == 0 else 0
        op("pool", lambda e: e.tensor_copy(out=cd[:, 0:8], in_=E[:, far:512:64]), reads=[E.r], writes=[cd.r])
        cd4 = cd[:, 0:8].rearrange("p (b c) -> p b c", c=2)
        op("pool", lambda e: e.tensor_tensor(out=cb[:, 0:4], in0=cd4[:, :, 0], in1=cd4[:, :, 1], op=ALU.mult), reads=[cd.r], writes=[cb.r])
        yield
        op("dve", lambda e: e.tensor_tensor(out=v8(Fh[:]), in0=v8(C[:]), in1=bc(cd[:, 0:8], 64), op=ALU.mult),
           reads=[C.r, cd.r], writes=[Fh.r])
        yield
        km = KM[s]
        F4, C4, km4 = v4(Fh[:]), v4(C[:]), v4(km[:, 0, :])
        op("pool", lambda e: e.tensor_tensor(out=km4[:, :, first, :], in0=F4[:, :, first, :], in1=bc(cd4[:, :, second], 64), op=ALU.mult),
           reads=[Fh.r, cd.r], writes=[km.r])
        op("act", lambda e: e.copy(out=km4[:, :, second, :], in_=F4[:, :, second, :]), reads=[Fh.r], writes=[km.r])
        yield
        if prescan:
            return
        qt, qm, ka, kb = QT[s], QM[s], KA[s], KB[s]
        op("dve", lambda e: e.tensor_tensor(out=qt[:, 0, :], in0=qsrc, in1=E[:], op=ALU.mult), reads=[qres, E.r], writes=[qt.r])
        op("act", lambda e: e.copy(out=ka[:, 0, :], in_=C[:]), reads=[C.r], writes=[ka.r])
        yield
        kb4, qm4, qt4 = v4(kb[:, 0, :]), v4(qm[:, 0, :]), v4(qt[:, 0, :])
        op("act", lambda e: e.copy(out=kb4[:, :, first, :], in_=F4[:, :, first, :]), reads=[Fh.r], writes=[kb.r])
        op("act", lambda e: e.copy(out=kb4[:, :, second, :], in_=C4[:, :, second, :]), reads=[C.r], writes=[kb.r])
        yield
        op("act", lambda e: e.copy(out=qm4[:, :, first, :], in_=qt4[:, :, first, :]), reads=[qt.r], writes=[qm.r])
        op("pool", lambda e: e.tensor_tensor(out=qm4[:, :, second, :], in0=qt4[:, :, second, :], in1=bc(cd4[:, :, first], 64), op=ALU.mult),
           reads=[qt.r, cd.r], writes=[qm.r])
        yield

    def silu_from_psum(pm, tmp, dst_ap, dst_res):
        op("act", lambda e: e.activation(out=tmp[:], in_=pm[:, :], func=AF.Exp, scale=-1.0), reads=[pm.r], writes=[tmp.r])
        op("act", lambda e: e.activation(out=tmp[:], in_=tmp[:], func=AF.Ln, bias=1.0), reads=[tmp.r], writes=[tmp.r])
        op("act", lambda e: e.activation(out=tmp[:], in_=tmp[:], func=AF.Exp, scale=-1.0), reads=[tmp.r], writes=[tmp.r])
        op("dve", lambda e: e.tensor_tensor(out=dst_ap, in0=pm[:, :], in1=tmp[:], op=ALU.mult), reads=[pm.r, tmp.r], writes=[dst_res])

    def step(g, n):
        if g is None:
            return
        for _ in range(n):
            try:
                next(g)
            except StopIteration:
                return

    def exhaust(g):
        if g is None:
            return
        for _ in g:
            pass

    def pipeline(tiles, make_A, do_B):
        tiles = list(tiles)
        ctxs = [dict() for _ in tiles]
        nx = lambda i: tiles[i + 1] if i + 1 < len(tiles) else None
        exhaust(make_A(tiles[0], ctxs[0], nx(0)))
        for i, j in enumerate(tiles):
            g = make_A(tiles[i + 1], ctxs[i + 1], nx(i + 1)) if i + 1 < len(tiles) else None
            do_B(j, ctxs[i], g)
            exhaust(g)

    def ret_tables(hr):
        lg = math.log(1.0 - 2.0 ** (-5.0 - hr))
        for k in range(10):
            kind = k % 5
            bias = math.log(0.0625) if kind >= 2 else 0.0
            op("act", lambda e, k=k, bias=bias: e.activation(out=rtab[:, k, :], in_=rexpo[:, k, :], func=AF.Exp, scale=lg, bias=bias),
               reads=[rexpo.r], writes=[rtab.r])

    def ret_rot(pmA, pmB, C_, dst, dst_res, s):
        t1, t2 = tA[s], tB[s]
        op("dve", lambda e: e.tensor_tensor(out=t1[:], in0=pmA[:, :], in1=C_[:, 0, :], op=ALU.mult), reads=[pmA.r, C_.r], writes=[t1.r])
        op("dve", lambda e: e.tensor_tensor(out=t2[:], in0=pmB[:, :], in1=C_[:, 1, :], op=ALU.mult), reads=[pmB.r, C_.r], writes=[t2.r])
        op("pool", lambda e: e.tensor_tensor(out=dst[0], in0=t1[:], in1=t2[:], op=ALU.subtract), reads=[t1.r, t2.r], writes=[dst_res])
        t3, t4 = tC[s], tG[s]
        op("dve", lambda e: e.tensor_tensor(out=t3[:], in0=pmA[:, :], in1=C_[:, 1, :], op=ALU.mult), reads=[pmA.r, C_.r], writes=[t3.r])
        op("dve", lambda e: e.tensor_tensor(out=t4[:], in0=pmB[:, :], in1=C_[:, 0, :], op=ALU.mult), reads=[pmB.r, C_.r], writes=[t4.r])
        op("pool", lambda e: e.tensor_tensor(out=dst[1], in0=t3[:], in1=t4[:], op=ALU.add), reads=[t3.r, t4.r], writes=[dst_res])

    def ret_mix(s, dirn, qsrc, qres, ksrc, kres, prescan):
        base = 5 * dirn

        def tb(kind):
            return rtab[:, base + kind, :].unsqueeze(1).to_broadcast([128, 4, 128])

        def b4(ap):
            return ap.rearrange("p (b i) -> p b i", i=128)

        for kc in range(2):
            eng = "dve" if kc == 0 else "pool"
            op(eng, lambda e, kc=kc: e.tensor_tensor(out=b4(KM[s][:, kc, :]), in0=b4(ksrc[kc]), in1=tb(4), op=ALU.mult),
               reads=[kres, rtab.r], writes=[KM[s].r])
            yield
            if prescan:
                continue
            op("dve", lambda e, kc=kc: e.tensor_tensor(out=b4(QT[s][:, kc, :]), in0=b4(qsrc[kc]), in1=tb(0), op=ALU.mult),
               reads=[qres, rtab.r], writes=[QT[s].r])
            op("pool", lambda e, kc=kc: e.tensor_tensor(out=b4(QM[s][:, kc, :]), in0=b4(qsrc[kc]), in1=tb(1), op=ALU.mult),
               reads=[qres, rtab.r], writes=[QM[s].r])
            op("dve", lambda e, kc=kc: e.tensor_tensor(out=b4(KA[s][:, kc, :]), in0=b4(ksrc[kc]), in1=tb(2), op=ALU.mult),
               reads=[kres, rtab.r], writes=[KA[s].r])
            op("pool", lambda e, kc=kc: e.tensor_tensor(out=b4(KB[s][:, kc, :]), in0=b4(ksrc[kc]), in1=tb(3), op=ALU.mult),
               reads=[kres, rtab.r], writes=[KB[s].r])
            yield

    def scan_block(s, b, dirn, nkc, nmc, vap, vres, cdb, mode, oF_slice=None, oF_res=None, osum_t=None):
        dv = nmc * 128
        bl = slice(b * 128, (b + 1) * 128)
        visit["blk"] += 1
        nb = visit["blk"]
        cur = scur["i"]
        Sbf, Sn = Sbfs[cur], Sbfs[1 - cur]
        first = 0 if dirn == 0 else 1
        pt = next_ptr()
        for kc in range(nkc):
            op("pe", lambda e, kc=kc: e.transpose(pt[:, kc, :], KM[s][:, kc, bl], ident[:]), reads=[KM[s].r, ident.r], writes=[pt.r],
               inc=(kc == nkc - 1))
        kt = kTs[nb % 4]
        op("act", lambda e: e.copy(out=kt[:, 0:nkc, :], in_=pt[:, 0:nkc, :]), reads=[pt.r], writes=[kt.r])
        spv = [psS[:, 0:256], psS[:, 256:512]]
        sprl = list(psS.res)
        for kc in range(nkc):
            op("pe", lambda e, kc=kc: e.matmul(spv[kc], lhsT=kt[:, kc, :], rhs=vap, start=True, stop=True),
               reads=[kt.r, vres], writes=sprl, inc=(kc == nkc - 1))
        for kc in range(nkc):
            sc = cdb[kc] if isinstance(cdb, (list, tuple)) else cdb
            op("dve", lambda e, kc=kc, sc=sc: e.scalar_tensor_tensor(out=Sst[:, kc, 0:dv], in0=Sst[:, kc, 0:dv], scalar=sc, in1=spv[kc],
                                                                   op0=ALU.mult, op1=ALU.add),
               reads=[Sst.r] + sprl + ([tcb[s].r] if not isinstance(sc, float) else []), writes=[Sst.r])
        op("act", lambda e: e.copy(out=Sn[:, 0:nkc, 0:dv], in_=Sst[:, 0:nkc, 0:dv]), reads=[Sst.r], writes=[Sn.r])
        scur["i"] = 1 - cur
        if mode == "pre":
            return
        c1 = slice(first * 64, first * 64 + 64)
        c2 = slice((1 - first) * 64, (1 - first) * 64 + 64)
        asl = nb % 4
        at = pat[:, asl * 128:(asl + 1) * 128]
        atr = pat.res[asl]
        for (cols, Ksrc) in ((c1, KA[s]), (c2, KB[s])):
            for kc in range(nkc):
                op("pe", lambda e, cols=cols, Ksrc=Ksrc, kc=kc: e.matmul(
                    at[:, cols], lhsT=Ksrc[:, kc, bl], rhs=QT[s][:, kc, b * 128 + cols.start:b * 128 + cols.stop],
                    start=(kc == 0), stop=(kc == nkc - 1)),
                   reads=[Ksrc.r, QT[s].r], writes=[atr], inc=(kc == nkc - 1))
        am = ATm[nb % 4]
        mk = maskF if dirn == 0 else maskB
        op("dve", lambda e: e.tensor_tensor(out=am[:], in0=at, in1=mk[:], op=ALU.mult), reads=[atr, mk.r], writes=[am.r])
        osl = nb % 2
        ot = po[:, osl * 256:osl * 256 + dv]
        otr = po.res[osl]
        for mc in range(nmc):
            op("pe", lambda e, mc=mc: e.matmul(ot[:, mc * 128:(mc + 1) * 128], lhsT=vap[:, mc * 128:(mc + 1) * 128], rhs=am[:],
                                               start=True, stop=False), reads=[vres, am.r], writes=[otr], inc=False)
            for kc in range(nkc):
                op("pe", lambda e, mc=mc, kc=kc: e.matmul(ot[:, mc * 128:(mc + 1) * 128], lhsT=Sbf[:, kc, mc * 128:(mc + 1) * 128],
                                                          rhs=QM[s][:, kc, bl], start=False, stop=(kc == nkc - 1)),
                   reads=[Sbf.r, QM[s].r], writes=[otr], inc=(kc == nkc - 1 and mc == nmc - 1))
        otv = ot.rearrange("p (m t) -> p m t", t=128)
        if mode == "F":
            op("act", lambda e: e.copy(out=oF_slice, in_=otv), reads=[otr], writes=[oF_res])
        else:
            op("dve", lambda e: e.tensor_tensor(out=osum_t[:, 0:nmc, bl], in0=otv, in1=oF_slice, op=ALU.add),
               reads=[otr, oF_res], writes=[osum_t.r])

    def post_tile(j, nmc, gain_ap, orow0):
        u = j % 2
        os_, sq_, sg_, rs_, oo_ = osum[u], sq[u], sg[u], rstd[u], oOut[u]
        dv = nmc * 128
        op("act", lambda e: e.activation(out=sq_[:, 0:nmc, :], in_=os_[:, 0:nmc, :], func=AF.Square), reads=[os_.r], writes=[sq_.r])
        pm = next_pmm()
        for mc in range(nmc):
            op("pe", lambda e, mc=mc: e.matmul(pm[:, :], lhsT=onesb[:], rhs=sq_[:, mc, :], start=(mc == 0), stop=(mc == nmc - 1)),
               reads=[onesb.r, sq_.r], writes=[pm.r], inc=(mc == nmc - 1))
        op("act", lambda e: e.activation(out=rs_[:], in_=pm[:, :], func=AF.Ln, scale=1.0 / dv, bias=epsr[:, 0:1]),
           reads=[pm.r, epsr.r], writes=[rs_.r])
        op("act", lambda e: e.activation(out=rs_[:], in_=rs_[:], func=AF.Exp, scale=-0.5), reads=[rs_.r], writes=[rs_.r])
        for mc in range(nmc):
            op("dve", lambda e, mc=mc: e.tensor_tensor(out=os_[:, mc, :], in0=os_[:, mc, :], in1=rs_[:], op=ALU.mult),
               reads=[os_.r, rs_.r], writes=[os_.r])
            op("dve", lambda e, mc=mc: e.scalar_tensor_tensor(out=oo_[:, mc, :], in0=os_[:, mc, :], scalar=gain_ap[mc], in1=sg_[:, mc, :],
                                                             op0=ALU.mult, op1=ALU.mult),
               reads=[os_.r, sg_.r, hggT.r, retgT.r], writes=[oo_.r])
        dma("sp", lambda e: e.dma_start(out=oS[orow0:orow0 + dv, j * TT:(j + 1) * TT].rearrange("(m p) t -> p m t", p=128),
                                        in_=oo_[:, 0:nmc, :]), reads=[oo_.r])

    def init_state(src_ap, nkc, dv, src_res=None):
        if src_ap is None:
            op("dve", lambda e: e.memset(Sst[:, 0:nkc, 0:dv], 0.0), writes=[Sst.r])
        else:
            op("dve", lambda e: e.tensor_copy(out=Sst[:, 0:nkc, 0:dv], in_=src_ap), reads=[src_res], writes=[Sst.r])
        Sbf = Sbfs[scur["i"]]
        op("act", lambda e: e.copy(out=Sbf[:, 0:nkc, 0:dv], in_=Sst[:, 0:nkc, 0:dv]), reads=[Sst.r], writes=[Sbf.r])

    def scan_tile_hg(s, order, dirn, vaps, vres, cds, mode, g, oF_slices=None, oF_res=None, osum_t=None):
        first = 0 if dirn == 0 else 1
        c1 = slice(first * 64, first * 64 + 64)
        c2 = slice((1 - first) * 64, (1 - first) * 64 + 64)
        mk = maskF if dirn == 0 else maskB
        for b in order:
            bl = slice(b * 128, (b + 1) * 128)
            if mode != "pre":
                at, atr = pat[:, b * 128:(b + 1) * 128], pat.res[b]
                for (cols, Ksrc) in ((c1, KA[s]), (c2, KB[s])):
                    op("pe", lambda e, cols=cols, Ksrc=Ksrc, at=at, bl=bl, b=b: e.matmul(
                        at[:, cols], lhsT=Ksrc[:, 0, bl], rhs=QT[s][:, 0, b * 128 + cols.start:b * 128 + cols.stop], start=True, stop=True),
                       reads=[Ksrc.r, QT[s].r], writes=[atr], inc=True)
                am = ATm[b]
                op("dve", lambda e, am=am, at=at: e.tensor_tensor(out=am[:], in0=at, in1=mk[:], op=ALU.mult), reads=[atr, mk.r], writes=[am.r])
            pt = next_ptr()
            op("pe", lambda e, pt=pt, bl=bl: e.transpose(pt[:, 0, :], KM[s][:, 0, bl], ident[:]), reads=[KM[s].r, ident.r], writes=[pt.r], inc=True)
            kt = kTs[b]
            op("act", lambda e, kt=kt, pt=pt: e.copy(out=kt[:, 0, :], in_=pt[:, 0, :]), reads=[pt.r], writes=[kt.r])
            op("pe", lambda e, kt=kt, b=b: e.matmul(psS[:, b * 128:(b + 1) * 128], lhsT=kt[:, 0, :], rhs=vaps[b], start=True, stop=True),
               reads=[kt.r, vres], writes=[psS.res[b]], inc=True)
            step(g, 2)
        for b in order:
            bl = slice(b * 128, (b + 1) * 128)
            cur = scur["i"]
            Sb, Sn = Sbfs[cur], Sbfs[1 - cur]
            op("dve", lambda e, b=b: e.scalar_tensor_tensor(out=Sst[:, 0, 0:128], in0=Sst[:, 0, 0:128], scalar=cds[b], in1=psS[:, b * 128:(b + 1) * 128],
                                                           op0=ALU.mult, op1=ALU.add),
               reads=[Sst.r, psS.res[b], tcb[s].r], writes=[Sst.r])
            op("act", lambda e, Sn=Sn: e.copy(out=Sn[:, 0, 0:128], in_=Sst[:, 0, 0:128]), reads=[Sst.r], writes=[Sn.r])
            if mode != "pre":
                osl = b % 2
                ot, otr = po[:, osl * 256:osl * 256 + 128], po.res[osl]
                am = ATm[b]
                op("pe", lambda e, ot=ot, am=am, b=b: e.matmul(ot, lhsT=vaps[b], rhs=am[:], start=True, stop=False),
                   reads=[vres, am.r], writes=[otr], inc=False)
                op("pe", lambda e, ot=ot, Sb=Sb, bl=bl: e.matmul(ot, lhsT=Sb[:, 0, 0:128], rhs=QM[s][:, 0, bl], start=False, stop=True),
                   reads=[Sb.r, QM[s].r], writes=[otr], inc=True)
                if mode == "F":
                    op("act", lambda e, ot=ot, b=b: e.copy(out=oF_slices[b], in_=ot), reads=[otr], writes=[oF_res])
                else:
                    op("dve", lambda e, ot=ot, b=b, bl=bl: e.tensor_tensor(out=osum_t[:, 0, bl], in0=ot, in1=oF_slices[b], op=ALU.add),
                       reads=[otr, oF_res], writes=[osum_t.r])
            scur["i"] = 1 - cur
            step(g, 2)

    NSTEP = 4

    def hg_head(ph, h):
        gbase = NT if ph == 0 else 0
        W = load_w(h * 640, 640)
        prefetch_w()

        def A_fwd(j, ctx, nxt):
            X = load_xT(gbase + j, None if nxt is None else gbase + nxt)
            visit["n"] += 1
            s = visit["n"] % NSET
            ctx["s"], ctx["X"] = s, X
            pmq = inproj_fm(W, 0, X)
            yield
            silu_from_psum(pmq, tE[s], qS[:, 0, j * TT:(j + 1) * TT], qS.res[j])
            yield
            pmf = inproj_fm(W, 128, X)
            yield
            inproj_tm(W, 384, 128, X, vS, vS.res[j], j * 4)
            yield
            yield from hg_prep(h, pmf, qS[:, 0, j * TT:(j + 1) * TT], qS.res[j], s, 0, False)

        def B_fwd(j, ctx, g):
            s = ctx["s"]
            scan_tile_hg(s, list(range(4)), 0, [vS[:, j * 4 + b, 0:128] for b in range(4)], vS.res[j],
                         [tcb[s][:, b:b + 1] for b in range(4)], "F", g,
                         oF_slices=[oF[:, 0, j * TT + b * 128:j * TT + (b + 1) * 128] for b in range(4)], oF_res=oF.res[j])

        def A_bwd(j, ctx, nxt):
            X = load_xT(gbase + j, None if nxt is None else gbase + nxt)
            visit["n"] += 1
            s = visit["n"] % NSET
            ctx["s"], ctx["X"] = s, X
            if ph == 1:
                pmg = inproj_fm(W, 512, X)
                silu_from_psum(pmg, tE[s], sg[j % 2][:, 0, :], sg[j % 2].r)
                yield
            pmf = inproj_fm(W, 256, X)
            yield
            if ph == 0:
                inproj_tm(W, 384, 128, X, vT[s], vT[s].r, 0)
                yield
                yield from hg_prep(h, pmf, None, None, s, 1, True)
            else:
                yield from hg_prep(h, pmf, qS[:, 0, j * TT:(j + 1) * TT], qS.res[j], s, 1, False)

        def B_bwd(j, ctx, g):
            s, X = ctx["s"], ctx["X"]
            cds = [tcb[s][:, b:b + 1] for b in range(4)]
            if ph == 0:
                scan_tile_hg(s, [3, 2, 1, 0], 1, [vT[s][:, b, 0:128] for b in range(4)], vT[s].r, cds, "pre", g)
            else:
                scan_tile_hg(s, [3, 2, 1, 0], 1, [vS[:, j * 4 + b, 0:128] for b in range(4)], vS.res[j], cds, "B", g,
                             oF_slices=[oF[:, 0, j * TT + b * 128:j * TT + (b + 1) * 128] for b in range(4)], oF_res=oF.res[j],
                             osum_t=osum[j % 2])
            if ph == 1:
                post_tile(j, 1, [hggT[:, h:h + 1]], h * 128)

        if ph == 1:
            init_state(None, 1, 128)
            pipeline(range(NT), A_fwd, B_fwd)
            init_state(Sinit_h[:, h, :].unsqueeze(1), 1, 128, Sinit_h.r)
        else:
            init_state(None, 1, 128)
        pipeline(reversed(range(NT)), A_bwd, B_bwd)
        if ph == 0:
            op("dve", lambda e: e.tensor_copy(out=Sinit_h[:, h, :], in_=Sst[:, 0, 0:128]), reads=[Sst.r], writes=[Sinit_h.r])

    def ret_head(ph, hr):
        gbase = NT if ph == 0 else 0
        W = load_w(OFF_RET + hr * 1024, 1024)
        prefetch_w()
        ret_tables(hr)
        gam128 = float((1.0 - 2.0 ** (-5.0 - hr)) ** 128)

        def A_fwd(j, ctx, nxt):
            X = load_xT(gbase + j, None if nxt is None else gbase + nxt)
            C_ = load_cs(gbase + j)
            visit["n"] += 1
            s = visit["n"] % NSET
            ctx["s"], ctx["X"] = s, X
            sl = slice(j * TT, (j + 1) * TT)
            pa, pb = inproj_fm(W, 0, X), inproj_fm(W, 128, X)
            yield
            ret_rot(pa, pb, C_, [qS[:, 0, sl], qS[:, 1, sl]], qS.res[j], s)
            yield
            pa, pb = inproj_fm(W, 256, X), inproj_fm(W, 384, X)
            yield
            kr = krot[s]
            ret_rot(pa, pb, C_, [kr[:, 0, :], kr[:, 1, :]], kr.r, s)
            yield
            inproj_tm(W, 512, 256, X, vS, vS.res[j], j * 4)
            yield
            yield from ret_mix(s, 0, [qS[:, 0, sl], qS[:, 1, sl]], qS.res[j], [kr[:, 0, :], kr[:, 1, :]], kr.r, False)

        def B_fwd(j, ctx, g):
            s = ctx["s"]
            for b in range(4):
                scan_block(s, b, 0, 2, 2, vS[:, j * 4 + b, 0:256], vS.res[j], gam128, "F",
                           oF_slice=oF[:, 0:2, j * TT + b * 128:j * TT + (b + 1) * 128], oF_res=oF.res[j])
                step(g, NSTEP)

        def A_bwd(j, ctx, nxt):
            X = load_xT(gbase + j, None if nxt is None else gbase + nxt)
            C_ = load_cs(gbase + j)
            visit["n"] += 1
            s = visit["n"] % NSET
            ctx["s"], ctx["X"] = s, X
            sl = slice(j * TT, (j + 1) * TT)
            if ph == 1:
                for mc in range(2):
                    pmg = inproj_fm(W, 768 + mc * 128, X)
                    silu_from_psum(pmg, tE[s], sg[j % 2][:, mc, :], sg[j % 2].r)
                yield
            pa, pb = inproj_fm(W, 256, X), inproj_fm(W, 384, X)
            yield
            kr = krot[s]
            ret_rot(pa, pb, C_, [kr[:, 0, :], kr[:, 1, :]], kr.r, s)
            yield
            if ph == 0:
                inproj_tm(W, 512, 256, X, vT[s], vT[s].r, 0)
                yield
                yield from ret_mix(s, 1, None, None, [kr[:, 0, :], kr[:, 1, :]], kr.r, True)
            else:
                yield from ret_mix(s, 1, [qS[:, 0, sl], qS[:, 1, sl]], qS.res[j], [kr[:, 0, :], kr[:, 1, :]], kr.r, False)

        def B_bwd(j, ctx, g):
            s, X = ctx["s"], ctx["X"]
            for b in reversed(range(4)):
                if ph == 0:
                    scan_block(s, b, 1, 2, 2, vT[s][:, b, 0:256], vT[s].r, gam128, "pre")
                else:
                    scan_block(s, b, 1, 2, 2, vS[:, j * 4 + b, 0:256], vS.res[j], gam128, "B",
                               oF_slice=oF[:, 0:2, j * TT + b * 128:j * TT + (b + 1) * 128], oF_res=oF.res[j], osum_t=osum[j % 2])
                step(g, NSTEP)
            if ph == 1:
                post_tile(j, 2, [retgT[:, 2 * hr:2 * hr + 1], retgT[:, 2 * hr + 1:2 * hr + 2]], D + hr * 256)

        if ph == 1:
            init_state(None, 2, 256)
            pipeline(range(NT), A_fwd, B_fwd)
            init_state(Sinit_r[:, hr, :, :], 2, 256, Sinit_r.r)
        else:
            init_state(None, 2, 256)
        pipeline(reversed(range(NT)), A_bwd, B_bwd)
        if ph == 0:
            op("dve", lambda e: e.tensor_copy(out=Sinit_r[:, hr, :, :], in_=Sst[:, 0:2, 0:256]), reads=[Sst.r], writes=[Sinit_r.r])

    for ph in (0, 1):
        for h in range(HG_H):
            hg_head(ph, h)
        for hr in range(RET_H):
            ret_head(ph, hr)

    osync = fence(("sp", "pool"))
    barrier(osync)
    ph_stack.close()

    p23 = ExitStack()
    mmbanks["l"] = [pmm[0], pmm[1], pmm[2], pat, po, psS]
    bgr = S.sb("bgr", [128, 36], F32, stack=p23)
    lng = S.sb("lng", [128, 4, D], F32, stack=p23)
    wgtS = S.sb("wgtS", [128, NB, 32], F32, stack=p23, nres=NB)
    p2 = ExitStack()
    st = p2
    Wg = S.sb("Wg", [128, NKC, 2048], BF16, stack=st)
    Wbh = S.sb("Wbh", [128, NKC, D], BF16, stack=st)
    Wbr = S.sb("Wbr", [128, NKC, D], BF16, stack=st)
    Wo = S.sb("Wo", [128, NKC, D], BF16, stack=st)
    Wgr = S.sb("Wgr", [128, NKC, 36], BF16, stack=st)
    dma("pool", lambda e: e.dma_start(out=Wg[:], in_=win_d[:, OFF_G:OFF_G + 2048].rearrange("(k p) c -> p k c", p=128)), writes=[Wg.r])
    for (Wt, src) in ((Wbh, wbh_d), (Wbr, wbr_d), (Wo, wo_d)):
        dma("pool", lambda e, Wt=Wt, src=src: e.dma_start(out=Wt[:], in_=src.rearrange("(k p) c -> p k c", p=128)), writes=[Wt.r])
    dma("pool", lambda e: e.dma_start(out=Wgr[:], in_=wgr_d.rearrange("(k p) c -> p k c", p=128)), writes=[Wgr.r])
    dma("sp", lambda e: e.dma_start(out=bgr[:], in_=bgr_d[0:1, :].partition_broadcast(128)), writes=[bgr.r])
    for i, src in enumerate((l1g_d, l1b_d, l2g_d, l2b_d)):
        dma("sp", lambda e, i=i, src=src: e.dma_start(out=lng[:, i, :], in_=src[0:1, :].partition_broadcast(128)), writes=[lng.r])

    def layer_norm(z, gi, outt, stt, mv, mul_eng="pool"):
        op("dve", lambda e: e.bn_stats(out=stt[:, 0:6], in_=z[:, 0:512]), reads=[z.r], writes=[stt.r])
        op("dve", lambda e: e.bn_stats(out=stt[:, 6:12], in_=z[:, 512:1024]), reads=[z.r], writes=[stt.r])
        op("dve", lambda e: e.bn_aggr(out=mv[:, 0:2], in_=stt[:, 0:12]), reads=[stt.r], writes=[mv.r])
        op("act", lambda e: e.activation(out=mv[:, 2:3], in_=mv[:, 1:2], func=AF.Sqrt, bias=epsr[:, 1:2]), reads=[mv.r, epsr.r], writes=[mv.r])
        op("dve", lambda e: e.reciprocal(out=mv[:, 3:4], in_=mv[:, 2:3]), reads=[mv.r], writes=[mv.r])
        op("dve", lambda e: e.tensor_scalar(out=z[:], in0=z[:], scalar1=mv[:, 0:1], scalar2=mv[:, 3:4], op0=ALU.subtract, op1=ALU.mult),
           reads=[z.r, mv.r], writes=[z.r])
        op(mul_eng, lambda e: e.tensor_tensor(out=z[:], in0=z[:], in1=lng[:, gi, :], op=ALU.mult), reads=[z.r, lng.r], writes=[z.r])
        op("dve", lambda e: e.tensor_tensor(out=outt[:], in0=z[:], in1=lng[:, gi + 1, :], op=ALU.add), reads=[z.r, lng.r], writes=[outt.r])

    with ExitStack() as st2:
        xT2 = [S.sb(f"xT2{i}", [128, NKC, TT], BF16, stack=st2) for i in range(1)] * 2
        oSt = [S.sb(f"oSt{i}", [128, 16, TT], BF16, stack=st2) for i in range(1)] * 2
        xtok = [S.sb(f"xtok{i}", [128, D], F32, stack=st2) for i in range(2)]
        sga = [S.sb(f"sga{i}", [128, TT], F32, stack=st2) for i in range(2)]
        sgb = [S.sb(f"sgb{i}", [128, TT], F32, stack=st2) for i in range(2)]
        mg = [S.sb(f"mg{i}", [128, NKC, TT], BF16, stack=st2) for i in range(1)] * 2
        zt = [S.sb(f"zt{i}", [128, D], F32, stack=st2) for i in range(2)]
        x1t = [S.sb(f"x1t{i}", [128, D], F32, stack=st2) for i in range(2)]
        x1b = [S.sb(f"x1b{i}", [128, D], BF16, stack=st2) for i in range(2)]
        x1T = [S.sb(f"x1T{i}", [128, NKC, TT], BF16, stack=st2) for i in range(1)] * 2
        stt = [S.sb(f"stt{i}", [128, 12], F32, stack=st2) for i in range(2)]
        mv = [S.sb(f"mv{i}", [128, 4], F32, stack=st2) for i in range(2)]
        rt = [S.sb(f"rt{i}", [128, 96], F32, stack=st2) for i in range(2)]
        nblk2 = 0
        nonlocal_state = {"n": 0}
        for j in range(NT):
            X, O_, M_, XT1 = xT2[j % 2], oSt[j % 2], mg[j % 2], x1T[j % 2]
            dma("sp", lambda e, X=X, j=j: e.dma_start(out=X[:], in_=xTd[j].rearrange("p (k t) -> p k t", t=TT)), reads=[xsync], writes=[X.r])
            dma("sp", lambda e, O_=O_, j=j: e.dma_start(out=O_[:], in_=oS[:, j * TT:(j + 1) * TT].rearrange("(c p) t -> p c t", p=128)),
                reads=[osync], writes=[O_.r])
            for dc in range(NKC):
                u = dc % 2
                pga = inproj_fm(Wg, dc * 128, X)
                op("act", lambda e, pga=pga, u=u: e.activation(out=sga[u][:], in_=pga[:, :], func=AF.Sigmoid), reads=[pga.r], writes=[sga[u].r])
                pgb = inproj_fm(Wg, D + dc * 128, X)
                op("act", lambda e, pgb=pgb, u=u: e.activation(out=sgb[u][:], in_=pgb[:, :], func=AF.Sigmoid), reads=[pgb.r], writes=[sgb[u].r])
                pyh = next_pmm()
                for kc in range(NKC):
                    op("pe", lambda e, pyh=pyh, kc=kc, dc=dc: e.matmul(pyh[:, :], lhsT=Wbh[:, kc, dc * 128:(dc + 1) * 128], rhs=O_[:, kc, :],
                                                                      start=(kc == 0), stop=(kc == NKC - 1)),
                       reads=[Wbh.r, O_.r], writes=[pyh.r], inc=(kc == NKC - 1))
                op("dve", lambda e, pyh=pyh, u=u: e.tensor_tensor(out=sga[u][:], in0=pyh[:, :], in1=sga[u][:], op=ALU.mult),
                   reads=[pyh.r, sga[u].r], writes=[sga[u].r])
                pyr = next_pmm()
                for kc in range(NKC):
                    op("pe", lambda e, pyr=pyr, kc=kc, dc=dc: e.matmul(pyr[:, :], lhsT=Wbr[:, kc, dc * 128:(dc + 1) * 128], rhs=O_[:, 8 + kc, :],
                                                                      start=(kc == 0), stop=(kc == NKC - 1)),
                       reads=[Wbr.r, O_.r], writes=[pyr.r], inc=(kc == NKC - 1))
                op("dve", lambda e, pyr=pyr, u=u: e.tensor_tensor(out=sgb[u][:], in0=pyr[:, :], in1=sgb[u][:], op=ALU.mult),
                   reads=[pyr.r, sgb[u].r], writes=[sgb[u].r])
                op("pool", lambda e, u=u, dc=dc: e.tensor_tensor(out=M_[:, dc, :], in0=sga[u][:], in1=sgb[u][:], op=ALU.add),
                   reads=[sga[u].r, sgb[u].r], writes=[M_.r])
            def part1(b):
                nonlocal_state["n"] += 1
                u = nonlocal_state["n"] % 2
                gb = j * 4 + b
                xk, z_, x1_, x1b_ = xtok[u], zt[u], x1t[u], x1b[u]
                dma("sp", lambda e, xk=xk, gb=gb: e.dma_start(out=xk[:], in_=x_d[gb * 128:(gb + 1) * 128, :]), writes=[xk.r])
                for hf in range(2):
                    pm = next_pmm()
                    for kc in range(NKC):
                        op("pe", lambda e, pm=pm, kc=kc, hf=hf, b=b: e.matmul(pm[:, :], lhsT=M_[:, kc, b * 128:(b + 1) * 128],
                                                                             rhs=Wo[:, kc, hf * 512:(hf + 1) * 512], start=(kc == 0), stop=(kc == NKC - 1)),
                           reads=[M_.r, Wo.r], writes=[pm.r], inc=(kc == NKC - 1))
                    op("dve", lambda e, pm=pm, hf=hf, xk=xk, z_=z_: e.scalar_tensor_tensor(out=z_[:, hf * 512:(hf + 1) * 512], in0=xk[:, hf * 512:(hf + 1) * 512],
                                                                                         scalar=ALPHA, in1=pm[:, :], op0=ALU.mult, op1=ALU.add),
                       reads=[pm.r, xk.r], writes=[z_.r])
                layer_norm(z_, 0, x1_, stt[u], mv[u])
                dma("sp", lambda e, x1_=x1_, gb=gb: e.dma_start(out=x1f[gb * 128:(gb + 1) * 128, :], in_=x1_[:]), reads=[x1_.r])
                op("act", lambda e, x1_=x1_, x1b_=x1b_: e.copy(out=x1b_[:], in_=x1_[:]), reads=[x1_.r], writes=[x1b_.r])
                dma("sp", lambda e, x1b_=x1b_, gb=gb: e.dma_start(out=x1bd[gb * 128:(gb + 1) * 128, :], in_=x1b_[:]), reads=[x1b_.r])

                return u, gb

            def part2(b, u, gb):
                x1b_ = x1b[u]
                pt = next_ptr()
                for kc in range(NKC):
                    op("pe", lambda e, pt=pt, kc=kc, x1b_=x1b_: e.transpose(pt[:, kc, :], x1b_[:, kc * 128:(kc + 1) * 128], ident[:]),
                       reads=[x1b_.r, ident.r], writes=[pt.r], inc=(kc == NKC - 1))
                op("act", lambda e, pt=pt, b=b: e.copy(out=XT1[:, :, b * 128:(b + 1) * 128], in_=pt[:]), reads=[pt.r], writes=[XT1.r])
                pl = next_pmm()
                for kc in range(NKC):
                    op("pe", lambda e, pl=pl, kc=kc, b=b: e.matmul(pl[:, 0:36], lhsT=XT1[:, kc, b * 128:(b + 1) * 128], rhs=Wgr[:, kc, :],
                                                                  start=(kc == 0), stop=(kc == NKC - 1)),
                       reads=[XT1.r, Wgr.r], writes=[pl.r], inc=(kc == NKC - 1))
                R = rt[u]
                rr_ = [R.r]
                op("dve", lambda e, pl=pl, R=R: e.tensor_tensor(out=R[:, 0:36], in0=pl[:, 0:36], in1=bgr[:], op=ALU.add), reads=[pl.r, bgr.r], writes=rr_)
                op("dve", lambda e, R=R: e.reduce_max(out=R[:, 36:37], in_=R[:, 0:4], axis=mybir.AxisListType.X), reads=rr_, writes=rr_)
                op("dve", lambda e, R=R: e.tensor_scalar(out=R[:, 40:44], in0=R[:, 0:4], scalar1=R[:, 36:37], scalar2=None, op0=ALU.is_equal), reads=rr_, writes=rr_)
                op("dve", lambda e, R=R: e.tensor_scalar(out=R[:, 37:38], in0=R[:, 36:37], scalar1=-1.0, scalar2=None, op0=ALU.mult), reads=rr_, writes=rr_)
                op("act", lambda e, R=R: e.activation(out=R[:, 44:48], in_=R[:, 0:4], func=AF.Exp, bias=R[:, 37:38]), reads=rr_, writes=rr_)
                op("dve", lambda e, R=R: e.reduce_sum(out=R[:, 38:39], in_=R[:, 44:48], axis=mybir.AxisListType.X), reads=rr_, writes=rr_)
                op("dve", lambda e, R=R: e.reciprocal(out=R[:, 39:40], in_=R[:, 38:39]), reads=rr_, writes=rr_)
                op("dve", lambda e, R=R: e.tensor_scalar(out=R[:, 48:56], in0=R[:, 4:12], scalar1=R[:, 40:41], scalar2=None, op0=ALU.mult), reads=rr_, writes=rr_)
                for g in range(1, 4):
                    op("dve", lambda e, R=R, g=g: e.scalar_tensor_tensor(out=R[:, 48:56], in0=R[:, 4 + 8 * g:12 + 8 * g], scalar=R[:, 40 + g:41 + g],
                                                                         in1=R[:, 48:56], op0=ALU.mult, op1=ALU.add), reads=rr_, writes=rr_)
                op("dve", lambda e, R=R: e.max(out=R[:, 56:64], in_=R[:, 48:56]), reads=rr_, writes=rr_)
                op("dve", lambda e, R=R: e.tensor_scalar(out=R[:, 64:72], in0=R[:, 48:56], scalar1=R[:, 56:57], scalar2=None, op0=ALU.is_equal), reads=rr_, writes=rr_)
                op("dve", lambda e, R=R: e.tensor_scalar(out=R[:, 72:80], in0=R[:, 48:56], scalar1=R[:, 57:58], scalar2=None, op0=ALU.is_equal), reads=rr_, writes=rr_)
                op("dve", lambda e, R=R: e.tensor_tensor(out=R[:, 80:81], in0=R[:, 57:58], in1=R[:, 56:57], op=ALU.subtract), reads=rr_, writes=rr_)
                op("act", lambda e, R=R: e.activation(out=R[:, 81:82], in_=R[:, 80:81], func=AF.Exp), reads=rr_, writes=rr_)
                op("dve", lambda e, R=R: e.tensor_scalar(out=R[:, 82:83], in0=R[:, 81:82], scalar1=1.0, scalar2=None, op0=ALU.add), reads=rr_, writes=rr_)
                op("dve", lambda e, R=R: e.reciprocal(out=R[:, 83:84], in_=R[:, 82:83]), reads=rr_, writes=rr_)
                op("dve", lambda e, R=R: e.tensor_tensor(out=R[:, 84:85], in0=R[:, 81:82], in1=R[:, 83:84], op=ALU.mult), reads=rr_, writes=rr_)
                op("dve", lambda e, R=R: e.tensor_scalar(out=R[:, 85:87], in0=R[:, 83:85], scalar1=R[:, 39:40], scalar2=None, op0=ALU.mult), reads=rr_, writes=rr_)
                op("dve", lambda e, R=R: e.tensor_scalar(out=R[:, 88:96], in0=R[:, 64:72], scalar1=R[:, 85:86], scalar2=None, op0=ALU.mult), reads=rr_, writes=rr_)
                op("dve", lambda e, R=R: e.scalar_tensor_tensor(out=R[:, 88:96], in0=R[:, 72:80], scalar=R[:, 86:87], in1=R[:, 88:96],
                                                                op0=ALU.mult, op1=ALU.add), reads=rr_, writes=rr_)
                for g in range(4):
                    op("dve", lambda e, R=R, g=g, gb=gb: e.tensor_scalar(out=wgtS[:, gb, g * 8:(g + 1) * 8], in0=R[:, 88:96], scalar1=R[:, 40 + g:41 + g],
                                                                         scalar2=None, op0=ALU.mult), reads=rr_, writes=[wgtS.res[gb]])


            prev = None
            for b in range(4):
                cur_ = part1(b)
                if prev is not None:
                    part2(b - 1, *prev)
                prev = cur_
            part2(3, *prev)
    msync = fence(("sp", "pool"))
    barrier(msync)
    p2.close()

    precast_step(len(pc_jobs))
    with ExitStack() as st3:
        Uup = S.sb("Uup", [128, 128], BF16, stack=st3)
        Mb = S.sb("Mb", [128, NB, NEXP], BF16, stack=st3)
        Mf = S.sb("Mf", [128, NB, NEXP], F32, stack=st3)
        rankS = S.sb("rankS", [128, NB, NEXP], F32, stack=st3)
        Dm = S.sb("Dm", [128, NB, NEXP], F32, stack=st3)
        Eq = S.sb("Eq", [128, NB, NEXP], F32, stack=st3)
        cntS = S.sb("cntS", [128, NEXP], F32, stack=st3)
        thr = S.sb("thr", [128, NEXP], F32, stack=st3)
        thri = S.sb("thri", [128, NEXP], I32, stack=st3)
        cmp1 = S.sb("cmp1", [128, NEXP, NEXP], F32, stack=st3)
        nblk = S.sb("nblk", [128, NEXP], F32, stack=st3)
        pend = S.sb("pend", [128, NEXP], F32, stack=st3)
        pstr = S.sb("pstr", [128, NEXP], F32, stack=st3)
        bidx = S.sb("bidx", [128, NBLK], F32, stack=st3)
        bidxi = S.sb("bidxi", [128, NBLK], I32, stack=st3)
        cmp2 = S.sb("cmp2", [128, NBLK, NEXP], F32, stack=st3)
        bef = S.sb("bef", [128, NBLK], F32, stack=st3)
        pidx = S.sb("pidx", [128, 1], F32, stack=st3)
        pidxi = S.sb("pidxi", [128, 1], I32, stack=st3)
        idxW = S.sb("idxW", [128, NBLK], I32, stack=st3)
        dBf = S.sb("dBf", [128, NB], F32, stack=st3)
        dAf = S.sb("dAf", [128, NB], F32, stack=st3)
        wBf = S.sb("wBf", [128, NB], F32, stack=st3)
        wAf = S.sb("wAf", [128, NB], F32, stack=st3)
        dAi = S.sb("dAi", [128, NB], I32, stack=st3)
        dBi = S.sb("dBi", [128, NB], I32, stack=st3)
        zero = S.sb("zero", [128, 4, D], BF16, stack=st3)
        xblk = [S.sb(f"xblk{i}", [128, D], BF16, stack=st3) for i in range(2)]
        Wt = [S.sb(f"Wt{i}", [128, 12288], BF16, stack=st3) for i in range(3)]
        xsb = [S.sb(f"xsb{i}", [128, D], BF16, stack=st3) for i in range(2)]
        xsT = [S.sb(f"xsT{i}", [128, NKC, 128], BF16, stack=st3) for i in range(2)]
        sl_ = [S.sb(f"sl{i}", [128, 4, 128], F32, stack=st3) for i in range(2)]
        gT = [S.sb(f"gT{i}", [128, 4, 128], BF16, stack=st3) for i in range(2)]
        ysb = [S.sb(f"ysb{i}", [128, D], F32, stack=st3) for i in range(2)]
        yA = [S.sb(f"yA{i}", [128, D], F32, stack=st3) for i in range(3)]
        yB = [S.sb(f"yB{i}", [128, D], F32, stack=st3) for i in range(3)]
        x1l = [S.sb(f"x1l{i}", [128, D], F32, stack=st3) for i in range(3)]
        stt3 = [S.sb(f"stt3{i}", [128, 12], F32, stack=st3) for i in range(4)]
        mv3 = [S.sb(f"mv3{i}", [128, 4], F32, stack=st3) for i in range(4)]
        wres = [wgtS.res[b] for b in range(NB)]
        op("pool", lambda e: e.memset(Uup[:], 1.0), writes=[Uup.r])
        op("pool", lambda e: e.affine_select(out=Uup[:], in_=Uup[:], pattern=[[1, 128]], compare_op=ALU.is_ge, fill=0.0, base=-1,
                                             channel_multiplier=-1), reads=[Uup.r], writes=[Uup.r])
        op("pool", lambda e: e.iota(thri[:], pattern=[[128, NEXP]], base=0, channel_multiplier=0), writes=[thri.r])
        op("dve", lambda e: e.tensor_copy(out=thr[:], in_=thri[:]), reads=[thri.r], writes=[thr.r])
        op("pool", lambda e: e.iota(bidxi[:], pattern=[[1, NBLK]], base=0, channel_multiplier=0), writes=[bidxi.r])
        op("dve", lambda e: e.tensor_copy(out=bidx[:], in_=bidxi[:]), reads=[bidxi.r], writes=[bidx.r])
        op("pool", lambda e: e.iota(pidxi[:], pattern=[[0, 1]], base=0, channel_multiplier=1), writes=[pidxi.r])
        op("dve", lambda e: e.tensor_copy(out=pidx[:], in_=pidxi[:]), reads=[pidxi.r], writes=[pidx.r])
        op("pool", lambda e: e.memset(zero[:], 0.0), writes=[zero.r])
        ztoks = []
        for i0 in range(0, NBLK, 4):
            ztoks.append(dma("sp", lambda e, i0=i0: e.dma_start(out=xs_d[i0 * 128:(i0 + 4) * 128, :].rearrange("(i p) d -> p i d", p=128), in_=zero[:]),
                             reads=[zero.r]))
        flat = lambda t: t[:].rearrange("p b e -> p (b e)")
        op("dve", lambda e: e.tensor_scalar(out=flat(Mb), in0=flat(wgtS), scalar1=0.0, scalar2=None, op0=ALU.is_gt), reads=wres, writes=[Mb.r])
        op("dve", lambda e: e.tensor_scalar(out=flat(Mf), in0=flat(wgtS), scalar1=0.0, scalar2=None, op0=ALU.is_gt), reads=wres, writes=[Mf.r])
        for b in range(NB):
            pm = next_pmm()
            for b2 in range(b):
                op("pe", lambda e, pm=pm, b2=b2: e.matmul(pm[:, 0:NEXP], lhsT=onesb[:], rhs=Mb[:, b2, :], start=(b2 == 0), stop=False),
                   reads=[onesb.r, Mb.r], writes=[pm.r], inc=False)
            op("pe", lambda e, pm=pm, b=b: e.matmul(pm[:, 0:NEXP], lhsT=Uup[:], rhs=Mb[:, b, :], start=(b == 0), stop=True),
               reads=[Uup.r, Mb.r], writes=[pm.r], inc=True)
            op("act", lambda e, pm=pm, b=b: e.copy(out=rankS[:, b, :], in_=pm[:, 0:NEXP]), reads=[pm.r], writes=[rankS.r])
        pm = next_pmm()
        for b2 in range(NB):
            op("pe", lambda e, pm=pm, b2=b2: e.matmul(pm[:, 0:NEXP], lhsT=onesb[:], rhs=Mb[:, b2, :], start=(b2 == 0), stop=(b2 == NB - 1)),
               reads=[onesb.r, Mb.r], writes=[pm.r], inc=(b2 == NB - 1))
        op("act", lambda e, pm=pm: e.copy(out=cntS[:], in_=pm[:, 0:NEXP]), reads=[pm.r], writes=[cntS.r])
        op("dve", lambda e: e.tensor_tensor(out=cmp1[:], in0=bc(cntS[:], NEXP), in1=thr[:].unsqueeze(1).to_broadcast([128, NEXP, NEXP]), op=ALU.is_gt),
           reads=[cntS.r, thr.r], writes=[cmp1.r])
        op("dve", lambda e: e.reduce_sum(out=nblk[:], in_=cmp1[:], axis=mybir.AxisListType.X), reads=[cmp1.r], writes=[nblk.r])
        op("dve", lambda e: e.tensor_tensor_scan(out=pend[:], data0=ones[:, 0:NEXP], data1=nblk[:], initial=0.0, op0=ALU.mult, op1=ALU.add),
           reads=[ones.r, nblk.r], writes=[pend.r])
        op("dve", lambda e: e.tensor_tensor(out=pstr[:], in0=pend[:], in1=nblk[:], op=ALU.subtract), reads=[pend.r, nblk.r], writes=[pstr.r])
        op("dve", lambda e: e.tensor_scalar(out=pstr[:], in0=pstr[:], scalar1=128.0, scalar2=None, op0=ALU.mult), reads=[pstr.r], writes=[pstr.r])
        op("dve", lambda e: e.tensor_tensor(out=cmp2[:], in0=pend[:].unsqueeze(1).to_broadcast([128, NBLK, NEXP]), in1=bc(bidx[:], NEXP), op=ALU.is_le),
           reads=[pend.r, bidx.r], writes=[cmp2.r])
        op("dve", lambda e: e.reduce_sum(out=bef[:], in_=cmp2[:], axis=mybir.AxisListType.X), reads=[cmp2.r], writes=[bef.r])
        op("dve", lambda e: e.tensor_scalar(out=bef[:], in0=bef[:], scalar1=float(NEXP - 1), scalar2=128.0, op0=ALU.min, op1=ALU.mult), reads=[bef.r], writes=[bef.r])
        op("dve", lambda e: e.tensor_scalar(out=idxW[:], in0=bef[:], scalar1=pidx[:, 0:1], scalar2=None, op0=ALU.add), reads=[bef.r, pidx.r], writes=[idxW.r])
        op("dve", lambda e: e.tensor_tensor(out=Dm[:], in0=rankS[:], in1=pstr[:].unsqueeze(1).to_broadcast([128, NB, NEXP]), op=ALU.add),
           reads=[rankS.r, pstr.r], writes=[Dm.r])
        op("dve", lambda e: e.tensor_tensor(out=Dm[:], in0=Dm[:], in1=Mf[:], op=ALU.mult), reads=[Dm.r, Mf.r], writes=[Dm.r])
        op("dve", lambda e: e.reduce_max(out=dBf[:], in_=Dm[:], axis=mybir.AxisListType.X), reads=[Dm.r], writes=[dBf.r])
        op("dve", lambda e: e.reduce_sum(out=dAf[:], in_=Dm[:], axis=mybir.AxisListType.X), reads=[Dm.r], writes=[dAf.r])
        op("dve", lambda e: e.tensor_tensor(out=dAf[:], in0=dAf[:], in1=dBf[:], op=ALU.subtract), reads=[dAf.r, dBf.r], writes=[dAf.r])
        op("dve", lambda e: e.tensor_tensor(out=Eq[:], in0=Dm[:], in1=bc(dBf[:], NEXP), op=ALU.is_equal), reads=[Dm.r, dBf.r], writes=[Eq.r])
        op("dve", lambda e: e.tensor_tensor(out=flat(Eq), in0=flat(Eq), in1=flat(wgtS), op=ALU.mult), reads=[Eq.r] + wres, writes=[Eq.r])
        op("dve", lambda e: e.reduce_sum(out=wBf[:], in_=Eq[:], axis=mybir.AxisListType.X), reads=[Eq.r], writes=[wBf.r])
        op("dve", lambda e: e.reduce_sum(out=wAf[:], in_=wgtS[:], axis=mybir.AxisListType.X), reads=wres, writes=[wAf.r])
        op("dve", lambda e: e.tensor_tensor(out=wAf[:], in0=wAf[:], in1=wBf[:], op=ALU.subtract), reads=[wAf.r, wBf.r], writes=[wAf.r])
        op("dve", lambda e: e.tensor_copy(out=dAi[:], in_=dAf[:]), reads=[dAf.r], writes=[dAi.r])
        op("dve", lambda e: e.tensor_copy(out=dBi[:], in_=dBf[:]), reads=[dBf.r], writes=[dBi.r])
        zres = Res()
        S.wait_all("pool", ztoks[-S.NDS:])
        zres.w = ("pool", S.cnt["pool"])
        stoks = []
        for b in range(NB):
            xb_ = xblk[b % 2]
            dma("sp", lambda e, xb_=xb_, b=b: e.dma_start(out=xb_[:], in_=x1bd[b * 128:(b + 1) * 128, :]), reads=[msync], writes=[xb_.r])
            for di in (dAi, dBi):
                stoks.append(dma("pool", lambda e, xb_=xb_, b=b, di=di: e.indirect_dma_start(
                    out=xs_d[:, :], out_offset=bass.IndirectOffsetOnAxis(ap=di[:, b:b + 1], axis=0), in_=xb_[:, :], in_offset=None),
                    reads=[xb_.r, di.r, zres]))
        sres = Res()
        S.wait_all("sp", stoks[-S.NDS:])
        sres.w = ("sp", S.cnt["sp"])
        banks = [pmm[0], pmm[1], pmm[2], pat, po, psS]
        bstate = {"i": 0}

        def next_bank():
            bstate["i"] += 1
            return banks[bstate["i"] % len(banks)]

        ytoks = []

        def stage1(i):
            u = i % 2
            W_, xs_, xT_, s_, g_ = Wt[i % 3], xsb[u], xsT[u], sl_[u], gT[u]
            dma("pool", lambda e: e.indirect_dma_start(out=W_[:, :], out_offset=None, in_=WS[:, :],
                                                       in_offset=bass.IndirectOffsetOnAxis(ap=idxW[:, i:i + 1], axis=0)),
                reads=[idxW.r, msync], writes=[W_.r])
            dma("sp", lambda e: e.dma_start(out=xs_[:], in_=xs_d[i * 128:(i + 1) * 128, :]), reads=[sres], writes=[xs_.r])
            pt = next_ptr()
            for kc in range(NKC):
                op("pe", lambda e, kc=kc: e.transpose(pt[:, kc, :], xs_[:, kc * 128:(kc + 1) * 128], ident[:]),
                   reads=[xs_.r, ident.r], writes=[pt.r], inc=(kc == NKC - 1))
            op("act", lambda e: e.copy(out=xT_[:], in_=pt[:]), reads=[pt.r], writes=[xT_.r])
            W1 = W_[:, 0:4096].rearrange("p (k f) -> p k f", f=DEXP)
            W3 = W_[:, 4096:8192].rearrange("p (k f) -> p k f", f=DEXP)
            p1, p3 = next_bank(), next_bank()
            for (pp, Wm) in ((p1, W1), (p3, W3)):
                for fc in range(4):
                    for kc in range(NKC):
                        op("pe", lambda e, pp=pp, Wm=Wm, fc=fc, kc=kc: e.matmul(pp[:, fc * 128:(fc + 1) * 128], lhsT=Wm[:, kc, fc * 128:(fc + 1) * 128],
                                                                              rhs=xT_[:, kc, :], start=(kc == 0), stop=(kc == NKC - 1)),
                           reads=[W_.r, xT_.r], writes=[pp.r], inc=(kc == NKC - 1 and fc == 3))
            op("act", lambda e: e.activation(out=s_[:].rearrange("p a b -> p (a b)"), in_=p1[:, :], func=AF.Silu), reads=[p1.r], writes=[s_.r])
            op("dve", lambda e: e.tensor_tensor(out=g_[:].rearrange("p a b -> p (a b)"), in0=p3[:, :], in1=s_[:].rearrange("p a b -> p (a b)"), op=ALU.mult),
               reads=[p3.r, s_.r], writes=[g_.r])

        def stage2(i):
            u = i % 2
            W_, g_, y_ = Wt[i % 3], gT[u], ysb[u]
            W2 = W_[:, 8192:12288].rearrange("p (k f) -> p k f", f=D)
            for h2 in range(2):
                py = next_bank()
                for fc in range(4):
                    op("pe", lambda e, py=py, fc=fc, h2=h2: e.matmul(py[:, :], lhsT=g_[:, fc, :], rhs=W2[:, fc, h2 * 512:(h2 + 1) * 512],
                                                                   start=(fc == 0), stop=(fc == 3)),
                       reads=[g_.r, W_.r], writes=[py.r], inc=(fc == 3))
                if h2 == 0:
                    op("act", lambda e, py=py: e.copy(out=y_[:, 0:512], in_=py[:, :]), reads=[py.r], writes=[y_.r])
                else:
                    op("dve", lambda e, py=py: e.tensor_copy(out=y_[:, 512:1024], in_=py[:, :]), reads=[py.r], writes=[y_.r])
            ytoks.append(dma("sp", lambda e: e.dma_start(out=ys_d[i * 128:(i + 1) * 128, :], in_=y_[:]), reads=[y_.r]))

        stage1(0)
        for i in range(NBLK):
            if i + 1 < NBLK:
                stage1(i + 1)
            stage2(i)
        yres = Res()
        S.wait_all("pool", ytoks[-S.NDS:])
        yres.w = ("pool", S.cnt["pool"])
        for b in range(NB):
            u = b % 3
            dma("sp", lambda e, u=u, b=b: e.dma_start(out=x1l[u][:], in_=x1f[b * 128:(b + 1) * 128, :]), reads=[msync], writes=[x1l[u].r])
            for (yt, di) in ((yA[u], dAi), (yB[u], dBi)):
                dma("pool", lambda e, yt=yt, di=di, b=b: e.indirect_dma_start(out=yt[:, :], out_offset=None, in_=ys_d[:, :],
                                                                            in_offset=bass.IndirectOffsetOnAxis(ap=di[:, b:b + 1], axis=0)),
                    reads=[di.r, yres], writes=[yt.r])
            z_ = x1l[u]
            op("dve", lambda e, u=u, b=b, z_=z_: e.scalar_tensor_tensor(out=yA[u][:], in0=yA[u][:], scalar=wAf[:, b:b + 1], in1=yB[u][:], op0=ALU.mult, op1=ALU.bypass)
               if False else e.tensor_scalar(out=yA[u][:], in0=yA[u][:], scalar1=wAf[:, b:b + 1], scalar2=None, op0=ALU.mult),
               reads=[yA[u].r, wAf.r], writes=[yA[u].r])
            op("dve", lambda e, u=u, b=b: e.scalar_tensor_tensor(out=yA[u][:], in0=yB[u][:], scalar=wBf[:, b:b + 1], in1=yA[u][:], op0=ALU.mult, op1=ALU.add),
               reads=[yA[u].r, yB[u].r, wBf.r], writes=[yA[u].r])
            op("dve", lambda e, u=u, z_=z_: e.scalar_tensor_tensor(out=z_[:], in0=z_[:], scalar=ALPHA, in1=yA[u][:], op0=ALU.mult, op1=ALU.add),
               reads=[z_.r, yA[u].r], writes=[z_.r])
            layer_norm(z_, 2, yB[u], stt3[u], mv3[u], mul_eng="dve")
            dma("sp", lambda e, u=u, b=b: e.dma_start(out=out_d[b * 128:(b + 1) * 128, :], in_=yB[u][:]), reads=[yB[u].r])
        fence(("sp", "pool"))
    p23.close()
    S.emit()
    S.stack.close()
    return nc


def _ret_expo():
    i = np.arange(128)
    ii = i % 64
    c = i // 64
    f = np.zeros((5, 128), np.float64)
    f[0] = ii + 1
    f[1] = i + 1
    f[2] = -(ii + 1)
    f[3] = np.where(c == 0, 63 - ii, -(ii + 1))
    f[4] = 127 - i
    b = f[:, ::-1]
    tab = np.concatenate([f, b], 0).astype(np.float32)
    return np.ascontiguousarray(np.broadcast_to(tab.reshape(1, 1280), (128, 1280)))


def _win_perm(swap):
    K = 1024
    off = {n: i * K for i, n in enumerate(["hq", "hff", "hfb", "hi", "hg", "rq", "rk", "rv", "rg", "ga", "gb"])}
    ff, fb = ("hfb", "hff") if swap else ("hff", "hfb")
    cols = []
    for h in range(HG_H):
        for n in ("hq", ff, fb, "hi", "hg"):
            cols.append(off[n] + h * 128 + np.arange(128))
    for r in range(RET_H):
        perm = np.concatenate([np.arange(0, 256, 2), np.arange(1, 256, 2)])
        cols.append(off["rq"] + r * 256 + perm)
        cols.append(off["rk"] + r * 256 + perm)
        cols.append(off["rv"] + r * 256 + np.arange(256))
        cols.append(off["rg"] + r * 256 + np.arange(256))
    cols.append(off["ga"] + np.arange(K))
    cols.append(off["gb"] + np.arange(K))
    return np.concatenate(cols)


_NC_CACHE = {}


def kernel(x, positions, w_in, hg_lb_logits, hg_norm_g, ret_norm_g, w_branch_hg, w_branch_ret, w_out, ln1_g, ln1_b,
           w_group, b_group, w_router, b_router, w1, w3, w2, ln2_g, ln2_b, _debug=False):
    x = np.asarray(x, np.float32)
    B, L, _ = x.shape
    T = L // 2
    ncores = 2 * B
    key = (T, _debug)
    if key not in _NC_CACHE:
        _NC_CACHE[key] = build(T, _debug)
    nc = _NC_CACHE[key]
    positions = np.asarray(positions, np.int32)
    w_in0 = np.asarray(w_in, np.float32)[0]
    wins = [np.ascontiguousarray(w_in0[:, _win_perm(False)]), np.ascontiguousarray(w_in0[:, _win_perm(True)])]
    invf = (1.0 / (np.float32(10000.0) ** np.linspace(0.0, 1.0, 128, dtype=np.float32))).astype(np.float32).reshape(128, 1)
    f32 = lambda a: np.ascontiguousarray(np.asarray(a, np.float32))
    common = {
        "lbl": f32(hg_lb_logits), "hgg": f32(hg_norm_g), "retg": f32(ret_norm_g),
        "wbh": f32(w_branch_hg)[0], "wbr": f32(w_branch_ret)[0], "wo": f32(w_out)[0],
        "l1g": f32(ln1_g), "l1b": f32(ln1_b), "l2g": f32(ln2_g), "l2b": f32(ln2_b),
        "wgr": np.ascontiguousarray(np.concatenate([f32(w_group)[0], f32(w_router)[0]], 1)),
        "bgr": np.ascontiguousarray(np.concatenate([f32(b_group), f32(b_router)], 1)),
        "w1": f32(w1)[0], "w3": f32(w3)[0], "w2": f32(w2)[0],
        "invf": invf, "rexpo": _ret_expo(),
    }
    in_maps = []
    for c in range(ncores):
        b, half = c // 2, c % 2
        xb, pb = x[b], positions[b]
        if half == 1:
            xb, pb = xb[::-1], pb[::-1]
        m = dict(common)
        m["x"] = np.ascontiguousarray(xb)
        m["pos"] = np.ascontiguousarray(pb).reshape(1, L)
        m["w_in"] = wins[half]
        in_maps.append(m)
    res = run_bass_kernel_spmd(nc, in_maps, core_ids=list(range(ncores)))
    out = np.empty((B, L, D), np.float32)
    for c in range(ncores):
        b, half = c // 2, c % 2
        o = np.asarray(res.results[c]["out"])
        if half == 0:
            out[b, :T] = o
        else:
            out[b, T:] = o[::-1]
    if _debug:
        return out, res.results
    return out
```

```python
from contextlib import ExitStack
import math
import numpy as np
import concourse.bass as bass
import concourse.mybir as mybir
from concourse.bass_utils import run_bass_kernel_spmd

F32 = mybir.dt.float32
BF16 = mybir.dt.bfloat16
I32 = mybir.dt.int32
ALU = mybir.AluOpType
AF = mybir.ActivationFunctionType

D = 1024
NKC = 8
TT = 512
HG_H = 8
RET_H = 4
NEXP = 32
DEXP = 512
ALPHA = 2.0 ** 0.25
LN_EPS = 1e-5
RMS_EPS = 1e-6
OFF_RET = HG_H * 640
OFF_G = OFF_RET + RET_H * 1024
TWO_PI = 2.0 * math.pi
CW1 = 6.28125
CW2 = TWO_PI - CW1


class Res:
    __slots__ = ("w", "r")

    def __init__(self):
        self.w = None
        self.r = {}


class Tile:
    def __init__(self, t, nres=1):
        self.t = t
        self.res = [Res() for _ in range(nres)]

    @property
    def r(self):
        return self.res[0]

    def __getitem__(self, k):
        return self.t[k]


class Sched:
    ENG = ("pe", "act", "dve", "pool", "sp")
    NDS = 8

    def __init__(self, nc):
        self.nc = nc
        self.ops = {e: [] for e in self.ENG}
        self.cnt = {e: 0 for e in self.ENG}
        self.waited = {e: {} for e in self.ENG}
        self.dcount = {e: 0 for e in self.ENG}
        self.stack = ExitStack()

    def sb(self, name, shape, dtype, nres=1, stack=None):
        t = (stack or self.stack).enter_context(self.nc.sbuf_tensor("sb_" + name, list(shape), dtype))
        return Tile(t, nres)

    def ps(self, name, shape, dtype, nres=1):
        t = self.stack.enter_context(self.nc.psum_tensor("ps_" + name, list(shape), dtype))
        return Tile(t, nres)

    def _collect(self, eng, reads, writes, extra=()):
        deps = {}

        def add(tok):
            if tok is None:
                return
            k, v = tok
            if deps.get(k, 0) < v:
                deps[k] = v

        for r in reads:
            add(r.w)
        for w in writes:
            add(w.w)
            for k, v in w.r.items():
                add((k, v))
        for t in extra:
            add(t)
        waits = []
        for k, v in deps.items():
            if k == eng and eng == "pe":
                continue
            if self.waited[eng].get(k, 0) >= v:
                continue
            self.waited[eng][k] = v
            waits.append((k, v))
        return waits

    def _record(self, tok, reads, writes):
        k, v = tok
        for r in reads:
            if r.r.get(k, 0) < v:
                r.r[k] = v
        for w in writes:
            w.w = tok
            w.r = {}

    def op(self, eng, fn, reads=(), writes=(), inc=True):
        assert inc or eng == "pe"
        waits = self._collect(eng, reads, writes)
        if inc:
            self.cnt[eng] += 1
            tok = (eng, self.cnt[eng])
            incinfo = (eng, 1)
        else:
            tok = (eng, self.cnt[eng] + 1)
            incinfo = None
        self.ops[eng].append((waits, fn, incinfo))
        self._record(tok, reads, writes)
        return tok

    def dma(self, q, fn, reads=(), writes=()):
        n = self.dcount[q]
        self.dcount[q] += 1
        slot, rnd = n % self.NDS, n // self.NDS
        key = ("dma", q, slot)
        extra = [(key, 16 * rnd)] if rnd > 0 else []
        waits = self._collect(q, reads, writes, extra)
        tok = (key, 16 * (rnd + 1))
        self.ops[q].append((waits, fn, (key, 16)))
        self._record(tok, reads, writes)
        return tok

    def wait_all(self, eng, toks):
        waits = self._collect(eng, (), (), toks)
        self.cnt[eng] += 1
        self.ops[eng].append((waits, None, (eng, 1)))

    def emit(self):
        nc = self.nc
        keys = set()
        for e in self.ENG:
            for waits, fn, incinfo in self.ops[e]:
                for k, v in waits:
                    keys.add(k)
                if incinfo:
                    keys.add(incinfo[0])
        sems = {}
        for k in sorted(keys, key=str):
            nm = k if isinstance(k, str) else f"d_{k[1]}_{k[2]}"
            sems[k] = self.stack.enter_context(nc.semaphore("s_" + nm))

        def run(name, e):
            for waits, fn, incinfo in self.ops[name]:
                for k, v in waits:
                    e.wait_ge(sems[k], v)
                ins = e.nop() if fn is None else fn(e)
                if incinfo:
                    ins.then_inc(sems[incinfo[0]], incinfo[1])

        with nc.Block() as block:
            @block.tensor
            def _(e):
                run("pe", e)

            @block.scalar
            def _(e):
                run("act", e)

            @block.vector
            def _(e):
                run("dve", e)

            @block.gpsimd
            def _(e):
                run("pool", e)

            @block.sync
            def _(e):
                run("sp", e)


def bc(ap, n):
    return ap.unsqueeze(ap.ndim).to_broadcast(list(ap.shape) + [n])


def build(T, debug=False):
    assert T % TT == 0
    NT = T // TT
    NB = T // 128
    nc = bass.Bass("TRN2", target_bir_lowering=False)
    dt_in = lambda name, shape, dt=F32: nc.dram_tensor(name, list(shape), dt, kind="ExternalInput").ap()
    x_d = dt_in("x", [2 * T, D])
    pos_d = dt_in("pos", [1, 2 * T], I32)
    win_d = dt_in("w_in", [D, 11264])
    lbl_d = dt_in("lbl", [2, D])
    hgg_d = dt_in("hgg", [1, D])
    retg_d = dt_in("retg", [1, D])
    wbh_d = dt_in("wbh", [D, D])
    wbr_d = dt_in("wbr", [D, D])
    wo_d = dt_in("wo", [D, D])
    l1g_d = dt_in("l1g", [1, D])
    l1b_d = dt_in("l1b", [1, D])
    l2g_d = dt_in("l2g", [1, D])
    l2b_d = dt_in("l2b", [1, D])
    wgr_d = dt_in("wgr", [D, 36])
    bgr_d = dt_in("bgr", [1, 36])
    w1_d = dt_in("w1", [NEXP, D, DEXP])
    w3_d = dt_in("w3", [NEXP, D, DEXP])
    w2_d = dt_in("w2", [NEXP, DEXP, D])
    invf_d = dt_in("invf", [128, 1])
    rexpo_d = dt_in("rexpo", [128, 10 * 128])
    out_d = nc.dram_tensor("out", [T, D], F32, kind="ExternalOutput").ap()
    dk = "ExternalOutput" if debug else "Internal"
    xTd = nc.dram_tensor("xTd", [2 * NT, 128, NKC * TT], BF16, kind="Internal").ap()
    csd = nc.dram_tensor("csd", [2 * NT, 128, 2 * TT], F32, kind="Internal").ap()
    oS = nc.dram_tensor("oS", [2 * D, T], BF16, kind=dk).ap()
    x1f = nc.dram_tensor("x1f", [T, D], F32, kind=dk).ap()
    x1Td = nc.dram_tensor("x1Td", [NT, 128, NKC * TT], BF16, kind="Internal").ap()
    NBLK = (2 * T) // 128 + NEXP
    x1bd = nc.dram_tensor("x1bd", [T, D], BF16, kind="Internal").ap()
    xs_d = nc.dram_tensor("xs_d", [NBLK * 128, D], BF16, kind="Internal").ap()
    ys_d = nc.dram_tensor("ys_d", [NBLK * 128, D], F32, kind="Internal").ap()
    WS = nc.dram_tensor("WS", [NEXP * 128, 12288], BF16, kind="Internal").ap()

    S = Sched(nc)
    op, dma = S.op, S.dma

    pmm = [S.ps(f"pmm{i}", [128, 512], F32) for i in range(3)]
    ptr = [S.ps(f"ptr{i}", [128, NKC, 128], BF16) for i in range(2)]
    pat = S.ps("pat", [128, 512], F32, nres=4)
    po = S.ps("po", [128, 512], F32, nres=2)
    psS = S.ps("psS", [128, 512], F32, nres=4)
    pat.res = [pat.res[0]] * 4
    po.res = [po.res[0]] * 2
    psS.res = [psS.res[0]] * 4
    cnt = {"mm": 0, "tr": 0}

    mmbanks = {"l": pmm}

    def next_pmm():
        cnt["mm"] += 1
        l = mmbanks["l"]
        return l[cnt["mm"] % len(l)]

    def next_ptr():
        cnt["tr"] += 1
        return ptr[cnt["tr"] % 2]

    ident = S.sb("ident", [128, 128], BF16)
    maskF = S.sb("maskF", [128, 128], F32)
    maskB = S.sb("maskB", [128, 128], F32)
    ones = S.sb("ones", [128, 512], F32)
    onesb = S.sb("onesb", [128, 128], BF16)
    lbT = S.sb("lbT", [128, 4, HG_H], F32)
    lraw = S.sb("lraw", [128, 2, HG_H], F32)
    hggT = S.sb("hggT", [128, HG_H], F32)
    retgT = S.sb("retgT", [128, 8], F32)
    invf = S.sb("invf", [128, 1], F32)
    epsr = S.sb("epsr", [128, 2], F32)

    op("pool", lambda e: e.memset(ident[:], 1.0), writes=[ident.r])
    op("pool", lambda e: e.affine_select(out=ident[:], in_=ident[:], pattern=[[-1, 128]], compare_op=ALU.is_equal,
                                         fill=0.0, base=0, channel_multiplier=1), reads=[ident.r], writes=[ident.r])
    op("pool", lambda e: e.memset(maskF[:], 1.0), writes=[maskF.r])
    op("pool", lambda e: e.affine_select(out=maskF[:], in_=maskF[:], pattern=[[1, 128]], compare_op=ALU.is_ge,
                                         fill=0.0, base=0, channel_multiplier=-1), reads=[maskF.r], writes=[maskF.r])
    op("pool", lambda e: e.memset(maskB[:], 1.0), writes=[maskB.r])
    op("pool", lambda e: e.affine_select(out=maskB[:], in_=maskB[:], pattern=[[-1, 128]], compare_op=ALU.is_ge,
                                         fill=0.0, base=0, channel_multiplier=1), reads=[maskB.r], writes=[maskB.r])
    op("dve", lambda e: e.memset(ones[:], 1.0), writes=[ones.r])
    op("dve", lambda e: e.memset(onesb[:], 1.0), writes=[onesb.r])
    op("dve", lambda e: e.memset(epsr[:, 0:1], RMS_EPS), writes=[epsr.r])
    op("dve", lambda e: e.memset(epsr[:, 1:2], LN_EPS), writes=[epsr.r])
    dma("sp", lambda e: e.dma_start(out=lraw[:], in_=lbl_d.rearrange("r (h p) -> p r h", p=128), allow_slow_non_contiguous=True), writes=[lraw.r])
    dma("sp", lambda e: e.dma_start(out=hggT[:], in_=hgg_d.rearrange("r (h p) -> p (r h)", p=128), allow_slow_non_contiguous=True), writes=[hggT.r])
    dma("sp", lambda e: e.dma_start(out=retgT[:], in_=retg_d.rearrange("r (h p) -> p (r h)", p=128), allow_slow_non_contiguous=True), writes=[retgT.r])
    dma("sp", lambda e: e.dma_start(out=invf[:], in_=invf_d), writes=[invf.r])
    op("dve", lambda e: e.tensor_tensor(out=lbT[:, 3, :], in0=lraw[:, 1, :], in1=lraw[:, 0, :], op=ALU.subtract),
       reads=[lraw.r], writes=[lbT.r])
    op("act", lambda e: e.activation(out=lbT[:, 0, :], in_=lbT[:, 3, :], func=AF.Sigmoid), reads=[lbT.r], writes=[lbT.r])
    op("dve", lambda e: e.tensor_scalar(out=lbT[:, 1, :], in0=lbT[:, 0, :], scalar1=-1.0, scalar2=1.0, op0=ALU.mult, op1=ALU.add),
       reads=[lbT.r], writes=[lbT.r])
    op("dve", lambda e: e.tensor_scalar(out=lbT[:, 2, :], in0=lbT[:, 1, :], scalar1=-1.0, scalar2=None, op0=ALU.mult),
       reads=[lbT.r], writes=[lbT.r])

    with ExitStack() as st:
        xb = [S.sb(f"xb{i}", [128, 4, D], BF16, stack=st) for i in range(2)]
        xf = [S.sb(f"xf{i}", [128, 4, D], F32, stack=st) for i in range(2)]

        def xload(jj):
            xff = xf[jj % 2]
            dma("sp", lambda e: e.dma_start(out=xff[:], in_=x_d[jj * TT:(jj + 1) * TT, :].rearrange("(b p) d -> p b d", p=128)), writes=[xff.r])

        xload(0)
        xTt = [S.sb(f"xTt{i}", [128, NKC, TT], BF16, stack=st) for i in range(2)]
        posi = [S.sb(f"posi{i}", [128, TT], I32, stack=st) for i in range(2)]
        ang = [S.sb(f"ang{i}", [128, TT], F32, stack=st) for i in range(2)]
        kf = [S.sb(f"kf{i}", [128, TT], F32, stack=st) for i in range(2)]
        ki = [S.sb(f"ki{i}", [128, TT], I32, stack=st) for i in range(2)]
        rr = [S.sb(f"rr{i}", [128, TT], F32, stack=st) for i in range(2)]
        yy = [S.sb(f"yy{i}", [128, TT], F32, stack=st) for i in range(2)]
        mm_ = [S.sb(f"mm{i}", [128, TT], F32, stack=st) for i in range(2)]
        cs = [S.sb(f"cs{i}", [128, 2, TT], F32, stack=st) for i in range(2)]
        nblk = 0
        for j in range(2 * NT):
            xt = xTt[j % 2]
            xbb = xb[j % 2]
            xff = xf[j % 2]
            if j + 1 < 2 * NT:
                xload(j + 1)
            for b in range(4):
                if b < 3:
                    op("act", lambda e, xff=xff, xbb=xbb, b=b: e.copy(out=xbb[:, b, :], in_=xff[:, b, :]), reads=[xff.r], writes=[xbb.r])
                else:
                    op("dve", lambda e, xff=xff, xbb=xbb, b=b: e.tensor_copy(out=xbb[:, b, :], in_=xff[:, b, :]), reads=[xff.r], writes=[xbb.r])
            for b in range(4):
                pt = next_ptr()
                for kc in range(NKC):
                    op("pe", lambda e, pt=pt, xbb=xbb, kc=kc, b=b: e.transpose(pt[:, kc, :], xbb[:, b, kc * 128:(kc + 1) * 128], ident[:]),
                       reads=[xbb.r, ident.r], writes=[pt.r], inc=(kc == NKC - 1))
                eng = "act" if b % 2 == 0 else "dve"
                if eng == "act":
                    op("act", lambda e, pt=pt, xt=xt, b=b: e.copy(out=xt[:, :, b * 128:(b + 1) * 128], in_=pt[:]),
                       reads=[pt.r], writes=[xt.r])
                else:
                    op("dve", lambda e, pt=pt, xt=xt, b=b: e.tensor_copy(out=xt[:, :, b * 128:(b + 1) * 128], in_=pt[:]),
                       reads=[pt.r], writes=[xt.r])
            dma("sp", lambda e, xt=xt, j=j: e.dma_start(out=xTd[j].rearrange("p (k t) -> p k t", t=TT), in_=xt[:]), reads=[xt.r])
            s = j % 2
            pi_, an, kf_, ki_, r_, y_, m_, c_ = posi[s], ang[s], kf[s], ki[s], rr[s], yy[s], mm_[s], cs[s]
            dma("sp", lambda e, pi_=pi_, j=j: e.dma_start(out=pi_[:], in_=pos_d[0:1, j * TT:(j + 1) * TT].partition_broadcast(128)),
                writes=[pi_.r])
            op("dve", lambda e, pi_=pi_, an=an: e.tensor_copy(out=an[:], in_=pi_[:]), reads=[pi_.r], writes=[an.r])
            op("dve", lambda e, an=an: e.tensor_scalar(out=an[:], in0=an[:], scalar1=invf[:, 0:1], scalar2=None, op0=ALU.mult),
               reads=[an.r, invf.r], writes=[an.r])
            op("dve", lambda e, an=an, ki_=ki_: e.tensor_scalar(out=ki_[:], in0=an[:], scalar1=1.0 / TWO_PI, scalar2=None, op0=ALU.mult),
               reads=[an.r], writes=[ki_.r])
            op("dve", lambda e, kf_=kf_, ki_=ki_: e.tensor_copy(out=kf_[:], in_=ki_[:]), reads=[ki_.r], writes=[kf_.r])
            op("dve", lambda e, r_=r_, kf_=kf_, an=an: e.scalar_tensor_tensor(out=r_[:], in0=kf_[:], scalar=-CW1, in1=an[:], op0=ALU.mult, op1=ALU.add),
               reads=[kf_.r, an.r], writes=[r_.r])
            op("dve", lambda e, r_=r_, kf_=kf_: e.scalar_tensor_tensor(out=r_[:], in0=kf_[:], scalar=-CW2, in1=r_[:], op0=ALU.mult, op1=ALU.add),
               reads=[kf_.r, r_.r], writes=[r_.r])
            for which, shift in ((1, 0.0), (0, math.pi / 2)):
                op("pool", lambda e, y_=y_, r_=r_, shift=shift: e.tensor_scalar(out=y_[:], in0=r_[:], scalar1=shift, scalar2=None, op0=ALU.add),
                   reads=[r_.r], writes=[y_.r])
                op("dve", lambda e, y_=y_, m_=m_: e.tensor_scalar(out=m_[:], in0=y_[:], scalar1=math.pi, scalar2=-TWO_PI, op0=ALU.is_gt, op1=ALU.mult),
                   reads=[y_.r], writes=[m_.r])
                op("dve", lambda e, y_=y_, m_=m_: e.tensor_tensor(out=y_[:], in0=y_[:], in1=m_[:], op=ALU.add), reads=[y_.r, m_.r], writes=[y_.r])
                op("dve", lambda e, y_=y_, m_=m_: e.tensor_scalar(out=m_[:], in0=y_[:], scalar1=-math.pi, scalar2=TWO_PI, op0=ALU.is_lt, op1=ALU.mult),
                   reads=[y_.r], writes=[m_.r])
                op("dve", lambda e, y_=y_, m_=m_: e.tensor_tensor(out=y_[:], in0=y_[:], in1=m_[:], op=ALU.add), reads=[y_.r, m_.r], writes=[y_.r])
                op("dve", lambda e, y_=y_: e.tensor_scalar(out=y_[:], in0=y_[:], scalar1=-3.1415925, scalar2=3.1415925, op0=ALU.max, op1=ALU.min),
                   reads=[y_.r], writes=[y_.r])
                op("act", lambda e, y_=y_, c_=c_, which=which: e.activation(out=c_[:, which, :], in_=y_[:], func=AF.Sin), reads=[y_.r], writes=[c_.r])
            dma("sp", lambda e, c_=c_, j=j: e.dma_start(out=csd[j].rearrange("p (k t) -> p k t", t=TT), in_=c_[:]), reads=[c_.r])
    def fence(queues=("sp",)):
        toks = []
        for q in queues:
            n = S.dcount[q]
            for sl in range(S.NDS):
                if n > sl:
                    toks.append((("dma", q, sl), 16 * ((n - 1 - ((n - 1 - sl) % S.NDS)) // S.NDS + 1)))
        waits = S._collect("sp", (), (), toks)
        S.cnt["sp"] += 1
        S.ops["sp"].append((waits, None, ("sp", 1)))
        r = Res()
        r.w = ("sp", S.cnt["sp"])
        return r

    def barrier(extra_res):
        toks = [(eng, S.cnt[eng]) for eng in ("pe", "act", "dve", "pool")] + [extra_res.w]
        for eng in S.ENG:
            waits = S._collect(eng, (), (), toks)
            if waits:
                S.cnt[eng] += 1
                S.ops[eng].append((waits, None, (eng, 1)))

    xsync = fence(("sp", "pool"))
    barrier(xsync)

    ph_stack = ExitStack()
    st = ph_stack
    rexpo = S.sb("rexpo", [128, 10, 128], F32, stack=st)
    rtab = S.sb("rtab", [128, 10, 128], F32, stack=st)
    Sinit_h = S.sb("Sinit_h", [128, HG_H, 128], F32, stack=st)
    Sinit_r = S.sb("Sinit_r", [128, RET_H, 2, 256], F32, stack=st)
    dma("sp", lambda e: e.dma_start(out=rexpo[:], in_=rexpo_d.rearrange("p (k i) -> p k i", i=128)), writes=[rexpo.r])
    Wh = [S.sb(f"Wh{i}", [128, NKC, 1024], BF16, stack=st) for i in range(2)]
    xTt = [S.sb(f"xTl{i}", [128, NKC, TT], BF16, stack=st) for i in range(2)]
    cst = [S.sb(f"cst{i}", [128, 2, TT], F32, stack=st) for i in range(1)]
    qS = S.sb("qS", [128, 2, T], BF16, stack=st, nres=NT)
    vS = S.sb("vS", [128, NB, 256], BF16, stack=st, nres=NT)
    oF = S.sb("oF", [128, 2, T], BF16, stack=st, nres=NT)
    Sst = S.sb("Sst", [128, 2, 256], F32, stack=st)
    Sbfs = [S.sb(f"Sbf{i}", [128, 2, 256], BF16, stack=st) for i in range(2)]
    scur = {"i": 0}
    NSET = 2
    tA = [S.sb(f"tA{i}", [128, TT], F32, stack=st) for i in range(NSET)]
    tB = [S.sb(f"tB{i}", [128, TT], F32, stack=st) for i in range(NSET)]
    tC = [S.sb(f"tC{i}", [128, TT], F32, stack=st) for i in range(NSET)]
    tG = [S.sb(f"tG{i}", [128, TT], F32, stack=st) for i in range(NSET)]
    tE = [S.sb(f"tE{i}", [128, TT], F32, stack=st) for i in range(NSET)]
    tF = [S.sb(f"tF{i}", [128, TT], F32, stack=st) for i in range(NSET)]
    tBc = [S.sb(f"tBc{i}", [128, 8], F32, stack=st) for i in range(NSET)]
    tcd = [S.sb(f"tcd{i}", [128, 8], F32, stack=st) for i in range(NSET)]
    tcb = [S.sb(f"tcb{i}", [128, 4], F32, stack=st) for i in range(NSET)]
    QT = [S.sb(f"QT{i}", [128, 2, TT], BF16, stack=st) for i in range(NSET)]
    QM = [S.sb(f"QM{i}", [128, 2, TT], BF16, stack=st) for i in range(NSET)]
    KA = [S.sb(f"KA{i}", [128, 2, TT], BF16, stack=st) for i in range(NSET)]
    KB = [S.sb(f"KB{i}", [128, 2, TT], BF16, stack=st) for i in range(NSET)]
    KM = [S.sb(f"KM{i}", [128, 2, TT], BF16, stack=st) for i in range(NSET)]
    vT = [S.sb(f"vT{i}", [128, 4, 256], BF16, stack=st) for i in range(NSET)]
    krot = [S.sb(f"krot{i}", [128, 2, TT], BF16, stack=st) for i in range(NSET)]
    ATm = [S.sb(f"ATm{i}", [128, 128], BF16, stack=st) for i in range(4)]
    kTs = [S.sb(f"kTs{i}", [128, 2, 128], BF16, stack=st) for i in range(4)]
    osum = [S.sb(f"osum{i}", [128, 2, TT], F32, stack=st) for i in range(2)]
    sq = [S.sb(f"sq{i}", [128, 2, TT], BF16, stack=st) for i in range(1)] * 2
    sg = [S.sb(f"sg{i}", [128, 2, TT], BF16, stack=st) for i in range(2)]
    rstd = [S.sb(f"rstd{i}", [128, TT], F32, stack=st) for i in range(1)] * 2
    oOut = [S.sb(f"oOut{i}", [128, 2, TT], BF16, stack=st) for i in range(2)]
    visit = {"n": 0, "blk": 0, "wh": 0, "xl": 0}

    wsched = []
    for _ph in (0, 1):
        wsched += [(h * 640, 640) for h in range(HG_H)] + [(OFF_RET + hr * 1024, 1024) for hr in range(RET_H)]
    wstate = {"issued": 0, "cur": -1}

    def _issue_w():
        i = wstate["issued"]
        if i >= len(wsched):
            return
        c0, ncols = wsched[i]
        W = Wh[i % 2]
        dma("pool", lambda e: e.dma_start(out=W[:, :, 0:ncols], in_=win_d[:, c0:c0 + ncols].rearrange("(k p) c -> p k c", p=128)),
            writes=[W.r])
        wstate["issued"] += 1

    def load_w(c0, ncols):
        wstate["cur"] += 1
        i = wstate["cur"]
        assert wsched[i] == (c0, ncols)
        while wstate["issued"] <= i:
            _issue_w()
        return Wh[i % 2]

    def prefetch_w():
        if wstate["issued"] <= wstate["cur"] + 1:
            _issue_w()

    pc_jobs = []
    for ex in range(NEXP):
        pc_jobs.append((w1_d[ex].rearrange("(k p) f -> p k f", p=128), ex, 0, 4096, DEXP))
        pc_jobs.append((w3_d[ex].rearrange("(k p) f -> p k f", p=128), ex, 4096, 4096, DEXP))
        pc_jobs.append((w2_d[ex].rearrange("(k p) f -> p k f", p=128), ex, 8192, 4096, D))
    pc_state = {"i": 0}

    def precast_step(n=1):
        for _ in range(n):
            i = pc_state["i"]
            if i >= len(pc_jobs):
                return
            src, ex, c0, w, inner = pc_jobs[i]
            pc_state["i"] += 1
            dma("pool", lambda e, src=src, ex=ex, c0=c0, w=w, inner=inner: e.dma_start(
                out=WS[ex * 128:(ex + 1) * 128, c0:c0 + w].rearrange("p (k f) -> p k f", f=inner), in_=src))

    xpend = {}

    def prefetch_xT(gt):
        if gt is None or gt in xpend:
            return
        visit["xl"] += 1
        X = xTt[visit["xl"] % 2]
        dma("sp", lambda e: e.dma_start(out=X[:], in_=xTd[gt].rearrange("p (k t) -> p k t", t=TT)), reads=[xsync], writes=[X.r])
        xpend[gt] = X

    def load_xT(gt, nxt=None):
        prefetch_xT(gt)
        X = xpend.pop(gt)
        prefetch_xT(nxt)
        precast_step(1)
        return X

    def load_cs(gt):
        C = cst[0]
        dma("sp", lambda e: e.dma_start(out=C[:], in_=csd[gt].rearrange("p (k t) -> p k t", t=TT)), reads=[xsync], writes=[C.r])
        return C

    def inproj_fm(W, c0, X):
        pm = next_pmm()
        for kc in range(NKC):
            op("pe", lambda e, kc=kc: e.matmul(pm[:, :], lhsT=W[:, kc, c0:c0 + 128], rhs=X[:, kc, :], start=(kc == 0), stop=(kc == NKC - 1)),
               reads=[W.r, X.r], writes=[pm.r], inc=(kc == NKC - 1))
        return pm

    def inproj_tm(W, c0, dv, X, dst, dst_res, dst_b0):
        nb_per = 512 // dv
        for g in range(4 // nb_per):
            pm = next_pmm()
            for bb in range(nb_per):
                b = g * nb_per + bb
                for kc in range(NKC):
                    op("pe", lambda e, kc=kc, b=b, bb=bb, pm=pm: e.matmul(pm[:, bb * dv:(bb + 1) * dv], lhsT=X[:, kc, b * 128:(b + 1) * 128],
                                                             rhs=W[:, kc, c0:c0 + dv], start=(kc == 0), stop=(kc == NKC - 1)),
                       reads=[W.r, X.r], writes=[pm.r], inc=(kc == NKC - 1 and bb == nb_per - 1))
            b0 = dst_b0 + g * nb_per
            op("act", lambda e, pm=pm, b0=b0: e.copy(out=dst[:, b0:b0 + nb_per, 0:dv], in_=pm[:, :].rearrange("p (b v) -> p b v", v=dv)),
               reads=[pm.r], writes=[dst_res])

    def v4(ap):
        return ap.rearrange("p (b c i) -> p b c i", c=2, i=64)

    def v8(ap):
        return ap.rearrange("p (c i) -> p c i", i=64)

    def hg_prep(h, pm_f, qsrc, qres, s, dirn, prescan):
        A, B, C, G, E, Fh, Bc, cd, cb = tA[s], tB[s], tC[s], tG[s], tE[s], tF[s], tBc[s], tcd[s], tcb[s]
        first = 0 if dirn == 0 else 1
        second = 1 - first
        lb_h, oml_h, noml_h = lbT[:, 0, h:h + 1], lbT[:, 1, h:h + 1], lbT[:, 2, h:h + 1]
        op("act", lambda e: e.activation(out=B[:], in_=pm_f[:, :], func=AF.Exp, scale=-1.0), reads=[pm_f.r], writes=[B.r])
        yield
        op("act", lambda e: e.activation(out=B[:], in_=B[:], func=AF.Ln, bias=1.0), reads=[B.r], writes=[B.r])
        op("act", lambda e: e.activation(out=A[:], in_=B[:], func=AF.Exp, scale=-1.0), reads=[B.r], writes=[A.r])
        yield
        op("act", lambda e: e.activation(out=B[:], in_=A[:], func=AF.Ln, scale=oml_h, bias=lb_h), reads=[A.r, lbT.r], writes=[B.r])
        op("dve", lambda e: e.tensor_scalar(out=C[:], in0=A[:], scalar1=noml_h, scalar2=oml_h, op0=ALU.mult, op1=ALU.add),
           reads=[A.r, lbT.r], writes=[C.r])
        yield
        op("dve", lambda e: e.tensor_tensor_scan(out=G[:], data0=ones[:], data1=B[:], initial=0.0, op0=ALU.mult, op1=ALU.add),
           reads=[ones.r, B.r], writes=[G.r])
        yield
        if dirn == 0:
            op("pool", lambda e: e.memset(Bc[:, 0:1], 0.0), writes=[Bc.r])
            op("pool", lambda e: e.tensor_copy(out=Bc[:, 1:8], in_=G[:, 63:511:64]), reads=[G.r], writes=[Bc.r])
            op("dve", lambda e: e.tensor_tensor(out=v8(A[:]), in0=v8(G[:]), in1=bc(Bc[:, 0:8], 64), op=ALU.subtract),
               reads=[G.r, Bc.r], writes=[A.r])
        else:
            op("dve", lambda e: e.tensor_tensor(out=A[:], in0=B[:], in1=G[:], op=ALU.subtract), reads=[B.r, G.r], writes=[A.r])
            op("dve", lambda e: e.tensor_tensor(out=v8(A[:]), in0=v8(A[:]), in1=bc(G[:, 63:512:64], 64), op=ALU.add),
               reads=[A.r, G.r], writes=[A.r])
        yield
        op("act", lambda e: e.activation(out=E[:], in_=A[:], func=AF.Exp), reads=[A.r], writes=[E.r])
        op("act", lambda e: e.activation(out=Fh[:], in_=A[:], func=AF.Exp, scale=-1.0), reads=[A.r], writes=[Fh.r])
        yield
        op("dve", lambda e: e.tensor_tensor(out=C[:], in0=C[:], in1=Fh[:], op=ALU.mult), reads=[C.r, Fh.r], writes=[C.r])
        far = 63 if dirn == 0 else 0
        op("dve", lambda e: e.tensor_copy(out=cd[:, 0:8], in_=E[:, far:512:64]), reads=[E.r], writes=[cd.r])
        cd4 = cd[:, 0:8].rearrange("p (b c) -> p b c", c=2)
        op("dve", lambda e: e.tensor_tensor(out=cb[:, 0:4], in0=cd4[:, :, 0], in1=cd4[:, :, 1], op=ALU.mult), reads=[cd.r], writes=[cb.r])
        yield
        op("dve", lambda e: e.tensor_tensor(out=v8(Fh[:]), in0=v8(C[:]), in1=bc(cd[:, 0:8], 64), op=ALU.mult),
           reads=[C.r, cd.r], writes=[Fh.r])
        yield
        km = KM[s]
        F4, C4, km4 = v4(Fh[:]), v4(C[:]), v4(km[:, 0, :])
        op("dve", lambda e: e.tensor_tensor(out=km4[:, :, first, :], in0=F4[:, :, first, :], in1=bc(cd4[:, :, second], 64), op=ALU.mult),
           reads=[Fh.r, cd.r], writes=[km.r])
        op("act", lambda e: e.copy(out=km4[:, :, second, :], in_=F4[:, :, second, :]), reads=[Fh.r], writes=[km.r])
        yield
        if prescan:
            return
        qt, qm, ka, kb = QT[s], QM[s], KA[s], KB[s]
        op("dve", lambda e: e.tensor_tensor(out=qt[:, 0, :], in0=qsrc, in1=E[:], op=ALU.mult), reads=[qres, E.r], writes=[qt.r])
        op("act", lambda e: e.copy(out=ka[:, 0, :], in_=C[:]), reads=[C.r], writes=[ka.r])
        yield
        kb4, qm4, qt4 = v4(kb[:, 0, :]), v4(qm[:, 0, :]), v4(qt[:, 0, :])
        op("act", lambda e: e.copy(out=kb4[:, :, first, :], in_=F4[:, :, first, :]), reads=[Fh.r], writes=[kb.r])
        op("act", lambda e: e.copy(out=kb4[:, :, second, :], in_=C4[:, :, second, :]), reads=[C.r], writes=[kb.r])
        yield
        op("act", lambda e: e.copy(out=qm4[:, :, first, :], in_=qt4[:, :, first, :]), reads=[qt.r], writes=[qm.r])
        op("dve", lambda e: e.tensor_tensor(out=qm4[:, :, second, :], in0=qt4[:, :, second, :], in1=bc(cd4[:, :, first], 64), op=ALU.mult),
           reads=[qt.r, cd.r], writes=[qm.r])
        yield

    def silu_from_psum(pm, tmp, dst_ap, dst_res):
        op("act", lambda e: e.activation(out=tmp[:], in_=pm[:, :], func=AF.Exp, scale=-1.0), reads=[pm.r], writes=[tmp.r])
        op("act", lambda e: e.activation(out=tmp[:], in_=tmp[:], func=AF.Ln, bias=1.0), reads=[tmp.r], writes=[tmp.r])
        op("act", lambda e: e.activation(out=tmp[:], in_=tmp[:], func=AF.Exp, scale=-1.0), reads=[tmp.r], writes=[tmp.r])
        op("dve", lambda e: e.tensor_tensor(out=dst_ap, in0=pm[:, :], in1=tmp[:], op=ALU.mult), reads=[pm.r, tmp.r], writes=[dst_res])

    def step(g, n):
        if g is None:
            return
        for _ in range(n):
            try:
                next(g)
            except StopIteration:
                return

    def exhaust(g):
        if g is None:
            return
        for _ in g:
            pass

    def pipeline(tiles, make_A, do_B):
        tiles = list(tiles)
        ctxs = [dict() for _ in tiles]
        nx = lambda i: tiles[i + 1] if i + 1 < len(tiles) else None
        exhaust(make_A(tiles[0], ctxs[0], nx(0)))
        for i, j in enumerate(tiles):
            g = make_A(tiles[i + 1], ctxs[i + 1], nx(i + 1)) if i + 1 < len(tiles) else None
            do_B(j, ctxs[i], g)
            exhaust(g)

    def ret_tables(hr):
        lg = math.log(1.0 - 2.0 ** (-5.0 - hr))
        for k in range(10):
            kind = k % 5
            bias = math.log(0.0625) if kind >= 2 else 0.0
            op("act", lambda e, k=k, bias=bias: e.activation(out=rtab[:, k, :], in_=rexpo[:, k, :], func=AF.Exp, scale=lg, bias=bias),
               reads=[rexpo.r], writes=[rtab.r])

    def ret_rot(pmA, pmB, C_, dst, dst_res, s):
        t1, t2 = tA[s], tB[s]
        op("dve", lambda e: e.tensor_tensor(out=t1[:], in0=pmA[:, :], in1=C_[:, 0, :], op=ALU.mult), reads=[pmA.r, C_.r], writes=[t1.r])
        op("dve", lambda e: e.tensor_tensor(out=t2[:], in0=pmB[:, :], in1=C_[:, 1, :], op=ALU.mult), reads=[pmB.r, C_.r], writes=[t2.r])
        op("pool", lambda e: e.tensor_tensor(out=dst[0], in0=t1[:], in1=t2[:], op=ALU.subtract), reads=[t1.r, t2.r], writes=[dst_res])
        t3, t4 = tC[s], tG[s]
        op("dve", lambda e: e.tensor_tensor(out=t3[:], in0=pmA[:, :], in1=C_[:, 1, :], op=ALU.mult), reads=[pmA.r, C_.r], writes=[t3.r])
        op("dve", lambda e: e.tensor_tensor(out=t4[:], in0=pmB[:, :], in1=C_[:, 0, :], op=ALU.mult), reads=[pmB.r, C_.r], writes=[t4.r])
        op("pool", lambda e: e.tensor_tensor(out=dst[1], in0=t3[:], in1=t4[:], op=ALU.add), reads=[t3.r, t4.r], writes=[dst_res])

    def ret_mix(s, dirn, qsrc, qres, ksrc, kres, prescan):
        base = 5 * dirn

        def tb(kind):
            return rtab[:, base + kind, :].unsqueeze(1).to_broadcast([128, 4, 128])

        def b4(ap):
            return ap.rearrange("p (b i) -> p b i", i=128)

        for kc in range(2):
            eng = "dve" if kc == 0 else "pool"
            op(eng, lambda e, kc=kc: e.tensor_tensor(out=b4(KM[s][:, kc, :]), in0=b4(ksrc[kc]), in1=tb(4), op=ALU.mult),
               reads=[kres, rtab.r], writes=[KM[s].r])
            yield
            if prescan:
                continue
            op("dve", lambda e, kc=kc: e.tensor_tensor(out=b4(QT[s][:, kc, :]), in0=b4(qsrc[kc]), in1=tb(0), op=ALU.mult),
               reads=[qres, rtab.r], writes=[QT[s].r])
            op("pool", lambda e, kc=kc: e.tensor_tensor(out=b4(QM[s][:, kc, :]), in0=b4(qsrc[kc]), in1=tb(1), op=ALU.mult),
               reads=[qres, rtab.r], writes=[QM[s].r])
            op("dve", lambda e, kc=kc: e.tensor_tensor(out=b4(KA[s][:, kc, :]), in0=b4(ksrc[kc]), in1=tb(2), op=ALU.mult),
               reads=[kres, rtab.r], writes=[KA[s].r])
            op("pool", lambda e, kc=kc: e.tensor_tensor(out=b4(KB[s][:, kc, :]), in0=b4(ksrc[kc]), in1=tb(3), op=ALU.mult),
               reads=[kres, rtab.r], writes=[KB[s].r])
            yield

    def scan_block(s, b, dirn, nkc, nmc, vap, vres, cdb, mode, oF_slice=None, oF_res=None, osum_t=None):
        dv = nmc * 128
        bl = slice(b * 128, (b + 1) * 128)
        visit["blk"] += 1
        nb = visit["blk"]
        cur = scur["i"]
        Sbf, Sn = Sbfs[cur], Sbfs[1 - cur]
        first = 0 if dirn == 0 else 1
        pt = next_ptr()
        for kc in range(nkc):
            op("pe", lambda e, kc=kc: e.transpose(pt[:, kc, :], KM[s][:, kc, bl], ident[:]), reads=[KM[s].r, ident.r], writes=[pt.r],
               inc=(kc == nkc - 1))
        kt = kTs[nb % 4]
        op("act", lambda e: e.copy(out=kt[:, 0:nkc, :], in_=pt[:, 0:nkc, :]), reads=[pt.r], writes=[kt.r])
        spv = [psS[:, 0:256], psS[:, 256:512]]
        sprl = list(psS.res)
        for kc in range(nkc):
            op("pe", lambda e, kc=kc: e.matmul(spv[kc], lhsT=kt[:, kc, :], rhs=vap, start=True, stop=True),
               reads=[kt.r, vres], writes=sprl, inc=(kc == nkc - 1))
        for kc in range(nkc):
            sc = cdb[kc] if isinstance(cdb, (list, tuple)) else cdb
            op("dve", lambda e, kc=kc, sc=sc: e.scalar_tensor_tensor(out=Sst[:, kc, 0:dv], in0=Sst[:, kc, 0:dv], scalar=sc, in1=spv[kc],
                                                                   op0=ALU.mult, op1=ALU.add),
               reads=[Sst.r] + sprl + ([tcb[s].r] if not isinstance(sc, float) else []), writes=[Sst.r])
        op("act", lambda e: e.copy(out=Sn[:, 0:nkc, 0:dv], in_=Sst[:, 0:nkc, 0:dv]), reads=[Sst.r], writes=[Sn.r])
        scur["i"] = 1 - cur
        if mode == "pre":
            return
        c1 = slice(first * 64, first * 64 + 64)
        c2 = slice((1 - first) * 64, (1 - first) * 64 + 64)
        asl = nb % 4
        at = pat[:, asl * 128:(asl + 1) * 128]
        atr = pat.res[asl]
        for (cols, Ksrc) in ((c1, KA[s]), (c2, KB[s])):
            for kc in range(nkc):
                op("pe", lambda e, cols=cols, Ksrc=Ksrc, kc=kc: e.matmul(
                    at[:, cols], lhsT=Ksrc[:, kc, bl], rhs=QT[s][:, kc, b * 128 + cols.start:b * 128 + cols.stop],
                    start=(kc == 0), stop=(kc == nkc - 1)),
                   reads=[Ksrc.r, QT[s].r], writes=[atr], inc=(kc == nkc - 1))
        am = ATm[nb % 4]
        mk = maskF if dirn == 0 else maskB
        op("dve", lambda e: e.tensor_tensor(out=am[:], in0=at, in1=mk[:], op=ALU.mult), reads=[atr, mk.r], writes=[am.r])
        osl = nb % 2
        ot = po[:, osl * 256:osl * 256 + dv]
        otr = po.res[osl]
        for mc in range(nmc):
            op("pe", lambda e, mc=mc: e.matmul(ot[:, mc * 128:(mc + 1) * 128], lhsT=vap[:, mc * 128:(mc + 1) * 128], rhs=am[:],
                                               start=True, stop=False), reads=[vres, am.r], writes=[otr], inc=False)
            for kc in range(nkc):
                op("pe", lambda e, mc=mc, kc=kc: e.matmul(ot[:, mc * 128:(mc + 1) * 128], lhsT=Sbf[:, kc, mc * 128:(mc + 1) * 128],
                                                          rhs=QM[s][:, kc, bl], start=False, stop=(kc == nkc - 1)),
                   reads=[Sbf.r, QM[s].r], writes=[otr], inc=(kc == nkc - 1 and mc == nmc - 1))
        otv = ot.rearrange("p (m t) -> p m t", t=128)
        if mode == "F":
            op("act", lambda e: e.copy(out=oF_slice, in_=otv), reads=[otr], writes=[oF_res])
        else:
            op("dve", lambda e: e.tensor_tensor(out=osum_t[:, 0:nmc, bl], in0=otv, in1=oF_slice, op=ALU.add),
               reads=[otr, oF_res], writes=[osum_t.r])

    def post_tile(j, nmc, gain_ap, orow0):
        u = j % 2
        os_, sq_, sg_, rs_, oo_ = osum[u], sq[u], sg[u], rstd[u], oOut[u]
        dv = nmc * 128
        op("act", lambda e: e.activation(out=sq_[:, 0:nmc, :], in_=os_[:, 0:nmc, :], func=AF.Square), reads=[os_.r], writes=[sq_.r])
        pm = next_pmm()
        for mc in range(nmc):
            op("pe", lambda e, mc=mc: e.matmul(pm[:, :], lhsT=onesb[:], rhs=sq_[:, mc, :], start=(mc == 0), stop=(mc == nmc - 1)),
               reads=[onesb.r, sq_.r], writes=[pm.r], inc=(mc == nmc - 1))
        op("act", lambda e: e.activation(out=rs_[:], in_=pm[:, :], func=AF.Ln, scale=1.0 / dv, bias=epsr[:, 0:1]),
           reads=[pm.r, epsr.r], writes=[rs_.r])
        op("act", lambda e: e.activation(out=rs_[:], in_=rs_[:], func=AF.Exp, scale=-0.5), reads=[rs_.r], writes=[rs_.r])
        for mc in range(nmc):
            op("dve", lambda e, mc=mc: e.tensor_tensor(out=os_[:, mc, :], in0=os_[:, mc, :], in1=rs_[:], op=ALU.mult),
               reads=[os_.r, rs_.r], writes=[os_.r])
            op("dve", lambda e, mc=mc: e.scalar_tensor_tensor(out=oo_[:, mc, :], in0=os_[:, mc, :], scalar=gain_ap[mc], in1=sg_[:, mc, :],
                                                             op0=ALU.mult, op1=ALU.mult),
               reads=[os_.r, sg_.r, hggT.r, retgT.r], writes=[oo_.r])
        dma("sp", lambda e: e.dma_start(out=oS[orow0:orow0 + dv, j * TT:(j + 1) * TT].rearrange("(m p) t -> p m t", p=128),
                                        in_=oo_[:, 0:nmc, :]), reads=[oo_.r])

    def init_state(src_ap, nkc, dv, src_res=None):
        if src_ap is None:
            op("dve", lambda e: e.memset(Sst[:, 0:nkc, 0:dv], 0.0), writes=[Sst.r])
        else:
            op("dve", lambda e: e.tensor_copy(out=Sst[:, 0:nkc, 0:dv], in_=src_ap), reads=[src_res], writes=[Sst.r])
        Sbf = Sbfs[scur["i"]]
        op("act", lambda e: e.copy(out=Sbf[:, 0:nkc, 0:dv], in_=Sst[:, 0:nkc, 0:dv]), reads=[Sst.r], writes=[Sbf.r])

    def scan_tile_hg(s, order, dirn, vaps, vres, cds, mode, g, oF_slices=None, oF_res=None, osum_t=None):
        first = 0 if dirn == 0 else 1
        c1 = slice(first * 64, first * 64 + 64)
        c2 = slice((1 - first) * 64, (1 - first) * 64 + 64)
        mk = maskF if dirn == 0 else maskB
        for b in order:
            bl = slice(b * 128, (b + 1) * 128)
            if mode != "pre":
                at, atr = pat[:, b * 128:(b + 1) * 128], pat.res[b]
                for (cols, Ksrc) in ((c1, KA[s]), (c2, KB[s])):
                    op("pe", lambda e, cols=cols, Ksrc=Ksrc, at=at, bl=bl, b=b: e.matmul(
                        at[:, cols], lhsT=Ksrc[:, 0, bl], rhs=QT[s][:, 0, b * 128 + cols.start:b * 128 + cols.stop], start=True, stop=True),
                       reads=[Ksrc.r, QT[s].r], writes=[atr], inc=True)
                am = ATm[b]
                op("dve", lambda e, am=am, at=at: e.tensor_tensor(out=am[:], in0=at, in1=mk[:], op=ALU.mult), reads=[atr, mk.r], writes=[am.r])
            pt = next_ptr()
            op("pe", lambda e, pt=pt, bl=bl: e.transpose(pt[:, 0, :], KM[s][:, 0, bl], ident[:]), reads=[KM[s].r, ident.r], writes=[pt.r], inc=True)
            kt = kTs[b]
            op("act", lambda e, kt=kt, pt=pt: e.copy(out=kt[:, 0, :], in_=pt[:, 0, :]), reads=[pt.r], writes=[kt.r])
            op("pe", lambda e, kt=kt, b=b: e.matmul(psS[:, b * 128:(b + 1) * 128], lhsT=kt[:, 0, :], rhs=vaps[b], start=True, stop=True),
               reads=[kt.r, vres], writes=[psS.res[b]], inc=True)
            step(g, 2)
        for b in order:
            bl = slice(b * 128, (b + 1) * 128)
            cur = scur["i"]
            Sb, Sn = Sbfs[cur], Sbfs[1 - cur]
            op("dve", lambda e, b=b: e.scalar_tensor_tensor(out=Sst[:, 0, 0:128], in0=Sst[:, 0, 0:128], scalar=cds[b], in1=psS[:, b * 128:(b + 1) * 128],
                                                           op0=ALU.mult, op1=ALU.add),
               reads=[Sst.r, psS.res[b], tcb[s].r], writes=[Sst.r])
            op("act", lambda e, Sn=Sn: e.copy(out=Sn[:, 0, 0:128], in_=Sst[:, 0, 0:128]), reads=[Sst.r], writes=[Sn.r])
            if mode != "pre":
                osl = b % 2
                ot, otr = po[:, osl * 256:osl * 256 + 128], po.res[osl]
                am = ATm[b]
                op("pe", lambda e, ot=ot, am=am, b=b: e.matmul(ot, lhsT=vaps[b], rhs=am[:], start=True, stop=False),
                   reads=[vres, am.r], writes=[otr], inc=False)
                op("pe", lambda e, ot=ot, Sb=Sb, bl=bl: e.matmul(ot, lhsT=Sb[:, 0, 0:128], rhs=QM[s][:, 0, bl], start=False, stop=True),
                   reads=[Sb.r, QM[s].r], writes=[otr], inc=True)
                if mode == "F":
                    op("act", lambda e, ot=ot, b=b: e.copy(out=oF_slices[b], in_=ot), reads=[otr], writes=[oF_res])
                else:
                    op("dve", lambda e, ot=ot, b=b, bl=bl: e.tensor_tensor(out=osum_t[:, 0, bl], in0=ot, in1=oF_slices[b], op=ALU.add),
                       reads=[otr, oF_res], writes=[osum_t.r])
            scur["i"] = 1 - cur
            step(g, 2)

    NSTEP = 4

    def hg_head(ph, h):
        gbase = NT if ph == 0 else 0
        W = load_w(h * 640, 640)
        prefetch_w()

        def A_fwd(j, ctx, nxt):
            X = load_xT(gbase + j, None if nxt is None else gbase + nxt)
            visit["n"] += 1
            s = visit["n"] % NSET
            ctx["s"], ctx["X"] = s, X
            pmq = inproj_fm(W, 0, X)
            yield
            silu_from_psum(pmq, tE[s], qS[:, 0, j * TT:(j + 1) * TT], qS.res[j])
            yield
            pmf = inproj_fm(W, 128, X)
            yield
            inproj_tm(W, 384, 128, X, vS, vS.res[j], j * 4)
            yield
            yield from hg_prep(h, pmf, qS[:, 0, j * TT:(j + 1) * TT], qS.res[j], s, 0, False)

        def B_fwd(j, ctx, g):
            s = ctx["s"]
            scan_tile_hg(s, list(range(4)), 0, [vS[:, j * 4 + b, 0:128] for b in range(4)], vS.res[j],
                         [tcb[s][:, b:b + 1] for b in range(4)], "F", g,
                         oF_slices=[oF[:, 0, j * TT + b * 128:j * TT + (b + 1) * 128] for b in range(4)], oF_res=oF.res[j])

        def A_bwd(j, ctx, nxt):
            X = load_xT(gbase + j, None if nxt is None else gbase + nxt)
            visit["n"] += 1
            s = visit["n"] % NSET
            ctx["s"], ctx["X"] = s, X
            if ph == 1:
                pmg = inproj_fm(W, 512, X)
                silu_from_psum(pmg, tE[s], sg[j % 2][:, 0, :], sg[j % 2].r)
                yield
            pmf = inproj_fm(W, 256, X)
            yield
            if ph == 0:
                inproj_tm(W, 384, 128, X, vT[s], vT[s].r, 0)
                yield
                yield from hg_prep(h, pmf, None, None, s, 1, True)
            else:
                yield from hg_prep(h, pmf, qS[:, 0, j * TT:(j + 1) * TT], qS.res[j], s, 1, False)

        def B_bwd(j, ctx, g):
            s, X = ctx["s"], ctx["X"]
            cds = [tcb[s][:, b:b + 1] for b in range(4)]
            if ph == 0:
                scan_tile_hg(s, [3, 2, 1, 0], 1, [vT[s][:, b, 0:128] for b in range(4)], vT[s].r, cds, "pre", g)
            else:
                scan_tile_hg(s, [3, 2, 1, 0], 1, [vS[:, j * 4 + b, 0:128] for b in range(4)], vS.res[j], cds, "B", g,
                             oF_slices=[oF[:, 0, j * TT + b * 128:j * TT + (b + 1) * 128] for b in range(4)], oF_res=oF.res[j],
                             osum_t=osum[j % 2])
            if ph == 1:
                post_tile(j, 1, [hggT[:, h:h + 1]], h * 128)

        if ph == 1:
            init_state(None, 1, 128)
            pipeline(range(NT), A_fwd, B_fwd)
            init_state(Sinit_h[:, h, :].unsqueeze(1), 1, 128, Sinit_h.r)
        else:
            init_state(None, 1, 128)
        pipeline(reversed(range(NT)), A_bwd, B_bwd)
        if ph == 0:
            op("dve", lambda e: e.tensor_copy(out=Sinit_h[:, h, :], in_=Sst[:, 0, 0:128]), reads=[Sst.r], writes=[Sinit_h.r])

    def ret_head(ph, hr):
        gbase = NT if ph == 0 else 0
        W = load_w(OFF_RET + hr * 1024, 1024)
        prefetch_w()
        ret_tables(hr)
        gam128 = float((1.0 - 2.0 ** (-5.0 - hr)) ** 128)

        def A_fwd(j, ctx, nxt):
            X = load_xT(gbase + j, None if nxt is None else gbase + nxt)
            C_ = load_cs(gbase + j)
            visit["n"] += 1
            s = visit["n"] % NSET
            ctx["s"], ctx["X"] = s, X
            sl = slice(j * TT, (j + 1) * TT)
            pa, pb = inproj_fm(W, 0, X), inproj_fm(W, 128, X)
            yield
            ret_rot(pa, pb, C_, [qS[:, 0, sl], qS[:, 1, sl]], qS.res[j], s)
            yield
            pa, pb = inproj_fm(W, 256, X), inproj_fm(W, 384, X)
            yield
            kr = krot[s]
            ret_rot(pa, pb, C_, [kr[:, 0, :], kr[:, 1, :]], kr.r, s)
            yield
            inproj_tm(W, 512, 256, X, vS, vS.res[j], j * 4)
            yield
            yield from ret_mix(s, 0, [qS[:, 0, sl], qS[:, 1, sl]], qS.res[j], [kr[:, 0, :], kr[:, 1, :]], kr.r, False)

        def B_fwd(j, ctx, g):
            s = ctx["s"]
            for b in range(4):
                scan_block(s, b, 0, 2, 2, vS[:, j * 4 + b, 0:256], vS.res[j], gam128, "F",
                           oF_slice=oF[:, 0:2, j * TT + b * 128:j * TT + (b + 1) * 128], oF_res=oF.res[j])
                step(g, NSTEP)

        def A_bwd(j, ctx, nxt):
            X = load_xT(gbase + j, None if nxt is None else gbase + nxt)
            C_ = load_cs(gbase + j)
            visit["n"] += 1
            s = visit["n"] % NSET
            ctx["s"], ctx["X"] = s, X
            sl = slice(j * TT, (j + 1) * TT)
            if ph == 1:
                for mc in range(2):
                    pmg = inproj_fm(W, 768 + mc * 128, X)
                    silu_from_psum(pmg, tE[s], sg[j % 2][:, mc, :], sg[j % 2].r)
                yield
            pa, pb = inproj_fm(W, 256, X), inproj_fm(W, 384, X)
            yield
            kr = krot[s]
            ret_rot(pa, pb, C_, [kr[:, 0, :], kr[:, 1, :]], kr.r, s)
            yield
            if ph == 0:
                inproj_tm(W, 512, 256, X, vT[s], vT[s].r, 0)
                yield
                yield from ret_mix(s, 1, None, None, [kr[:, 0, :], kr[:, 1, :]], kr.r, True)
            else:
                yield from ret_mix(s, 1, [qS[:, 0, sl], qS[:, 1, sl]], qS.res[j], [kr[:, 0, :], kr[:, 1, :]], kr.r, False)

        def B_bwd(j, ctx, g):
            s, X = ctx["s"], ctx["X"]
            for b in reversed(range(4)):
                if ph == 0:
                    scan_block(s, b, 1, 2, 2, vT[s][:, b, 0:256], vT[s].r, gam128, "pre")
                else:
                    scan_block(s, b, 1, 2, 2, vS[:, j * 4 + b, 0:256], vS.res[j], gam128, "B",
                               oF_slice=oF[:, 0:2, j * TT + b * 128:j * TT + (b + 1) * 128], oF_res=oF.res[j], osum_t=osum[j % 2])
                step(g, NSTEP)
            if ph == 1:
                post_tile(j, 2, [retgT[:, 2 * hr:2 * hr + 1], retgT[:, 2 * hr + 1:2 * hr + 2]], D + hr * 256)

        if ph == 1:
            init_state(None, 2, 256)
            pipeline(range(NT), A_fwd, B_fwd)
            init_state(Sinit_r[:, hr, :, :], 2, 256, Sinit_r.r)
        else:
            init_state(None, 2, 256)
        pipeline(reversed(range(NT)), A_bwd, B_bwd)
        if ph == 0:
            op("dve", lambda e: e.tensor_copy(out=Sinit_r[:, hr, :, :], in_=Sst[:, 0:2, 0:256]), reads=[Sst.r], writes=[Sinit_r.r])

    for ph in (0, 1):
        for h in range(HG_H):
            hg_head(ph, h)
        for hr in range(RET_H):
            ret_head(ph, hr)

    osync = fence(("sp", "pool"))
    barrier(osync)
    ph_stack.close()

    p23 = ExitStack()
    mmbanks["l"] = [pmm[0], pmm[1], pmm[2], pat, po, psS]
    bgr = S.sb("bgr", [128, 36], F32, stack=p23)
    lng = S.sb("lng", [128, 4, D], F32, stack=p23)
    wgtS = S.sb("wgtS", [128, NB, 32], F32, stack=p23, nres=NB)
    p2 = ExitStack()
    st = p2
    Wg = S.sb("Wg", [128, NKC, 2048], BF16, stack=st)
    Wbh = S.sb("Wbh", [128, NKC, D], BF16, stack=st)
    Wbr = S.sb("Wbr", [128, NKC, D], BF16, stack=st)
    Wo = S.sb("Wo", [128, NKC, D], BF16, stack=st)
    Wgr = S.sb("Wgr", [128, NKC, 36], BF16, stack=st)
    dma("pool", lambda e: e.dma_start(out=Wg[:], in_=win_d[:, OFF_G:OFF_G + 2048].rearrange("(k p) c -> p k c", p=128)), writes=[Wg.r])
    for (Wt, src) in ((Wbh, wbh_d), (Wbr, wbr_d), (Wo, wo_d)):
        dma("pool", lambda e, Wt=Wt, src=src: e.dma_start(out=Wt[:], in_=src.rearrange("(k p) c -> p k c", p=128)), writes=[Wt.r])
    dma("pool", lambda e: e.dma_start(out=Wgr[:], in_=wgr_d.rearrange("(k p) c -> p k c", p=128)), writes=[Wgr.r])
    dma("sp", lambda e: e.dma_start(out=bgr[:], in_=bgr_d[0:1, :].partition_broadcast(128)), writes=[bgr.r])
    for i, src in enumerate((l1g_d, l1b_d, l2g_d, l2b_d)):
        dma("sp", lambda e, i=i, src=src: e.dma_start(out=lng[:, i, :], in_=src[0:1, :].partition_broadcast(128)), writes=[lng.r])

    def layer_norm(z, gi, outt, stt, mv, mul_eng="pool"):
        op("dve", lambda e: e.bn_stats(out=stt[:, 0:6], in_=z[:, 0:512]), reads=[z.r], writes=[stt.r])
        op("dve", lambda e: e.bn_stats(out=stt[:, 6:12], in_=z[:, 512:1024]), reads=[z.r], writes=[stt.r])
        op("dve", lambda e: e.bn_aggr(out=mv[:, 0:2], in_=stt[:, 0:12]), reads=[stt.r], writes=[mv.r])
        op("act", lambda e: e.activation(out=mv[:, 2:3], in_=mv[:, 1:2], func=AF.Sqrt, bias=epsr[:, 1:2]), reads=[mv.r, epsr.r], writes=[mv.r])
        op("dve", lambda e: e.reciprocal(out=mv[:, 3:4], in_=mv[:, 2:3]), reads=[mv.r], writes=[mv.r])
        op("dve", lambda e: e.tensor_scalar(out=z[:], in0=z[:], scalar1=mv[:, 0:1], scalar2=mv[:, 3:4], op0=ALU.subtract, op1=ALU.mult),
           reads=[z.r, mv.r], writes=[z.r])
        op(mul_eng, lambda e: e.tensor_tensor(out=z[:], in0=z[:], in1=lng[:, gi, :], op=ALU.mult), reads=[z.r, lng.r], writes=[z.r])
        op("dve", lambda e: e.tensor_tensor(out=outt[:], in0=z[:], in1=lng[:, gi + 1, :], op=ALU.add), reads=[z.r, lng.r], writes=[outt.r])

    with ExitStack() as st2:
        xT2 = [S.sb(f"xT2{i}", [128, NKC, TT], BF16, stack=st2) for i in range(1)] * 2
        oSt = [S.sb(f"oSt{i}", [128, 16, TT], BF16, stack=st2) for i in range(1)] * 2
        xtok = [S.sb(f"xtok{i}", [128, D], F32, stack=st2) for i in range(2)]
        sga = [S.sb(f"sga{i}", [128, TT], F32, stack=st2) for i in range(2)]
        sgb = [S.sb(f"sgb{i}", [128, TT], F32, stack=st2) for i in range(2)]
        mg = [S.sb(f"mg{i}", [128, NKC, TT], BF16, stack=st2) for i in range(1)] * 2
        zt = [S.sb(f"zt{i}", [128, D], F32, stack=st2) for i in range(2)]
        x1t = [S.sb(f"x1t{i}", [128, D], F32, stack=st2) for i in range(2)]
        x1b = [S.sb(f"x1b{i}", [128, D], BF16, stack=st2) for i in range(2)]
        x1T = [S.sb(f"x1T{i}", [128, NKC, TT], BF16, stack=st2) for i in range(1)] * 2
        stt = [S.sb(f"stt{i}", [128, 12], F32, stack=st2) for i in range(2)]
        mv = [S.sb(f"mv{i}", [128, 4], F32, stack=st2) for i in range(2)]
        rt = [S.sb(f"rt{i}", [128, 96], F32, stack=st2) for i in range(2)]
        nblk2 = 0
        nonlocal_state = {"n": 0}
        for j in range(NT):
            X, O_, M_, XT1 = xT2[j % 2], oSt[j % 2], mg[j % 2], x1T[j % 2]
            dma("sp", lambda e, X=X, j=j: e.dma_start(out=X[:], in_=xTd[j].rearrange("p (k t) -> p k t", t=TT)), reads=[xsync], writes=[X.r])
            dma("sp", lambda e, O_=O_, j=j: e.dma_start(out=O_[:], in_=oS[:, j * TT:(j + 1) * TT].rearrange("(c p) t -> p c t", p=128)),
                reads=[osync], writes=[O_.r])
            for dc in range(NKC):
                u = dc % 2
                pga = inproj_fm(Wg, dc * 128, X)
                op("act", lambda e, pga=pga, u=u: e.activation(out=sga[u][:], in_=pga[:, :], func=AF.Sigmoid), reads=[pga.r], writes=[sga[u].r])
                pgb = inproj_fm(Wg, D + dc * 128, X)
                op("act", lambda e, pgb=pgb, u=u: e.activation(out=sgb[u][:], in_=pgb[:, :], func=AF.Sigmoid), reads=[pgb.r], writes=[sgb[u].r])
                pyh = next_pmm()
                for kc in range(NKC):
                    op("pe", lambda e, pyh=pyh, kc=kc, dc=dc: e.matmul(pyh[:, :], lhsT=Wbh[:, kc, dc * 128:(dc + 1) * 128], rhs=O_[:, kc, :],
                                                                      start=(kc == 0), stop=(kc == NKC - 1)),
                       reads=[Wbh.r, O_.r], writes=[pyh.r], inc=(kc == NKC - 1))
                op("dve", lambda e, pyh=pyh, u=u: e.tensor_tensor(out=sga[u][:], in0=pyh[:, :], in1=sga[u][:], op=ALU.mult),
                   reads=[pyh.r, sga[u].r], writes=[sga[u].r])
                pyr = next_pmm()
                for kc in range(NKC):
                    op("pe", lambda e, pyr=pyr, kc=kc, dc=dc: e.matmul(pyr[:, :], lhsT=Wbr[:, kc, dc * 128:(dc + 1) * 128], rhs=O_[:, 8 + kc, :],
                                                                      start=(kc == 0), stop=(kc == NKC - 1)),
                       reads=[Wbr.r, O_.r], writes=[pyr.r], inc=(kc == NKC - 1))
                op("dve", lambda e, pyr=pyr, u=u: e.tensor_tensor(out=sgb[u][:], in0=pyr[:, :], in1=sgb[u][:], op=ALU.mult),
                   reads=[pyr.r, sgb[u].r], writes=[sgb[u].r])
                op("pool", lambda e, u=u, dc=dc: e.tensor_tensor(out=M_[:, dc, :], in0=sga[u][:], in1=sgb[u][:], op=ALU.add),
                   reads=[sga[u].r, sgb[u].r], writes=[M_.r])
            def part1(b):
                nonlocal_state["n"] += 1
                u = nonlocal_state["n"] % 2
                gb = j * 4 + b
                xk, z_, x1_, x1b_ = xtok[u], zt[u], x1t[u], x1b[u]
                dma("sp", lambda e, xk=xk, gb=gb: e.dma_start(out=xk[:], in_=x_d[gb * 128:(gb + 1) * 128, :]), writes=[xk.r])
                for hf in range(2):
                    pm = next_pmm()
                    for kc in range(NKC):
                        op("pe", lambda e, pm=pm, kc=kc, hf=hf, b=b: e.matmul(pm[:, :], lhsT=M_[:, kc, b * 128:(b + 1) * 128],
                                                                             rhs=Wo[:, kc, hf * 512:(hf + 1) * 512], start=(kc == 0), stop=(kc == NKC - 1)),
                           reads=[M_.r, Wo.r], writes=[pm.r], inc=(kc == NKC - 1))
                    op("dve", lambda e, pm=pm, hf=hf, xk=xk, z_=z_: e.scalar_tensor_tensor(out=z_[:, hf * 512:(hf + 1) * 512], in0=xk[:, hf * 512:(hf + 1) * 512],
                                                                                         scalar=ALPHA, in1=pm[:, :], op0=ALU.mult, op1=ALU.add),
                       reads=[pm.r, xk.r], writes=[z_.r])
                layer_norm(z_, 0, x1_, stt[u], mv[u])
                dma("sp", lambda e, x1_=x1_, gb=gb: e.dma_start(out=x1f[gb * 128:(gb + 1) * 128, :], in_=x1_[:]), reads=[x1_.r])
                op("act", lambda e, x1_=x1_, x1b_=x1b_: e.copy(out=x1b_[:], in_=x1_[:]), reads=[x1_.r], writes=[x1b_.r])
                dma("sp", lambda e, x1b_=x1b_, gb=gb: e.dma_start(out=x1bd[gb * 128:(gb + 1) * 128, :], in_=x1b_[:]), reads=[x1b_.r])

                return u, gb

            def part2(b, u, gb):
                x1b_ = x1b[u]
                pt = next_ptr()
                for kc in range(NKC):
                    op("pe", lambda e, pt=pt, kc=kc, x1b_=x1b_: e.transpose(pt[:, kc, :], x1b_[:, kc * 128:(kc + 1) * 128], ident[:]),
                       reads=[x1b_.r, ident.r], writes=[pt.r], inc=(kc == NKC - 1))
                op("act", lambda e, pt=pt, b=b: e.copy(out=XT1[:, :, b * 128:(b + 1) * 128], in_=pt[:]), reads=[pt.r], writes=[XT1.r])
                pl = next_pmm()
                for kc in range(NKC):
                    op("pe", lambda e, pl=pl, kc=kc, b=b: e.matmul(pl[:, 0:36], lhsT=XT1[:, kc, b * 128:(b + 1) * 128], rhs=Wgr[:, kc, :],
                                                                  start=(kc == 0), stop=(kc == NKC - 1)),
                       reads=[XT1.r, Wgr.r], writes=[pl.r], inc=(kc == NKC - 1))
                R = rt[u]
                rr_ = [R.r]
                op("dve", lambda e, pl=pl, R=R: e.tensor_tensor(out=R[:, 0:36], in0=pl[:, 0:36], in1=bgr[:], op=ALU.add), reads=[pl.r, bgr.r], writes=rr_)
                op("dve", lambda e, R=R: e.reduce_max(out=R[:, 36:37], in_=R[:, 0:4], axis=mybir.AxisListType.X), reads=rr_, writes=rr_)
                op("dve", lambda e, R=R: e.tensor_scalar(out=R[:, 40:44], in0=R[:, 0:4], scalar1=R[:, 36:37], scalar2=None, op0=ALU.is_equal), reads=rr_, writes=rr_)
                op("dve", lambda e, R=R: e.tensor_scalar(out=R[:, 37:38], in0=R[:, 36:37], scalar1=-1.0, scalar2=None, op0=ALU.mult), reads=rr_, writes=rr_)
                op("act", lambda e, R=R: e.activation(out=R[:, 44:48], in_=R[:, 0:4], func=AF.Exp, bias=R[:, 37:38]), reads=rr_, writes=rr_)
                op("dve", lambda e, R=R: e.reduce_sum(out=R[:, 38:39], in_=R[:, 44:48], axis=mybir.AxisListType.X), reads=rr_, writes=rr_)
                op("dve", lambda e, R=R: e.reciprocal(out=R[:, 39:40], in_=R[:, 38:39]), reads=rr_, writes=rr_)
                op("dve", lambda e, R=R: e.tensor_scalar(out=R[:, 48:56], in0=R[:, 4:12], scalar1=R[:, 40:41], scalar2=None, op0=ALU.mult), reads=rr_, writes=rr_)
                for g in range(1, 4):
                    op("dve", lambda e, R=R, g=g: e.scalar_tensor_tensor(out=R[:, 48:56], in0=R[:, 4 + 8 * g:12 + 8 * g], scalar=R[:, 40 + g:41 + g],
                                                                         in1=R[:, 48:56], op0=ALU.mult, op1=ALU.add), reads=rr_, writes=rr_)
                op("dve", lambda e, R=R: e.max(out=R[:, 56:64], in_=R[:, 48:56]), reads=rr_, writes=rr_)
                op("dve", lambda e, R=R: e.tensor_scalar(out=R[:, 64:72], in0=R[:, 48:56], scalar1=R[:, 56:57], scalar2=None, op0=ALU.is_equal), reads=rr_, writes=rr_)
                op("dve", lambda e, R=R: e.tensor_scalar(out=R[:, 72:80], in0=R[:, 48:56], scalar1=R[:, 57:58], scalar2=None, op0=ALU.is_equal), reads=rr_, writes=rr_)
                op("dve", lambda e, R=R: e.tensor_tensor(out=R[:, 80:81], in0=R[:, 57:58], in1=R[:, 56:57], op=ALU.subtract), reads=rr_, writes=rr_)
                op("act", lambda e, R=R: e.activation(out=R[:, 81:82], in_=R[:, 80:81], func=AF.Exp), reads=rr_, writes=rr_)
                op("dve", lambda e, R=R: e.tensor_scalar(out=R[:, 82:83], in0=R[:, 81:82], scalar1=1.0, scalar2=None, op0=ALU.add), reads=rr_, writes=rr_)
                op("dve", lambda e, R=R: e.reciprocal(out=R[:, 83:84], in_=R[:, 82:83]), reads=rr_, writes=rr_)
                op("dve", lambda e, R=R: e.tensor_tensor(out=R[:, 84:85], in0=R[:, 81:82], in1=R[:, 83:84], op=ALU.mult), reads=rr_, writes=rr_)
                op("dve", lambda e, R=R: e.tensor_scalar(out=R[:, 85:87], in0=R[:, 83:85], scalar1=R[:, 39:40], scalar2=None, op0=ALU.mult), reads=rr_, writes=rr_)
                op("dve", lambda e, R=R: e.tensor_scalar(out=R[:, 88:96], in0=R[:, 64:72], scalar1=R[:, 85:86], scalar2=None, op0=ALU.mult), reads=rr_, writes=rr_)
                op("dve", lambda e, R=R: e.scalar_tensor_tensor(out=R[:, 88:96], in0=R[:, 72:80], scalar=R[:, 86:87], in1=R[:, 88:96],
                                                                op0=ALU.mult, op1=ALU.add), reads=rr_, writes=rr_)
                for g in range(4):
                    op("dve", lambda e, R=R, g=g, gb=gb: e.tensor_scalar(out=wgtS[:, gb, g * 8:(g + 1) * 8], in0=R[:, 88:96], scalar1=R[:, 40 + g:41 + g],
                                                                         scalar2=None, op0=ALU.mult), reads=rr_, writes=[wgtS.res[gb]])


            prev = None
            for b in range(4):
                cur_ = part1(b)
                if prev is not None:
                    part2(b - 1, *prev)
                prev = cur_
            part2(3, *prev)
    msync = fence(("sp", "pool"))
    barrier(msync)
    p2.close()

    precast_step(len(pc_jobs))
    with ExitStack() as st3:
        Uup = S.sb("Uup", [128, 128], BF16, stack=st3)
        Mb = S.sb("Mb", [128, NB, NEXP], BF16, stack=st3)
        Mf = S.sb("Mf", [128, NB, NEXP], F32, stack=st3)
        rankS = S.sb("rankS", [128, NB, NEXP], F32, stack=st3)
        Dm = S.sb("Dm", [128, NB, NEXP], F32, stack=st3)
        Eq = S.sb("Eq", [128, NB, NEXP], F32, stack=st3)
        cntS = S.sb("cntS", [128, NEXP], F32, stack=st3)
        thr = S.sb("thr", [128, NEXP], F32, stack=st3)
        thri = S.sb("thri", [128, NEXP], I32, stack=st3)
        cmp1 = S.sb("cmp1", [128, NEXP, NEXP], F32, stack=st3)
        nblk = S.sb("nblk", [128, NEXP], F32, stack=st3)
        pend = S.sb("pend", [128, NEXP], F32, stack=st3)
        pstr = S.sb("pstr", [128, NEXP], F32, stack=st3)
        bidx = S.sb("bidx", [128, NBLK], F32, stack=st3)
        bidxi = S.sb("bidxi", [128, NBLK], I32, stack=st3)
        cmp2 = S.sb("cmp2", [128, NBLK, NEXP], F32, stack=st3)
        bef = S.sb("bef", [128, NBLK], F32, stack=st3)
        pidx = S.sb("pidx", [128, 1], F32, stack=st3)
        pidxi = S.sb("pidxi", [128, 1], I32, stack=st3)
        idxW = S.sb("idxW", [128, NBLK], I32, stack=st3)
        dBf = S.sb("dBf", [128, NB], F32, stack=st3)
        dAf = S.sb("dAf", [128, NB], F32, stack=st3)
        wBf = S.sb("wBf", [128, NB], F32, stack=st3)
        wAf = S.sb("wAf", [128, NB], F32, stack=st3)
        dAi = S.sb("dAi", [128, NB], I32, stack=st3)
        dBi = S.sb("dBi", [128, NB], I32, stack=st3)
        zero = S.sb("zero", [128, 4, D], BF16, stack=st3)
        xblk = [S.sb(f"xblk{i}", [128, D], BF16, stack=st3) for i in range(2)]
        Wt = [S.sb(f"Wt{i}", [128, 12288], BF16, stack=st3) for i in range(3)]
        xsb = [S.sb(f"xsb{i}", [128, D], BF16, stack=st3) for i in range(2)]
        xsT = [S.sb(f"xsT{i}", [128, NKC, 128], BF16, stack=st3) for i in range(2)]
        sl_ = [S.sb(f"sl{i}", [128, 4, 128], F32, stack=st3) for i in range(2)]
        gT = [S.sb(f"gT{i}", [128, 4, 128], BF16, stack=st3) for i in range(2)]
        ysb = [S.sb(f"ysb{i}", [128, D], F32, stack=st3) for i in range(2)]
        yA = [S.sb(f"yA{i}", [128, D], F32, stack=st3) for i in range(3)]
        yB = [S.sb(f"yB{i}", [128, D], F32, stack=st3) for i in range(3)]
        x1l = [S.sb(f"x1l{i}", [128, D], F32, stack=st3) for i in range(3)]
        stt3 = [S.sb(f"stt3{i}", [128, 12], F32, stack=st3) for i in range(4)]
        mv3 = [S.sb(f"mv3{i}", [128, 4], F32, stack=st3) for i in range(4)]
        wres = [wgtS.res[b] for b in range(NB)]
        op("pool", lambda e: e.memset(Uup[:], 1.0), writes=[Uup.r])
        op("pool", lambda e: e.affine_select(out=Uup[:], in_=Uup[:], pattern=[[1, 128]], compare_op=ALU.is_ge, fill=0.0, base=-1,
                                             channel_multiplier=-1), reads=[Uup.r], writes=[Uup.r])
        op("pool", lambda e: e.iota(thri[:], pattern=[[128, NEXP]], base=0, channel_multiplier=0), writes=[thri.r])
        op("dve", lambda e: e.tensor_copy(out=thr[:], in_=thri[:]), reads=[thri.r], writes=[thr.r])
        op("pool", lambda e: e.iota(bidxi[:], pattern=[[1, NBLK]], base=0, channel_multiplier=0), writes=[bidxi.r])
        op("dve", lambda e: e.tensor_copy(out=bidx[:], in_=bidxi[:]), reads=[bidxi.r], writes=[bidx.r])
        op("pool", lambda e: e.iota(pidxi[:], pattern=[[0, 1]], base=0, channel_multiplier=1), writes=[pidxi.r])
        op("dve", lambda e: e.tensor_copy(out=pidx[:], in_=pidxi[:]), reads=[pidxi.r], writes=[pidx.r])
        op("pool", lambda e: e.memset(zero[:], 0.0), writes=[zero.r])
        ztoks = []
        for i0 in range(0, NBLK, 4):
            ztoks.append(dma("sp", lambda e, i0=i0: e.dma_start(out=xs_d[i0 * 128:(i0 + 4) * 128, :].rearrange("(i p) d -> p i d", p=128), in_=zero[:]),
                             reads=[zero.r]))
        flat = lambda t: t[:].rearrange("p b e -> p (b e)")
        op("dve", lambda e: e.tensor_scalar(out=flat(Mb), in0=flat(wgtS), scalar1=0.0, scalar2=None, op0=ALU.is_gt), reads=wres, writes=[Mb.r])
        op("dve", lambda e: e.tensor_scalar(out=flat(Mf), in0=flat(wgtS), scalar1=0.0, scalar2=None, op0=ALU.is_gt), reads=wres, writes=[Mf.r])
        for b in range(NB):
            pm = next_pmm()
            for b2 in range(b):
                op("pe", lambda e, pm=pm, b2=b2: e.matmul(pm[:, 0:NEXP], lhsT=onesb[:], rhs=Mb[:, b2, :], start=(b2 == 0), stop=False),
                   reads=[onesb.r, Mb.r], writes=[pm.r], inc=False)
            op("pe", lambda e, pm=pm, b=b: e.matmul(pm[:, 0:NEXP], lhsT=Uup[:], rhs=Mb[:, b, :], start=(b == 0), stop=True),
               reads=[Uup.r, Mb.r], writes=[pm.r], inc=True)
            op("act", lambda e, pm=pm, b=b: e.copy(out=rankS[:, b, :], in_=pm[:, 0:NEXP]), reads=[pm.r], writes=[rankS.r])
        pm = next_pmm()
        for b2 in range(NB):
            op("pe", lambda e, pm=pm, b2=b2: e.matmul(pm[:, 0:NEXP], lhsT=onesb[:], rhs=Mb[:, b2, :], start=(b2 == 0), stop=(b2 == NB - 1)),
               reads=[onesb.r, Mb.r], writes=[pm.r], inc=(b2 == NB - 1))
        op("act", lambda e, pm=pm: e.copy(out=cntS[:], in_=pm[:, 0:NEXP]), reads=[pm.r], writes=[cntS.r])
        op("dve", lambda e: e.tensor_tensor(out=cmp1[:], in0=bc(cntS[:], NEXP), in1=thr[:].unsqueeze(1).to_broadcast([128, NEXP, NEXP]), op=ALU.is_gt),
           reads=[cntS.r, thr.r], writes=[cmp1.r])
        op("dve", lambda e: e.reduce_sum(out=nblk[:], in_=cmp1[:], axis=mybir.AxisListType.X), reads=[cmp1.r], writes=[nblk.r])
        op("dve", lambda e: e.tensor_tensor_scan(out=pend[:], data0=ones[:, 0:NEXP], data1=nblk[:], initial=0.0, op0=ALU.mult, op1=ALU.add),
           reads=[ones.r, nblk.r], writes=[pend.r])
        op("dve", lambda e: e.tensor_tensor(out=pstr[:], in0=pend[:], in1=nblk[:], op=ALU.subtract), reads=[pend.r, nblk.r], writes=[pstr.r])
        op("dve", lambda e: e.tensor_scalar(out=pstr[:], in0=pstr[:], scalar1=128.0, scalar2=None, op0=ALU.mult), reads=[pstr.r], writes=[pstr.r])
        op("dve", lambda e: e.tensor_tensor(out=cmp2[:], in0=pend[:].unsqueeze(1).to_broadcast([128, NBLK, NEXP]), in1=bc(bidx[:], NEXP), op=ALU.is_le),
           reads=[pend.r, bidx.r], writes=[cmp2.r])
        op("dve", lambda e: e.reduce_sum(out=bef[:], in_=cmp2[:], axis=mybir.AxisListType.X), reads=[cmp2.r], writes=[bef.r])
        op("dve", lambda e: e.tensor_scalar(out=bef[:], in0=bef[:], scalar1=float(NEXP - 1), scalar2=128.0, op0=ALU.min, op1=ALU.mult), reads=[bef.r], writes=[bef.r])
        op("dve", lambda e: e.tensor_scalar(out=idxW[:], in0=bef[:], scalar1=pidx[:, 0:1], scalar2=None, op0=ALU.add), reads=[bef.r, pidx.r], writes=[idxW.r])
        op("dve", lambda e: e.tensor_tensor(out=Dm[:], in0=rankS[:], in1=pstr[:].unsqueeze(1).to_broadcast([128, NB, NEXP]), op=ALU.add),
           reads=[rankS.r, pstr.r], writes=[Dm.r])
        op("dve", lambda e: e.tensor_tensor(out=Dm[:], in0=Dm[:], in1=Mf[:], op=ALU.mult), reads=[Dm.r, Mf.r], writes=[Dm.r])
        op("dve", lambda e: e.reduce_max(out=dBf[:], in_=Dm[:], axis=mybir.AxisListType.X), reads=[Dm.r], writes=[dBf.r])
        op("dve", lambda e: e.reduce_sum(out=dAf[:], in_=Dm[:], axis=mybir.AxisListType.X), reads=[Dm.r], writes=[dAf.r])
        op("dve", lambda e: e.tensor_tensor(out=dAf[:], in0=dAf[:], in1=dBf[:], op=ALU.subtract), reads=[dAf.r, dBf.r], writes=[dAf.r])
        op("dve", lambda e: e.tensor_tensor(out=Eq[:], in0=Dm[:], in1=bc(dBf[:], NEXP), op=ALU.is_equal), reads=[Dm.r, dBf.r], writes=[Eq.r])
        op("dve", lambda e: e.tensor_tensor(out=flat(Eq), in0=flat(Eq), in1=flat(wgtS), op=ALU.mult), reads=[Eq.r] + wres, writes=[Eq.r])
        op("dve", lambda e: e.reduce_sum(out=wBf[:], in_=Eq[:], axis=mybir.AxisListType.X), reads=[Eq.r], writes=[wBf.r])
        op("dve", lambda e: e.reduce_sum(out=wAf[:], in_=wgtS[:], axis=mybir.AxisListType.X), reads=wres, writes=[wAf.r])
        op("dve", lambda e: e.tensor_tensor(out=wAf[:], in0=wAf[:], in1=wBf[:], op=ALU.subtract), reads=[wAf.r, wBf.r], writes=[wAf.r])
        op("dve", lambda e: e.tensor_copy(out=dAi[:], in_=dAf[:]), reads=[dAf.r], writes=[dAi.r])
        op("dve", lambda e: e.tensor_copy(out=dBi[:], in_=dBf[:]), reads=[dBf.r], writes=[dBi.r])
        zres = Res()
        S.wait_all("pool", ztoks[-S.NDS:])
        zres.w = ("pool", S.cnt["pool"])
        stoks = []
        for b in range(NB):
            xb_ = xblk[b % 2]
            dma("sp", lambda e, xb_=xb_, b=b: e.dma_start(out=xb_[:], in_=x1bd[b * 128:(b + 1) * 128, :]), reads=[msync], writes=[xb_.r])
            for di in (dAi, dBi):
                stoks.append(dma("pool", lambda e, xb_=xb_, b=b, di=di: e.indirect_dma_start(
                    out=xs_d[:, :], out_offset=bass.IndirectOffsetOnAxis(ap=di[:, b:b + 1], axis=0), in_=xb_[:, :], in_offset=None),
                    reads=[xb_.r, di.r, zres]))
        sres = Res()
        S.wait_all("sp", stoks[-S.NDS:])
        sres.w = ("sp", S.cnt["sp"])
        banks = [pmm[0], pmm[1], pmm[2], pat, po, psS]
        bstate = {"i": 0}

        def next_bank():
            bstate["i"] += 1
            return banks[bstate["i"] % len(banks)]

        ytoks = []

        def stage1(i):
            u = i % 2
            W_, xs_, xT_, s_, g_ = Wt[i % 3], xsb[u], xsT[u], sl_[u], gT[u]
            dma("pool", lambda e: e.indirect_dma_start(out=W_[:, :], out_offset=None, in_=WS[:, :],
                                                       in_offset=bass.IndirectOffsetOnAxis(ap=idxW[:, i:i + 1], axis=0)),
                reads=[idxW.r, msync], writes=[W_.r])
            dma("sp", lambda e: e.dma_start(out=xs_[:], in_=xs_d[i * 128:(i + 1) * 128, :]), reads=[sres], writes=[xs_.r])
            pt = next_ptr()
            for kc in range(NKC):
                op("pe", lambda e, kc=kc: e.transpose(pt[:, kc, :], xs_[:, kc * 128:(kc + 1) * 128], ident[:]),
                   reads=[xs_.r, ident.r], writes=[pt.r], inc=(kc == NKC - 1))
            op("act", lambda e: e.copy(out=xT_[:], in_=pt[:]), reads=[pt.r], writes=[xT_.r])
            W1 = W_[:, 0:4096].rearrange("p (k f) -> p k f", f=DEXP)
            W3 = W_[:, 4096:8192].rearrange("p (k f) -> p k f", f=DEXP)
            p1, p3 = next_bank(), next_bank()
            for (pp, Wm) in ((p1, W1), (p3, W3)):
                for fc in range(4):
                    for kc in range(NKC):
                        op("pe", lambda e, pp=pp, Wm=Wm, fc=fc, kc=kc: e.matmul(pp[:, fc * 128:(fc + 1) * 128], lhsT=Wm[:, kc, fc * 128:(fc + 1) * 128],
                                                                              rhs=xT_[:, kc, :], start=(kc == 0), stop=(kc == NKC - 1)),
                           reads=[W_.r, xT_.r], writes=[pp.r], inc=(kc == NKC - 1 and fc == 3))
            op("act", lambda e: e.activation(out=s_[:].rearrange("p a b -> p (a b)"), in_=p1[:, :], func=AF.Silu), reads=[p1.r], writes=[s_.r])
            op("dve", lambda e: e.tensor_tensor(out=g_[:].rearrange("p a b -> p (a b)"), in0=p3[:, :], in1=s_[:].rearrange("p a b -> p (a b)"), op=ALU.mult),
               reads=[p3.r, s_.r], writes=[g_.r])

        def stage2(i):
            u = i % 2
            W_, g_, y_ = Wt[i % 3], gT[u], ysb[u]
            W2 = W_[:, 8192:12288].rearrange("p (k f) -> p k f", f=D)
            for h2 in range(2):
                py = next_bank()
                for fc in range(4):
                    op("pe", lambda e, py=py, fc=fc, h2=h2: e.matmul(py[:, :], lhsT=g_[:, fc, :], rhs=W2[:, fc, h2 * 512:(h2 + 1) * 512],
                                                                   start=(fc == 0), stop=(fc == 3)),
                       reads=[g_.r, W_.r], writes=[py.r], inc=(fc == 3))
                if h2 == 0:
                    op("act", lambda e, py=py: e.copy(out=y_[:, 0:512], in_=py[:, :]), reads=[py.r], writes=[y_.r])
                else:
                    op("dve", lambda e, py=py: e.tensor_copy(out=y_[:, 512:1024], in_=py[:, :]), reads=[py.r], writes=[y_.r])
            ytoks.append(dma("sp", lambda e: e.dma_start(out=ys_d[i * 128:(i + 1) * 128, :], in_=y_[:]), reads=[y_.r]))

        stage1(0)
        for i in range(NBLK):
            if i + 1 < NBLK:
                stage1(i + 1)
            stage2(i)
        yres = Res()
        S.wait_all("pool", ytoks[-S.NDS:])
        yres.w = ("pool", S.cnt["pool"])
        for b in range(NB):
            u = b % 3
            dma("sp", lambda e, u=u, b=b: e.dma_start(out=x1l[u][:], in_=x1f[b * 128:(b + 1) * 128, :]), reads=[msync], writes=[x1l[u].r])
            for (yt, di) in ((yA[u], dAi), (yB[u], dBi)):
                dma("pool", lambda e, yt=yt, di=di, b=b: e.indirect_dma_start(out=yt[:, :], out_offset=None, in_=ys_d[:, :],
                                                                            in_offset=bass.IndirectOffsetOnAxis(ap=di[:, b:b + 1], axis=0)),
                    reads=[di.r, yres], writes=[yt.r])
            z_ = x1l[u]
            op("dve", lambda e, u=u, b=b, z_=z_: e.scalar_tensor_tensor(out=yA[u][:], in0=yA[u][:], scalar=wAf[:, b:b + 1], in1=yB[u][:], op0=ALU.mult, op1=ALU.bypass)
               if False else e.tensor_scalar(out=yA[u][:], in0=yA[u][:], scalar1=wAf[:, b:b + 1], scalar2=None, op0=ALU.mult),
               reads=[yA[u].r, wAf.r], writes=[yA[u].r])
            op("dve", lambda e, u=u, b=b: e.scalar_tensor_tensor(out=yA[u][:], in0=yB[u][:], scalar=wBf[:, b:b + 1], in1=yA[u][:], op0=ALU.mult, op1=ALU.add),
               reads=[yA[u].r, yB[u].r, wBf.r], writes=[yA[u].r])
            op("dve", lambda e, u=u, z_=z_: e.scalar_tensor_tensor(out=z_[:], in0=z_[:], scalar=ALPHA, in1=yA[u][:], op0=ALU.mult, op1=ALU.add),
               reads=[z_.r, yA[u].r], writes=[z_.r])
            layer_norm(z_, 2, yB[u], stt3[u], mv3[u], mul_eng="dve")
            dma("sp", lambda e, u=u, b=b: e.dma_start(out=out_d[b * 128:(b + 1) * 128, :], in_=yB[u][:]), reads=[yB[u].r])
        fence(("sp", "pool"))
    p23.close()
    S.emit()
    S.stack.close()
    return nc


def _ret_expo():
    i = np.arange(128)
    ii = i % 64
    c = i // 64
    f = np.zeros((5, 128), np.float64)
    f[0] = ii + 1
    f[1] = i + 1
    f[2] = -(ii + 1)
    f[3] = np.where(c == 0, 63 - ii, -(ii + 1))
    f[4] = 127 - i
    b = f[:, ::-1]
    tab = np.concatenate([f, b], 0).astype(np.float32)
    return np.ascontiguousarray(np.broadcast_to(tab.reshape(1, 1280), (128, 1280)))


def _win_perm(swap):
    K = 1024
    off = {n: i * K for i, n in enumerate(["hq", "hff", "hfb", "hi", "hg", "rq", "rk", "rv", "rg", "ga", "gb"])}
    ff, fb = ("hfb", "hff") if swap else ("hff", "hfb")
    cols = []
    for h in range(HG_H):
        for n in ("hq", ff, fb, "hi", "hg"):
            cols.append(off[n] + h * 128 + np.arange(128))
    for r in range(RET_H):
        perm = np.concatenate([np.arange(0, 256, 2), np.arange(1, 256, 2)])
        cols.append(off["rq"] + r * 256 + perm)
        cols.append(off["rk"] + r * 256 + perm)
        cols.append(off["rv"] + r * 256 + np.arange(256))
        cols.append(off["rg"] + r * 256 + np.arange(256))
    cols.append(off["ga"] + np.arange(K))
    cols.append(off["gb"] + np.arange(K))
    return np.concatenate(cols)


_NC_CACHE = {}


def kernel(x, positions, w_in, hg_lb_logits, hg_norm_g, ret_norm_g, w_branch_hg, w_branch_ret, w_out, ln1_g, ln1_b,
           w_group, b_group, w_router, b_router, w1, w3, w2, ln2_g, ln2_b, _debug=False):
    x = np.asarray(x, np.float32)
    B, L, _ = x.shape
    T = L // 2
    ncores = 2 * B
    key = (T, _debug)
    if key not in _NC_CACHE:
        _NC_CACHE[key] = build(T, _debug)
    nc = _NC_CACHE[key]
    positions = np.asarray(positions, np.int32)
    w_in0 = np.asarray(w_in, np.float32)[0]
    wins = [np.ascontiguousarray(w_in0[:, _win_perm(False)]), np.ascontiguousarray(w_in0[:, _win_perm(True)])]
    invf = (1.0 / (np.float32(10000.0) ** np.linspace(0.0, 1.0, 128, dtype=np.float32))).astype(np.float32).reshape(128, 1)
    f32 = lambda a: np.ascontiguousarray(np.asarray(a, np.float32))
    common = {
        "lbl": f32(hg_lb_logits), "hgg": f32(hg_norm_g), "retg": f32(ret_norm_g),
        "wbh": f32(w_branch_hg)[0], "wbr": f32(w_branch_ret)[0], "wo": f32(w_out)[0],
        "l1g": f32(ln1_g), "l1b": f32(ln1_b), "l2g": f32(ln2_g), "l2b": f32(ln2_b),
        "wgr": np.ascontiguousarray(np.concatenate([f32(w_group)[0], f32(w_router)[0]], 1)),
        "bgr": np.ascontiguousarray(np.concatenate([f32(b_group), f32(b_router)], 1)),
        "w1": f32(w1)[0], "w3": f32(w3)[0], "w2": f32(w2)[0],
        "invf": invf, "rexpo": _ret_expo(),
    }
    in_maps = []
    for c in range(ncores):
        b, half = c // 2, c % 2
        xb, pb = x[b], positions[b]
        if half == 1:
            xb, pb = xb[::-1], pb[::-1]
        m = dict(common)
        m["x"] = np.ascontiguousarray(xb)
        m["pos"] = np.ascontiguousarray(pb).reshape(1, L)
        m["w_in"] = wins[half]
        in_maps.append(m)
    res = run_bass_kernel_spmd(nc, in_maps, core_ids=list(range(ncores)))
    out = np.empty((B, L, D), np.float32)
    for c in range(ncores):
        b, half = c // 2, c % 2
        o = np.asarray(res.results[c]["out"])
        if half == 0:
            out[b, :T] = o
        else:
            out[b, T:] = o[::-1]
    if _debug:
        return out, res.results
    return out
```
